# Optimizing a Trainium2 kernel written in Bass

```python
import math
import jax, jax.numpy as jnp
from jax import lax
import numpy as np

D_MODEL = 1024
BATCH = 8
SEQ = 2048
DEPTH = 1

ROPE_THETA = 500000.0
POS_OFFSET_MAX = 4096
MLA_HEADS = 8
MLA_NOPE = 64
MLA_ROPE = 32
MLA_V = 64
Q_LORA = 256
KV_LORA = 128
ATTN_QBLOCK = 128
MOBA_HEADS = 8
MOBA_HD = 64
MOBA_ROT = MOBA_HD // 4
MOBA_BLOCK = 256
MOBA_TOPK = 3
MOBA_QCHUNK = 128
MIX_WIDTH = MLA_HEADS * MLA_V + MOBA_HEADS * MOBA_HD
IN_COLS = Q_LORA + KV_LORA + MLA_ROPE + 3 * MOBA_HEADS * MOBA_HD
N_EXPERTS = 32
MOE_TOPK = 4
D_FF = 1024
SWIGLU_LIMIT = 7.0
SWIGLU_ALPHA = 1.702
MOE_BLOCK = 128
DN_ALPHA = (2.0 * DEPTH) ** 0.25
DN_BETA = (8.0 * DEPTH) ** -0.25

kernel_name = 'hymba_mla_moba_gptoss_deepnorm'


def _rms_norm(x, g, eps=1e-6):
    xf = x.astype(jnp.float32)
    y = xf * lax.rsqrt(jnp.mean(xf * xf, axis=-1, keepdims=True) + eps)
    return (y * g.astype(jnp.float32)).astype(x.dtype)


def _layer_norm(x, g, b, eps=1e-5):
    xf = x.astype(jnp.float32)
    mu = jnp.mean(xf, axis=-1, keepdims=True)
    var = jnp.mean(jnp.square(xf - mu), axis=-1, keepdims=True)
    y = (xf - mu) * lax.rsqrt(var + eps)
    return (y * g.astype(jnp.float32) + b.astype(jnp.float32)).astype(x.dtype)


def _rope_tables(positions, d_rot):
    inv_freq = ROPE_THETA ** (-jnp.arange(0, d_rot, 2, dtype=jnp.float32) / d_rot)
    ang = positions.astype(jnp.float32)[..., None] * inv_freq
    return jnp.cos(ang), jnp.sin(ang)


def _apply_rope(x, cos, sin):
    half = x.shape[-1] // 2
    x1, x2 = x[..., :half], x[..., half:]
    c = cos.astype(x.dtype)
    s = sin.astype(x.dtype)
    return jnp.concatenate([x1 * c - x2 * s, x2 * c + x1 * s], axis=-1)


def _mla(q_lat, kv_lat, k_rope_raw, cos, sin, q_a_norm, w_q_b, kv_a_norm, w_kv_b):
    B, T = q_lat.shape[:2]
    q = (_rms_norm(q_lat, q_a_norm) @ w_q_b).reshape(B, T, MLA_HEADS, MLA_NOPE + MLA_ROPE)
    q_nope = q[..., :MLA_NOPE]
    q_rope = _apply_rope(q[..., MLA_NOPE:], cos[:, :, None, :], sin[:, :, None, :])
    kv = (_rms_norm(kv_lat, kv_a_norm) @ w_kv_b).reshape(B, T, MLA_HEADS, MLA_NOPE + MLA_V)
    k_nope = kv[..., :MLA_NOPE].transpose(0, 2, 1, 3)
    v = kv[..., MLA_NOPE:].transpose(0, 2, 1, 3)
    k_rope = _apply_rope(k_rope_raw, cos, sin)
    nqb = T // ATTN_QBLOCK
    qn_b = q_nope.reshape(B, nqb, ATTN_QBLOCK, MLA_HEADS, MLA_NOPE).transpose(1, 0, 3, 2, 4)
    qr_b = q_rope.reshape(B, nqb, ATTN_QBLOCK, MLA_HEADS, MLA_ROPE).transpose(1, 0, 3, 2, 4)
    scale = 1.0 / math.sqrt(MLA_NOPE + MLA_ROPE)
    kpos = jnp.arange(T)

    def q_block(args):
        i, qn_i, qr_i = args
        s = (jnp.einsum('bhqd,bhkd->bhqk', qn_i, k_nope)
             + jnp.einsum('bhqd,bkd->bhqk', qr_i, k_rope)).astype(jnp.float32) * scale
        qpos = i * ATTN_QBLOCK + jnp.arange(ATTN_QBLOCK)
        s = jnp.where(kpos[None, :] <= qpos[:, None], s, -jnp.inf)
        p = jax.nn.softmax(s, axis=-1).astype(v.dtype)
        return jnp.einsum('bhqk,bhkd->bhqd', p, v)

    out = lax.map(q_block, (jnp.arange(nqb), qn_b, qr_b))
    return out.transpose(1, 0, 3, 2, 4).reshape(B, T, MLA_HEADS * MLA_V)


def _moba(q, k, v, cos, sin):
    B, T = q.shape[:2]
    H, d = MOBA_HEADS, MOBA_HD
    q = q.reshape(B, T, H, d)
    k = k.reshape(B, T, H, d)
    v = v.reshape(B, T, H, d)
    c4, s4 = cos[:, :, None, :], sin[:, :, None, :]
    q = jnp.concatenate([_apply_rope(q[..., :MOBA_ROT], c4, s4), q[..., MOBA_ROT:]], axis=-1)
    k = jnp.concatenate([_apply_rope(k[..., :MOBA_ROT], c4, s4), k[..., MOBA_ROT:]], axis=-1)
    q, k, v = (t.transpose(0, 2, 1, 3) for t in (q, k, v))
    nb = -(-T // MOBA_BLOCK)
    lp = nb * MOBA_BLOCK
    pad = ((0, 0), (0, 0), (0, lp - T), (0, 0))
    kblk = jnp.pad(k, pad).reshape(B, H, nb, MOBA_BLOCK, d)
    vblk = jnp.pad(v, pad).reshape(B, H, nb, MOBA_BLOCK, d)
    kmean = jnp.mean(kblk.astype(jnp.float32), axis=3)
    gate = jnp.einsum('bhtd,bhnd->bhtn', q.astype(jnp.float32), kmean)
    cur = jnp.arange(T) // MOBA_BLOCK
    gate = jnp.where(jnp.arange(nb)[None, :] < cur[:, None], gate, -jnp.inf)
    k_sel = min(MOBA_TOPK, nb)
    _, sel = lax.top_k(gate, k_sel)
    nqc = T // MOBA_QCHUNK
    q_items = q.reshape(B, H, nqc, MOBA_QCHUNK, d).transpose(0, 2, 1, 3, 4).reshape(B * nqc, H, MOBA_QCHUNK, d)
    sel_items = sel.reshape(B, H, nqc, MOBA_QCHUNK, k_sel).transpose(0, 2, 1, 3, 4).reshape(B * nqc, H, MOBA_QCHUNK, k_sel)
    scale = 1.0 / math.sqrt(d)
    hidx = jnp.arange(H)[:, None, None]

    def chunk(args):
        j, qc, selc = args
        b = j // nqc
        c = j % nqc
        kb = kblk[b]
        vb = vblk[b]
        ks = kb[hidx, selc]
        vs = vb[hidx, selc]
        qpos = c * MOBA_QCHUNK + jnp.arange(MOBA_QCHUNK)
        valid = jnp.arange(k_sel)[None, :] < (qpos // MOBA_BLOCK)[:, None]
        s_sel = jnp.einsum('hqd,hqkpd->hqkp', qc, ks).astype(jnp.float32) * scale
        s_sel = jnp.where(valid[None, :, :, None], s_sel, -jnp.inf).reshape(H, MOBA_QCHUNK, k_sel * MOBA_BLOCK)
        own = (c * MOBA_QCHUNK) // MOBA_BLOCK
        kown = lax.dynamic_index_in_dim(kb, own, axis=1, keepdims=False)
        vown = lax.dynamic_index_in_dim(vb, own, axis=1, keepdims=False)
        kpos = own * MOBA_BLOCK + jnp.arange(MOBA_BLOCK)
        s_own = jnp.einsum('hqd,hpd->hqp', qc, kown).astype(jnp.float32) * scale
        s_own = jnp.where(kpos[None, :] <= qpos[:, None], s_own, -jnp.inf)
        p = jax.nn.softmax(jnp.concatenate([s_sel, s_own], axis=-1), axis=-1).astype(vs.dtype)
        p_sel = p[..., :k_sel * MOBA_BLOCK].reshape(H, MOBA_QCHUNK, k_sel, MOBA_BLOCK)
        p_own = p[..., k_sel * MOBA_BLOCK:]
        return (jnp.einsum('hqkp,hqkpd->hqd', p_sel, vs)
                + jnp.einsum('hqp,hpd->hqd', p_own, vown))

    out = lax.map(chunk, (jnp.arange(B * nqc), q_items, sel_items))
    return out.reshape(B, nqc, H, MOBA_QCHUNK, d).transpose(0, 1, 3, 2, 4).reshape(B, T, H * d)


def _moe(x, w_router, b_router, w_gate, b_gate, w_up, b_up, w_down, b_down):
    B, T, D = x.shape
    n_tok = B * T
    xf = x.reshape(n_tok, D)
    logits = (xf @ w_router + b_router).astype(jnp.float32)
    top_vals, top_idx = lax.top_k(logits, MOE_TOPK)
    gates = jax.nn.softmax(top_vals, axis=-1)
    n_asg = n_tok * MOE_TOPK
    e_flat = top_idx.reshape(n_asg).astype(jnp.int32)
    order = jnp.argsort(e_flat, stable=True)
    sorted_e = e_flat[order]
    counts = jnp.bincount(e_flat, length=N_EXPERTS).astype(jnp.int32)
    padded = (counts + MOE_BLOCK - 1) // MOE_BLOCK * MOE_BLOCK
    pad_end = jnp.cumsum(padded).astype(jnp.int32)
    pad_start = pad_end - padded
    grp_start = (jnp.cumsum(counts) - counts).astype(jnp.int32)
    rank = jnp.arange(n_asg, dtype=jnp.int32) - grp_start[sorted_e]
    dest_sorted = (pad_start[sorted_e] + rank).astype(jnp.int32)
    dest = jnp.zeros((n_asg,), jnp.int32).at[order].set(dest_sorted)
    n_rows = n_asg + N_EXPERTS * MOE_BLOCK
    n_blk = n_rows // MOE_BLOCK
    row_tok = jnp.full((n_rows,), n_tok, jnp.int32).at[dest_sorted].set((order // MOE_TOPK).astype(jnp.int32))
    blk_start = jnp.arange(n_blk, dtype=jnp.int32) * MOE_BLOCK
    blk_expert = jnp.clip(jnp.searchsorted(pad_end, blk_start, side='right'), 0, N_EXPERTS - 1)
    x_pad = jnp.concatenate([xf, jnp.zeros((1, D), xf.dtype)], axis=0)

    def expert_block(args):
        e, rows = args
        xb = x_pad[rows]
        g = jnp.minimum(xb @ w_gate[e] + b_gate[e], SWIGLU_LIMIT)
        u = jnp.clip(xb @ w_up[e] + b_up[e], -SWIGLU_LIMIT, SWIGLU_LIMIT)
        h = g * jax.nn.sigmoid(SWIGLU_ALPHA * g) * (u + 1.0)
        return h @ w_down[e] + b_down[e]

    y_rows = lax.map(expert_block, (blk_expert, row_tok.reshape(n_blk, MOE_BLOCK))).reshape(n_rows, D)
    y = jnp.einsum('nk,nkd->nd', gates.astype(x.dtype), y_rows[dest.reshape(n_tok, MOE_TOPK)])
    return y.reshape(B, T, D)


def setup_inputs(seed: int = 0) -> dict:
    key = jax.random.key(seed)
    ks = jax.random.split(key, 32)
    L, D = DEPTH, D_MODEL
    nrm = jax.random.normal
    sD = D ** -0.5
    x = nrm(ks[0], (BATCH, SEQ, D), jnp.float32)
    offs = jax.random.randint(ks[1], (BATCH, 1), 0, POS_OFFSET_MAX, dtype=jnp.int32)
    positions = (offs + jnp.arange(SEQ, dtype=jnp.int32)[None, :]).astype(jnp.int32)
    mh = MOBA_HEADS * MOBA_HD
    w_in = jnp.concatenate([
        nrm(ks[2], (L, D, Q_LORA)) * sD,
        nrm(ks[3], (L, D, KV_LORA)) * sD,
        nrm(ks[4], (L, D, MLA_ROPE)) * sD,
        nrm(ks[5], (L, D, mh)) * sD,
        nrm(ks[6], (L, D, mh)) * sD,
        nrm(ks[7], (L, D, mh)) * sD * DN_BETA,
    ], axis=-1)
    q_a_norm = 1.0 + 0.01 * nrm(ks[8], (L, Q_LORA))
    w_q_b = nrm(ks[9], (L, Q_LORA, MLA_HEADS * (MLA_NOPE + MLA_ROPE))) * Q_LORA ** -0.5
    kv_a_norm = 1.0 + 0.01 * nrm(ks[10], (L, KV_LORA))
    wk_b = nrm(ks[11], (L, KV_LORA, MLA_HEADS, MLA_NOPE)) * KV_LORA ** -0.5
    wv_b = nrm(ks[12], (L, KV_LORA, MLA_HEADS, MLA_V)) * KV_LORA ** -0.5 * DN_BETA
    w_kv_b = jnp.concatenate([wk_b, wv_b], axis=-1).reshape(L, KV_LORA, MLA_HEADS * (MLA_NOPE + MLA_V))
    w_o = nrm(ks[13], (L, MIX_WIDTH, D)) * MIX_WIDTH ** -0.5 * DN_BETA
    ln1_g = 1.0 + 0.01 * nrm(ks[14], (L, D))
    ln1_b = 0.01 * nrm(ks[15], (L, D))
    w_router = nrm(ks[16], (L, D, N_EXPERTS)) * sD
    b_router = 0.01 * nrm(ks[17], (L, N_EXPERTS))
    w_gate = nrm(ks[18], (L, N_EXPERTS, D, D_FF)) * sD
    b_gate = 0.01 * nrm(ks[19], (L, N_EXPERTS, D_FF))
    w_up = nrm(ks[20], (L, N_EXPERTS, D, D_FF)) * sD
    b_up = 0.01 * nrm(ks[21], (L, N_EXPERTS, D_FF))
    w_down = nrm(ks[22], (L, N_EXPERTS, D_FF, D)) * D_FF ** -0.5 * DN_BETA
    b_down = 0.01 * nrm(ks[23], (L, N_EXPERTS, D))
    ln2_g = 1.0 + 0.01 * nrm(ks[24], (L, D))
    ln2_b = 0.01 * nrm(ks[25], (L, D))
    return {'x': x, 'positions': positions, 'w_in': w_in, 'q_a_norm': q_a_norm, 'w_q_b': w_q_b,
            'kv_a_norm': kv_a_norm, 'w_kv_b': w_kv_b, 'w_o': w_o, 'ln1_g': ln1_g, 'ln1_b': ln1_b,
            'w_router': w_router, 'b_router': b_router, 'w_gate': w_gate, 'b_gate': b_gate,
            'w_up': w_up, 'b_up': b_up, 'w_down': w_down, 'b_down': b_down,
            'ln2_g': ln2_g, 'ln2_b': ln2_b}


def reference(x, positions, w_in, q_a_norm, w_q_b, kv_a_norm, w_kv_b, w_o, ln1_g, ln1_b,
              w_router, b_router, w_gate, b_gate, w_up, b_up, w_down, b_down, ln2_g, ln2_b):
    cos_a, sin_a = _rope_tables(positions, MLA_ROPE)
    cos_b, sin_b = _rope_tables(positions, MOBA_ROT)
    o1 = Q_LORA
    o2 = o1 + KV_LORA
    o3 = o2 + MLA_ROPE
    mh = MOBA_HEADS * MOBA_HD
    h = x
    for l in range(DEPTH):
        proj = h @ w_in[l]
        a = _mla(proj[..., :o1], proj[..., o1:o2], proj[..., o2:o3], cos_a, sin_a,
                 q_a_norm[l], w_q_b[l], kv_a_norm[l], w_kv_b[l])
        m = _moba(proj[..., o3:o3 + mh], proj[..., o3 + mh:o3 + 2 * mh], proj[..., o3 + 2 * mh:],
                  cos_b, sin_b)
        mix = jnp.concatenate([a, m], axis=-1) @ w_o[l]
        h = _layer_norm(DN_ALPHA * h + mix, ln1_g[l], ln1_b[l])
        ffn = _moe(h, w_router[l], b_router[l], w_gate[l], b_gate[l], w_up[l], b_up[l], w_down[l], b_down[l])
        h = _layer_norm(DN_ALPHA * h + ffn, ln2_g[l], ln2_b[l])
    return h
```

```python
import math
from contextlib import ExitStack

import numpy as np
import ml_dtypes

import concourse.bass as bass
import concourse.mybir as mybir
from concourse.bass_utils import run_bass_kernel_spmd

F32 = mybir.dt.float32
BF16 = mybir.dt.bfloat16
I32 = mybir.dt.int32
U32 = mybir.dt.uint32
ALU = mybir.AluOpType
AF = mybir.ActivationFunctionType
AX = mybir.AxisListType

T = 2048
D = 1024
NE = 32
CAP = 384
NSLOT = NE * CAP
NBLK = CAP // 128
NPRE = 9
DN_ALPHA = 2.0 ** 0.25
NEG = -30000.0
SIGC = float(1.0 / (1.0 + math.exp(-1.702 * 7.0)))

COMPUTE = ("pe", "act", "dve", "pool")
SAME_ENGINE_SYNC = {"act": True, "dve": True, "pool": True, "pe": False}
DMA_POOL = {"sync": 24, "act": 8, "pool": 16}


class Op:
    __slots__ = ("eng", "fn", "dma", "deps", "idx", "has_dep", "tok", "pre")

    def __init__(self, eng, fn, dma, idx):
        self.eng = eng
        self.fn = fn
        self.dma = dma
        self.deps = set()
        self.idx = idx
        self.has_dep = False
        self.tok = None
        self.pre = None


class Sched:
    def __init__(self):
        self.ops = []
        self.last_writer = {}
        self.readers = {}

    def op(self, eng, fn, reads=(), writes=(), dma=False):
        o = Op(eng, fn, dma, len(self.ops))
        self.ops.append(o)
        reads = list(reads) + ["PHASE"]
        writes = list(writes) + [r for r in reads if isinstance(r, tuple) and r[0] == "ps" and r not in writes]
        for r in reads:
            w = self.last_writer.get(r)
            if w is not None:
                o.deps.add(w)
        for wkey in writes:
            w = self.last_writer.get(wkey)
            if w is not None:
                o.deps.add(w)
            rd = self.readers.get(wkey)
            if rd:
                for x in rd["c"].values():
                    o.deps.add(x)
                for x in rd["d"]:
                    o.deps.add(x)
            self.last_writer[wkey] = o
            self.readers[wkey] = {"c": {}, "d": []}
        for r in reads:
            rd = self.readers.setdefault(r, {"c": {}, "d": []})
            if dma:
                rd["d"].append(o)
            else:
                rd["c"][eng] = o
        o.deps.discard(o)
        return o

    def dma(self, eng, out, in_, reads=(), writes=(), **kw):
        return self.op(eng, lambda e: e.dma_start(out=out, in_=in_, **kw), reads, writes, dma=True)

    def emit(self, sems):
        ops = self.ops
        for o in ops:
            keep = set()
            for d in o.deps:
                if (not d.dma) and (not o.dma) and d.eng == o.eng and not SAME_ENGINE_SYNC[o.eng]:
                    continue
                keep.add(d)
            o.deps = keep
            for d in keep:
                d.has_dep = True
        cnt = {e: 0 for e in COMPUTE}
        dcnt = {e: 0 for e in DMA_POOL}
        for o in ops:
            if o.dma:
                i = dcnt[o.eng]
                dcnt[o.eng] += 1
                n = DMA_POOL[o.eng]
                sem = sems["d_%s_%d" % (o.eng, i % n)]
                o.tok = (sem, 16 * (i // n + 1))
                o.pre = (sem, 16 * (i // n)) if i >= n else None
            elif o.has_dep:
                cnt[o.eng] += 1
                o.tok = (sems["c_" + o.eng], cnt[o.eng])
        self.final_dma = []
        for e in DMA_POOL:
            n = DMA_POOL[e]
            tot = dcnt[e]
            for j in range(min(n, tot)):
                k = (tot - 1 - j) // n + 1
                self.final_dma.append((sems["d_%s_%d" % (e, j)], 16 * k))
        self.streams = {e: [] for e in ("pe", "act", "dve", "pool", "sync")}
        for o in ops:
            self.streams[o.eng].append(o)

    def run_stream(self, name, eng, extra_final=()):
        waited = {}

        def wait(sem, val):
            key = id(sem)
            if waited.get(key, 0) < val:
                eng.wait_ge(sem, val)
                waited[key] = val

        for o in self.streams[name]:
            for d in sorted(o.deps, key=lambda x: x.idx):
                wait(*d.tok)
            if o.pre is not None:
                wait(*o.pre)
            ins = o.fn(eng)
            if o.tok is not None:
                ins.then_inc(o.tok[0], 16 if o.dma else 1)
        for (sem, val) in extra_final:
            wait(sem, val)


def make_sems(nc, stack):
    sems = {}
    for e in COMPUTE:
        sems["c_" + e] = stack.enter_context(nc.semaphore("c_" + e))
    for e, n in DMA_POOL.items():
        for i in range(n):
            sems["d_%s_%d" % (e, i)] = stack.enter_context(nc.semaphore("d_%s_%d" % (e, i)))
    return sems


def run_block(nc, sched, sems):
    sched.emit(sems)
    finals = sched.final_dma
    with nc.Block() as block:
        @block.sync
        def _(eng):
            sched.run_stream("sync", eng, extra_final=finals)

        @block.scalar
        def _(eng):
            sched.run_stream("act", eng)

        @block.vector
        def _(eng):
            sched.run_stream("dve", eng)

        @block.gpsimd
        def _(eng):
            sched.run_stream("pool", eng)

        @block.tensor
        def _(eng):
            sched.run_stream("pe", eng)


DT_SIZE = {F32: 4, BF16: 2, I32: 4, U32: 4}


class Arena:
    def __init__(self, ar, size_f32):
        self.ar = ar
        self.size = size_f32
        self.off = 0
        self.peak = 0

    def reset(self, off=0):
        self.off = off

    def alloc(self, shape, dtype):
        n = 1
        for s in shape[1:]:
            n *= s
        n32 = (n * DT_SIZE[dtype] + 3) // 4
        n32 = (n32 + 1) // 2 * 2
        assert self.off + n32 <= min(self.size, getattr(self, "limit", self.size)), ("arena overflow", self.off, n32, self.size)
        v = self.ar[:, self.off:self.off + n32]
        self.off += n32
        self.peak = max(self.peak, self.off)
        if dtype != F32:
            v = v.bitcast(dtype)
        v = v[:, 0:n]
        if len(shape) == 3:
            v = v.rearrange("p (a b) -> p a b", a=shape[1])
        elif len(shape) == 4:
            v = v.rearrange("p (a b c) -> p a b c", a=shape[1], b=shape[2])
        return v[0:shape[0]]


class _Stop(Exception):
    pass


def build_program(debug=False, stop=99):
    nc = bass.Bass("TRN2", target_bir_lowering=False)
    S = Sched()

    def dram_in(name, shape, dt):
        return nc.dram_tensor(name, list(shape), dt, kind="ExternalInput").ap()

    xT = dram_in("xT", [D, T], F32)
    xtok = dram_in("xtok", [T, D], F32)
    pos = dram_in("pos", [T], I32)
    w1a = dram_in("w1a", [D, 448], F32)
    w1b = dram_in("w1b", [D, 1792], F32)
    wq = dram_in("wq", [256, 768], F32)
    wqs = dram_in("wqs", [256, 768], F32)
    qg = dram_in("qg", [128, 2], F32)
    wk = dram_in("wk", [128, 512], F32)
    wv = dram_in("wv", [128, 512], F32)
    kvg = dram_in("kvg", [128, 1], F32)
    wo = dram_in("wo", [D, D], F32)
    ln1g = dram_in("ln1g", [D], F32)
    ln1b = dram_in("ln1b", [D], F32)
    wr = dram_in("wr", [D, NE], F32)
    br = dram_in("br", [1, NE], F32)
    wg = dram_in("wg", [NE, D, D], F32)
    wu = dram_in("wu", [NE, D, D], F32)
    wd = dram_in("wd", [NE, D, D], F32)
    bg = dram_in("bg", [128, NE * 8], F32)
    bu = dram_in("bu", [128, NE * 8], F32)
    bd = dram_in("bd", [NE, D], F32)
    ln2g = dram_in("ln2g", [D], F32)
    ln2b = dram_in("ln2b", [D], F32)
    cbf = dram_in("cbf", [128, 384], BF16)
    cf = dram_in("cf", [128, 176], F32)
    cpm = dram_in("cpm", [128, 1024], F32)
    cind = dram_in("cind", [8, T], BF16)
    y = nc.dram_tensor("y", [T, D], F32, kind="ExternalOutput").ap()
    XG = nc.dram_tensor("XG", [NSLOT + 128, D], BF16, kind="Internal").ap()
    YG = nc.dram_tensor("YG", [NSLOT + 128, D], F32, kind="Internal").ap()
    WB = nc.dram_tensor("WB", [NPRE * 24 * 128, D], BF16, kind="Internal").ap()
    H1 = nc.dram_tensor("H1", [T, D], F32, kind="ExternalOutput" if debug else "Internal").ap()
    dbg = {}
    if debug:
        dbg["AT"] = nc.dram_tensor("dAT", [128, 8 * T], BF16, kind="ExternalOutput").ap()
        dbg["dest"] = nc.dram_tensor("ddest", [128, 64], I32, kind="ExternalOutput").ap()
        dbg["gate"] = nc.dram_tensor("dgate", [128, 64], F32, kind="ExternalOutput").ap()

    with ExitStack() as st:
        sems = make_sems(nc, st)
        ARN = 47616
        AR = st.enter_context(nc.sbuf_tensor("AR", [128, ARN], F32))
        CB = st.enter_context(nc.sbuf_tensor("CB", [128, 512], BF16))
        CF = st.enter_context(nc.sbuf_tensor("CF", [128, 176], F32))
        SM = st.enter_context(nc.sbuf_tensor("SM", [128, 2048], F32))
        PSUM = st.enter_context(nc.psum_tensor("PSUM", [128, 4096], F32))
        A = Arena(AR, ARN)

        def PS(b, rows=128, c0=0, c1=512):
            return PSUM[0:rows, b * 512 + c0:b * 512 + c1]

        IDENTb = CB[:, 0:128]
        TRIb = CB[:, 128:256]
        TRISb = CB[:, 256:384]
        ONESb = CB[:, 384:512]
        IDENTf = CF[:, 0:128]
        IOTA32 = CF[:, 128:160]
        HEADM = CF[:, 160:168]
        FA = CF[:, 168:170]
        FB = CF[:, 170:172]
        HALFPI = CF[:, 172:173]
        EPS6 = CF[:, 173:174]
        EPS5 = CF[:, 174:175]
        PCOL = CF[:, 175:176]
        smo = [0]

        def sm_alloc(n, dtype=F32):
            v = SM[:, smo[0]:smo[0] + n]
            smo[0] += n
            assert smo[0] <= 2048
            return v.bitcast(dtype) if dtype != F32 else v

        DEST = sm_alloc(64, I32)
        GATE = sm_alloc(64)
        ONESF = sm_alloc(128)
        BGS = sm_alloc(256)
        BG = sm_alloc(256)
        BU1 = sm_alloc(256)
        QG = sm_alloc(2)
        KVG = sm_alloc(2)
        BR = sm_alloc(32)

        def MM(out, lhsT, rhs, start, stop, R, W):
            S.op("pe", lambda e: e.matmul(out, lhsT, rhs, start=start, stop=stop), R, W)

        def TR(out, in_, ident, R, W):
            S.op("pe", lambda e: e.transpose(out, in_, ident), R, W)

        def ACTV(out, in_, func, R, W, **kw):
            S.op("act", lambda e: e.activation(out, in_, func, **kw), R, W)

        def CP(eng, out, in_, R, W):
            if eng == "act":
                S.op("act", lambda e: e.copy(out, in_), R, W)
            else:
                S.op(eng, lambda e: e.tensor_copy(out, in_), R, W)

        def TT(eng, out, a, b, op, R, W):
            S.op(eng, lambda e: e.tensor_tensor(out, a, b, op), R, W)

        def TS(eng, out, a, s1, op0, R, W, s2=None, op1=None):
            if op1 is None:
                S.op(eng, lambda e: e.tensor_scalar(out, a, s1, None, op0), R, W)
            else:
                S.op(eng, lambda e: e.tensor_scalar(out, a, s1, s2, op0, op1), R, W)

        def STT(out, in0, scalar, in1, op0, op1, R, W):
            S.op("dve", lambda e: e.scalar_tensor_tensor(out, in0, scalar, in1, op0, op1), R, W)

        def DMA(out, in_, R, W, eng="sync"):
            S.dma(eng, out, in_, R, W)

        def barrier():
            S.op("pool", lambda e: e.memset(SM[0:1, 2040:2042], 0.0), reads=[], writes=["PHASE"])

        def dump(name, ap2d, shape, dt, keys):
            if not debug:
                return
            t = nc.dram_tensor("dd_" + name, list(shape), dt, kind="ExternalOutput").ap()
            DMA(t, ap2d, keys, [])

        def ckpt(k):
            if k >= stop:
                raise _Stop()

        try:
            DMA(CB[:, 0:384], cbf, [], ["CB"])
            DMA(CF[:], cf, [], ["CF"])
            S.op("pool", lambda e: e.memset(ONESb, 1.0), [], ["CBo"])
            S.op("pool", lambda e: e.memset(ONESF, 1.0), [], ["ONESF"])
            DMA(BG, bg, [], ["BG"])
            DMA(BU1, bu, [], ["BU1"])
            DMA(QG, qg, [], ["QG"])
            DMA(KVG[:, 0:1], kvg, [], ["KVG"])
            DMA(BR[0:1, :], br, [], ["BR"])
            ZT = sm_alloc(512)
            S.op("pool", lambda e: e.memset(ZT, 0.0), [], ["ZT"])
            XGv = XG
            ZTb = ZT.bitcast(BF16)
            NZ = (NSLOT + 128) // 128
            zc = [0]

            def zfill(n):
                for _ in range(n):
                    if zc[0] >= NZ:
                        return
                    i = zc[0]
                    zc[0] += 1
                    DMA(XGv[i * 128:(i + 1) * 128, :], ZTb, ["ZT"], [("XGz", i)])

            TS("pool", BGS, BG, 1.702, ALU.mult, ["BG"], ["BGS"])
            TS("pool", BU1, BU1, 1.0, ALU.add, ["BU1"], ["BU1"])

            def build_tables(fcol, out_c, out_s, kc, ks, tmp):
                posi, ang, kk, rr = tmp
                DMA(posi, pos.partition_broadcast(128), [], ["t_posi"])
                CP("dve", ang, posi, ["t_posi"], ["t_ang"])
                TS("dve", ang, ang, fcol[:, 0:1], ALU.mult, ["t_ang", "CF"], ["t_ang"])
                TS("dve", kk, ang, 1.0 / (2.0 * math.pi), ALU.mult, ["t_ang"], ["t_k"], s2=12582912.0, op1=ALU.add)
                TS("dve", kk, kk, -12582912.0, ALU.add, ["t_k"], ["t_k"])
                C1 = 6.28125
                C2 = float(np.float32(2.0 * math.pi - 6.28125))
                STT(rr, kk, -C1, ang, ALU.mult, ALU.add, ["t_k", "t_ang"], ["t_r"])
                STT(rr, kk, -C2, rr, ALU.mult, ALU.add, ["t_k", "t_r"], ["t_r"])
                TS("dve", rr, rr, -3.1415925, ALU.max, ["t_r"], ["t_r"], s2=3.1415925, op1=ALU.min)
                ACTV(out_s, rr, AF.Sin, ["t_r", "CF"], [ks], scale=fcol[:, 1:2])
                STT(kk, rr, -1.0, rr, ALU.mult, ALU.max, ["t_r"], ["t_k"])
                ACTV(out_c, kk, AF.Sin, ["t_k", "CF"], [kc], scale=-1.0, bias=HALFPI)

            AT = A.alloc([128, 8, T], BF16)
            base_after_AT = A.off
            BGTOP = ARN - 4 * 512
            BGB = [AR[:, BGTOP + i * 512:BGTOP + (i + 1) * 512].bitcast(BF16) for i in range(4)]
            A.limit = BGTOP
            bg_seq = []

            def _bg_in(idx, src_ap):
                i = idx % 4
                return lambda: S.dma("pool", BGB[i], src_ap, [], [("BGB", i)])

            def _bg_out(idx):
                i = idx % 4
                return lambda: S.dma("pool", WB[idx * 128:(idx + 1) * 128, :], BGB[i], [("BGB", i)], [("WB", idx)])

            _ins, _outs = [], []
            for ei in range(NPRE):
                ee = NE - NPRE + ei
                for j in range(24):
                    idx = ei * 24 + j
                    if j < 16:
                        src = (wg if j % 2 == 0 else wu)[ee, (j // 2) * 128:(j // 2 + 1) * 128, :]
                    else:
                        src = wd[ee, (j - 16) * 128:(j - 15) * 128, :]
                    _ins.append(_bg_in(idx, src))
                    _outs.append(_bg_out(idx))
            nchunk = len(_ins)
            for k in range(nchunk + 2):
                if k < nchunk:
                    bg_seq.append(_ins[k])
                if k >= 2:
                    bg_seq.append(_outs[k - 2])
            bgc = [0]

            def bg(n):
                for _ in range(n):
                    if bgc[0] >= len(bg_seq):
                        return
                    bg_seq[bgc[0]]()
                    bgc[0] += 1


            TAc = A.alloc([128, T], F32)
            TAs = A.alloc([128, T], F32)
            off0 = A.off
            tmp_tab = [A.alloc([128, T], I32), A.alloc([128, T], F32), A.alloc([128, T], F32), A.alloc([128, T], F32)]
            build_tables(FA, TAc, TAs, "TAc", "TAs", tmp_tab)
            ckpt(0)
            barrier()
            A.reset(off0)
            QN = A.alloc([128, 2, T], BF16)
            KVN = A.alloc([128, T], BF16)
            KR = A.alloc([32, T], BF16)
            VALL = A.alloc([128, 16, 512], BF16)
            QAb = [A.alloc([128, T], BF16) for _ in range(2)]
            KAb = [A.alloc([128, T], BF16) for _ in range(2)]
            VAb = [A.alloc([128, 16, 128], BF16) for _ in range(2)]
            PT = [A.alloc([128, 512], BF16) for _ in range(4)]
            WQ = A.alloc([128, 2, 768], BF16)
            WQS = A.alloc([128, 2, 768], BF16)
            WK = A.alloc([128, 512], BF16)
            WV = A.alloc([128, 512], BF16)
            RC = [A.alloc([128, 512], F32) for _ in range(2)]
            T1 = A.alloc([128, 512], F32)
            T2 = A.alloc([128, 512], F32)
            ph12_common_end = A.off
            W1A = A.alloc([128, 8, 448], BF16)
            XG16 = [A.alloc([128, 8, 512], BF16) for _ in range(2)]
            XST = [A.alloc([128, 512], F32) for _ in range(4)]
            WST = [A.alloc([128, 768], F32) for _ in range(2)]
            SQ = [A.alloc([128, 512], BF16) for _ in range(2)]
            RQ = A.alloc([128, 512], F32)

            cast_rr = [0]
            CAST_ENG = ["act", "dve", "pool"]

            def cast(out, in_, R, W, eng=None):
                if eng is None:
                    eng = CAST_ENG[cast_rr[0] % 3]
                    cast_rr[0] += 1
                CP(eng, out, in_, R, W)

            for c in range(2):
                DMA(WST[0][:, 0:768], wq[c * 128:(c + 1) * 128, :], [], ["WST0"])
                TS("dve", WQ[:, c, :], WST[0][:, 0:768], QG[:, c:c + 1], ALU.mult, ["WST0", "QG"], ["WQ"])
                DMA(WST[1][:, 0:768], wqs[c * 128:(c + 1) * 128, :], [], ["WST1"])
                TS("pool", WQS[:, c, :], WST[1][:, 0:768], QG[:, c:c + 1], ALU.mult, ["WST1", "QG"], ["WQS"])
            DMA(WST[0][:, 0:512], wk, [], ["WST0"])
            TS("dve", WK, WST[0][:, 0:512], KVG[:, 0:1], ALU.mult, ["WST0", "KVG"], ["WK"])
            DMA(WST[1][:, 0:512], wv, [], ["WST1"])
            TS("pool", WV, WST[1][:, 0:512], KVG[:, 0:1], ALU.mult, ["WST1", "KVG"], ["WV"])
            for c in range(8):
                DMA(WST[c % 2][:, 0:448], w1a[c * 128:(c + 1) * 128, :], [], ["WST%d" % (c % 2)])
                cast(W1A[:, c, :], WST[c % 2][:, 0:448], ["WST%d" % (c % 2)], [("W1A", c)])

            psrr = [0]

            def nbank():
                b = psrr[0] % 8
                psrr[0] += 1
                return b

            xcnt = [0]

            def load_xgroup(g):
                xb = XG16[g % 2]
                for c in range(8):
                    i = xcnt[0] % 4
                    xcnt[0] += 1
                    DMA(XST[i], xT[c * 128:(c + 1) * 128, g * 512:(g + 1) * 512], [], [("XST", i)])
                    cast(xb[:, c, :], XST[i], [("XST", i)], [("XG16", g % 2, c)])
                    zfill(2)
                return xb

            def proj_chunk(xb, g, W, wkeys, c0, c1, bank):
                M = c1 - c0
                for c in range(8):
                    MM(PS(bank, M), W[:, c, c0:c1], xb[:, c, :], c == 0, c == 7,
                       [wkeys(c), ("XG16", g % 2, c)], [("ps", bank)])

            for g in range(4):
                gs = slice(g * 512, (g + 1) * 512)
                xb = load_xgroup(g)
                wkey = lambda c: ("W1A", c)
                for j in range(2):
                    b = nbank()
                    proj_chunk(xb, g, W1A, wkey, j * 128, (j + 1) * 128, b)
                    CP("act", QN[:, j, gs], PS(b), [("ps", b)], [("QN", j, g)])
                    ACTV(SQ[j], PS(b), AF.Square, [("ps", b)], [("SQ", j)])
                b = nbank()
                MM(PS(b), ONESb, SQ[0], True, False, ["CBo", ("SQ", 0)], [("ps", b)])
                MM(PS(b), ONESb, SQ[1], False, True, ["CBo", ("SQ", 1)], [("ps", b)])
                ACTV(RQ, PS(b), AF.Sqrt, [("ps", b), "CF"], ["RQ"], scale=1.0 / 256.0, bias=EPS6)
                S.op("dve", lambda e: e.reciprocal(RQ, RQ), ["RQ"], ["RQ"])
                for j in range(2):
                    TT("dve", QN[:, j, gs], QN[:, j, gs], RQ, ALU.mult, [("QN", j, g), "RQ"], [("QN", j, g)])
                b = nbank()
                proj_chunk(xb, g, W1A, wkey, 256, 384, b)
                CP("act", KVN[:, gs], PS(b), [("ps", b)], [("KVN", g)])
                ACTV(SQ[0], PS(b), AF.Square, [("ps", b)], [("SQ", 0)])
                b = nbank()
                MM(PS(b), ONESb, SQ[0], True, True, ["CBo", ("SQ", 0)], [("ps", b)])
                ACTV(RQ, PS(b), AF.Sqrt, [("ps", b), "CF"], ["RQ"], scale=1.0 / 128.0, bias=EPS6)
                S.op("dve", lambda e: e.reciprocal(RQ, RQ), ["RQ"], ["RQ"])
                TT("dve", KVN[:, gs], KVN[:, gs], RQ, ALU.mult, [("KVN", g), "RQ"], [("KVN", g)])
                ba = nbank()
                proj_chunk(xb, g, W1A, wkey, 384, 416, ba)
                bb = nbank()
                proj_chunk(xb, g, W1A, wkey, 416, 448, bb)
                TT("dve", T1[0:32, :], PS(ba, 32), TAc[0:32, gs], ALU.mult, [("ps", ba), "TAc"], ["T1"])
                TT("dve", T2[0:32, :], PS(bb, 32), TAs[0:32, gs], ALU.mult, [("ps", bb), "TAs"], ["T2"])
                TT("dve", KR[0:32, gs], T1[0:32, :], T2[0:32, :], ALU.add, ["T1", "T2"], [("KR", g)])
                for tt in range(4):
                    ti = g * 4 + tt
                    b = nbank()
                    MM(PS(b), KVN[:, ti * 128:(ti + 1) * 128], WV, True, True, [("KVN", g), "WV"], [("ps", b)])
                    CP("act", VALL[:, ti, :], PS(b), [("ps", b)], [("VALL", ti)])
            if stop == 1:
                dump("QN", QN.rearrange("p a t -> p (a t)"), [128, 2 * T], BF16, [("QN", j, g) for j in range(2) for g in range(4)])
                dump("KVN", KVN, [128, T], BF16, [("KVN", g) for g in range(4)])
                dump("KR", KR, [32, T], BF16, [("KR", g) for g in range(4)])
                dump("TAc", TAc, [128, T], F32, ["TAc"])
                dump("TAs", TAs, [128, T], F32, ["TAs"])
                dump("VALL", VALL.rearrange("p a t -> p (a t)"), [128, 16 * 512], BF16, [("VALL", ti) for ti in range(16)])
            ckpt(1)

            def attention(b, Kq, scale, parity, chunk, after_g, sbanks=(0, 1, 2)):
                Kq = 128
                NSB = len(sbanks)
                QA, KA, VA = QAb[b], KAb[b], VAb[b]
                steps = [(g, kt) for g in range(4) for kt in range(4 * g + 4)]
                n = len(steps)
                qa_keys = [("QA", b, "lo"), ("QA", b, "hi")]
                ka_keys = [("KA", b, "lo"), ("KA", b, "hi")]

                def qk(i):
                    g, kt = steps[i]
                    j = kt - 4 * g
                    c0 = 128 * max(0, j)
                    sb = sbanks[i % NSB]
                    MM(PS(sb, 128, c0, 512), KA[0:Kq, kt * 128:(kt + 1) * 128], QA[0:Kq, g * 512 + c0:(g + 1) * 512],
                       True, j < 0, qa_keys + ka_keys, [("ps", sb)])
                    if j >= 0:
                        MM(PS(sb, 128, c0, c0 + 128), IDENTb, TRIb, False, True, ["CB"], [("ps", sb)])

                def ex(i):
                    g, kt = steps[i]
                    c0 = 128 * max(0, kt - 4 * g)
                    sb = sbanks[i % NSB]
                    ACTV(PT[i % 4][:, c0:512], PS(sb, 128, c0, 512), AF.Exp, [("ps", sb)], [("PT", i % 4)], scale=scale)

                def pv(i):
                    g, kt = steps[i]
                    c0 = 128 * max(0, kt - 4 * g)
                    ob = 3 + (g % 2)
                    MM(PS(ob, 128, c0, 512), VA[:, kt, :], PT[i % 4][:, c0:512], kt == 0, kt == 4 * g + 3,
                       [("VA", b), ("PT", i % 4)], [("ps", ob)])

                def norm(g):
                    ob = 3 + (g % 2)
                    gs = slice(g * 512, (g + 1) * 512)
                    rc = RC[g % 2]
                    if parity == 0:
                        S.op("dve", lambda e: e.reciprocal(rc[0:64, :], PS(ob)[64:128, :]), [("ps", ob)], [("RC", g % 2)])
                        TT("dve", AT[0:64, chunk, gs], PS(ob)[0:64, :], rc[0:64, :], ALU.mult,
                           [("ps", ob), ("RC", g % 2)], [("AT", chunk, parity, g)])
                    else:
                        S.op("dve", lambda e: e.reciprocal(rc[64:128, :], PS(ob)[0:64, :]), [("ps", ob)], [("RC", g % 2)])
                        TT("dve", AT[64:128, chunk, gs], PS(ob)[64:128, :], rc[64:128, :], ALU.mult,
                           [("ps", ob), ("RC", g % 2)], [("AT", chunk, parity, g)])

                for i0 in range(NSB - 1):
                    qk(i0)
                for i in range(n):
                    ex(i)
                    pv(i)
                    if i + NSB - 1 < n:
                        qk(i + NSB - 1)
                    g, kt = steps[i]
                    if kt == 4 * g + 3:
                        norm(g)
                        after_g(g)
                        zfill(4)
                        bg(8)

            def mla_prep(h, g):
                b = h % 2
                gs = slice(g * 512, (g + 1) * 512)
                hs = slice(h * 96, (h + 1) * 96)
                MM(PS(5, 96), WQ[:, 0, hs], QN[:, 0, gs], True, False, ["WQ", ("QN", 0, g)], [("ps", 5)])
                MM(PS(5, 96), WQ[:, 1, hs], QN[:, 1, gs], False, True, ["WQ", ("QN", 1, g)], [("ps", 5)])
                MM(PS(6, 96), WQS[:, 0, hs], QN[:, 0, gs], True, False, ["WQS", ("QN", 0, g)], [("ps", 6)])
                MM(PS(6, 96), WQS[:, 1, hs], QN[:, 1, gs], False, True, ["WQS", ("QN", 1, g)], [("ps", 6)])
                MM(PS(7, 64), WK[:, h * 64:(h + 1) * 64], KVN[:, gs], True, True, ["WK", ("KVN", g)], [("ps", 7)])
                CP("act", QAb[b][0:64, gs], PS(5, 64), [("ps", 5)], [("QA", b, "lo")])
                TT("dve", T1[64:96, :], PS(5)[64:96, :], TAc[64:96, gs], ALU.mult, [("ps", 5), "TAc"], ["T1"])
                TT("dve", T2[64:96, :], PS(6)[64:96, :], TAs[64:96, gs], ALU.mult, [("ps", 6), "TAs"], ["T2"])
                TT("dve", QAb[b][64:96, gs], T1[64:96, :], T2[64:96, :], ALU.add, ["T1", "T2"], [("QA", b, "hi")])
                CP("act", KAb[b][0:64, gs], PS(7, 64), [("ps", 7)], [("KA", b, "lo")])
                if g == 0:
                    vsl = slice(0, 64) if b == 0 else slice(64, 128)
                    CP("pool", VAb[b][:, :, vsl], VALL[:, :, h * 64:(h + 1) * 64],
                       [("VALL", ti) for ti in range(16)], [("VA", b)])

            for b in range(2):
                osl = slice(64, 128) if b == 0 else slice(0, 64)
                S.op("pool", lambda e, ap=VAb[b][:, :, osl]: e.memset(ap, 1.0), [], [("VA", b)])
                S.op("pool", lambda e, ap=QAb[b][96:128, :]: e.memset(ap, 0.0), [], [("QA", b, "hi")])
                S.op("pool", lambda e, ap=KAb[b][96:128, :]: e.memset(ap, 0.0), [], [("KA", b, "hi")])
                DMA(KAb[b][64:96, :], KR[0:32, :], [("KR", g) for g in range(4)], [("KA", b, "hi")])
            for g in range(4):
                mla_prep(0, g)
            sc_mla = 1.0 / math.sqrt(96.0)
            for h in range(8):
                def after(g, h=h):
                    if h + 1 < 8:
                        mla_prep(h + 1, g)
                attention(h % 2, 96, sc_mla, h % 2, h // 2, after)
            ckpt(2)

            barrier()
            A.reset(base_after_AT)
            TBc = A.alloc([128, T], F32)
            TBs = A.alloc([128, T], F32)
            off1 = A.off
            tmp_tab = [A.alloc([128, T], I32), A.alloc([128, T], F32), A.alloc([128, T], F32), A.alloc([128, T], F32)]
            build_tables(FB, TBc, TBs, "TBc", "TBs", tmp_tab)
            barrier()
            A.reset(off1)
            QM = A.alloc([128, 4, T], BF16)
            KM = A.alloc([128, 4, T], BF16)
            VALLm = A.alloc([128, 16, 512], BF16)
            T1 = A.alloc([128, 512], F32)
            T2 = A.alloc([128, 512], F32)
            MASKT = A.alloc([64, T], BF16)
            KMF = A.alloc([128, 32], F32)
            KMB = A.alloc([128, 32], BF16)
            KMBLK = A.alloc([128, 4, 64], BF16)
            GM = [A.alloc([128, 64], F32) for _ in range(2)]
            MX = [A.alloc([128, 64], F32) for _ in range(2)]
            THR = [A.alloc([128, 8], F32) for _ in range(2)]
            SEL = [A.alloc([128, 64], F32) for _ in range(2)]
            MB = [A.alloc([128, 64], BF16) for _ in range(2)]
            CPM = A.alloc([128, 1024], F32)
            off2 = A.off
            W1B = A.alloc([128, 8, 1792], BF16)
            XG16 = [A.alloc([128, 8, 512], BF16) for _ in range(2)]
            XST = [A.alloc([128, 512], F32) for _ in range(4)]
            WSTb = [A.alloc([128, 1792], F32) for _ in range(2)]

            DMA(CPM, cpm, [], ["CPM"])
            for c in range(8):
                DMA(WSTb[c % 2], w1b[c * 128:(c + 1) * 128, :], [], ["WSTb%d" % (c % 2)])
                cast(W1B[:, c, :], WSTb[c % 2], ["WSTb%d" % (c % 2)], [("W1B", c)])
            for g in range(4):
                gs = slice(g * 512, (g + 1) * 512)
                xb = load_xgroup(g)
                wkey = lambda c: ("W1B", c)
                for (dst, nm, off) in ((QM, "QM", 0), (KM, "KM", 640)):
                    ba = nbank()
                    proj_chunk(xb, g, W1B, wkey, off, off + 128, ba)
                    bb = nbank()
                    proj_chunk(xb, g, W1B, wkey, off + 128, off + 256, bb)
                    TT("dve", T1, PS(ba), TBc[:, gs], ALU.mult, [("ps", ba), "TBc"], ["T1"])
                    TT("dve", T2, PS(bb), TBs[:, gs], ALU.mult, [("ps", bb), "TBs"], ["T2"])
                    TT("pool", dst[:, 0, gs], T1, T2, ALU.add, ["T1", "T2"], [(nm, g)])
                    for j in range(1, 4):
                        b = nbank()
                        proj_chunk(xb, g, W1B, wkey, off + 128 + 128 * j, off + 256 + 128 * j, b)
                        CP("act", dst[:, j, gs], PS(b), [("ps", b)], [(nm, g)])
                for tt in range(4):
                    ti = g * 4 + tt
                    b = nbank()
                    for c in range(8):
                        MM(PS(b), xb[:, c, tt * 128:(tt + 1) * 128], W1B[:, c, 1280:1792], c == 0, c == 7,
                           [("W1B", c), ("XG16", g % 2, c)], [("ps", b)])
                    CP("act", VALLm[:, ti, :], PS(b), [("ps", b)], [("VALLm", ti)])

            kmk = [("KM", g) for g in range(4)]
            S.op("dve", lambda e: e.tensor_reduce(KMF.rearrange("p (a b) -> p a b", a=4),
                                                  KM.rearrange("p a (n k) -> p a n k", n=8), AX.X, ALU.add),
                 kmk, ["KMF"])
            TS("dve", KMB, KMF, 1.0 / 256.0, ALU.mult, ["KMF"], ["KMB"])
            KMBv = KMB.rearrange("p (a n) -> p a n", a=4)
            for hh in range(8):
                TS("dve", KMBLK[:, :, hh * 8:(hh + 1) * 8], KMBv, HEADM[:, hh:hh + 1], ALU.mult, ["KMB", "CF"], ["KMBLK"])
            ckpt(3)

            barrier()
            A.reset(off2)
            QAb = [A.alloc([128, T], BF16) for _ in range(2)]
            KAb = [A.alloc([128, T], BF16) for _ in range(2)]
            VAb = [A.alloc([128, 16, 128], BF16) for _ in range(2)]
            PT = [A.alloc([128, 512], BF16) for _ in range(4)]
            RC = [A.alloc([128, 512], F32) for _ in range(2)]
            GMt = [A.alloc([128, 64], F32) for _ in range(16)]
            MXt = [A.alloc([128, 64], F32) for _ in range(16)]
            THRt = [A.alloc([128, 8], F32) for _ in range(16)]
            SELt = [A.alloc([128, 64], F32) for _ in range(16)]
            MBt = [A.alloc([128, 64], BF16) for _ in range(16)]

            gb = [nbank(), nbank()]
            for ti in range(16):
                bk = gb[ti // 8]
                c0 = (ti % 8) * 64
                for j in range(4):
                    MM(PS(bk, 128, c0, c0 + 64), QM[:, j, ti * 128:(ti + 1) * 128], KMBLK[:, j, :], j == 0, j == 3,
                       [("QM", ti // 4), "KMBLK"], [("ps", bk)])
            for ti in range(16):
                cur = ti // 2
                bk = gb[ti // 8]
                c0 = (ti % 8) * 64
                TT("dve", GMt[ti], PS(bk, 128, c0, c0 + 64), CPM[:, cur * 64:(cur + 1) * 64], ALU.add, [("ps", bk), "CPM"], [("GM", ti)])
            for ti in range(16):
                for hh in range(8):
                    S.op("dve", lambda e, o=MXt[ti][:, hh * 8:(hh + 1) * 8], i=GMt[ti][:, hh * 8:(hh + 1) * 8]: e.max(o, i),
                         [("GM", ti)], [("MX", ti, hh)])
            for ti in range(16):
                MXv = MXt[ti].rearrange("p (h n) -> p h n", h=8)
                TS("dve", THRt[ti], MXv[:, :, 2], -1e29, ALU.max, [("MX", ti, hh) for hh in range(8)], [("THR", ti)])
            for ti in range(16):
                for hh in range(8):
                    TS("dve", SELt[ti][:, hh * 8:(hh + 1) * 8], GMt[ti][:, hh * 8:(hh + 1) * 8], THRt[ti][:, hh:hh + 1], ALU.is_ge,
                       [("GM", ti), ("THR", ti)], [("SEL", ti, hh)])
            for ti in range(16):
                cur = ti // 2
                TT("dve", SELt[ti], SELt[ti], CPM[:, 512 + cur * 64:512 + (cur + 1) * 64], ALU.add,
                   [("SEL", ti, hh) for hh in range(8)] + ["CPM"], [("SEL", ti)])
            for ti in range(16):
                TS("dve", MBt[ti], SELt[ti], -1.0, ALU.add, [("SEL", ti)], [("MB", ti)], s2=-NEG, op1=ALU.mult)
            tb = [nbank(), nbank()]
            for ti in range(16):
                bk = tb[ti // 8]
                c0 = (ti % 8) * 64
                pst = PSUM[0:64, bk * 512 + c0:bk * 512 + c0 + 64].bitcast(BF16)
                TR(pst, MBt[ti], IDENTb, [("MB", ti), "CB"], [("ps", bk)])
            for hb in range(2):
                bk = tb[hb]
                pall = PSUM[0:64, bk * 512:(bk + 1) * 512].bitcast(BF16)
                CP("dve", MASKT[:, hb * 1024:(hb + 1) * 1024], pall, [("ps", bk)], [("MASKT", hb * 8 + q) for q in range(8)])

            def moba_prep(h):
                b = h % 2
                for j in range(4):
                    DMA(QAb[b][16 * j:16 * j + 16, :], QM[16 * h:16 * h + 16, j, :], [("QM", g) for g in range(4)], [("QA", b, "lo")])
                    DMA(KAb[b][16 * j:16 * j + 16, :], KM[16 * h:16 * h + 16, j, :], kmk, [("KA", b, "lo")])
                DMA(QAb[b][64:72, :], MASKT[8 * h:8 * h + 8, :], [("MASKT", ti) for ti in range(16)], [("QA", b, "hi")])
                vsl = slice(0, 64) if b == 0 else slice(64, 128)
                CP("pool", VAb[b][:, :, vsl], VALLm[:, :, h * 64:(h + 1) * 64], [("VALLm", ti) for ti in range(16)], [("VA", b)])

            for b in range(2):
                osl = slice(64, 128) if b == 0 else slice(0, 64)
                S.op("pool", lambda e, ap=VAb[b][:, :, osl]: e.memset(ap, 1.0), [], [("VA", b)])
                S.op("pool", lambda e, ap=QAb[b][64:128, :]: e.memset(ap, 0.0), [], [("QA", b, "hi")])
                S.op("pool", lambda e, ap=KAb[b][64:128, :]: e.memset(ap, 0.0), [], [("KA", b, "hi")])
                DMA(KAb[b][64:72, :], cind, [], [("KA", b, "hi")])
            moba_prep(0)
            for h in range(8):
                def after(g, h=h):
                    if g == 0 and h + 1 < 8:
                        moba_prep(h + 1)
                attention(h % 2, 72, 0.125, h % 2, 4 + h // 2, after, sbanks=(0, 1, 2, 5, 6, 7))
            zfill(1000)
            bg(100000)
            ckpt(4)

            if debug:
                DMA(dbg["AT"], AT.rearrange("p a t -> p (a t)"),
                    [("AT", c, p, g) for c in range(8) for p in range(2) for g in range(4)], [])
            barrier()
            A.limit = ARN
            A.reset(base_after_AT)
            WO = A.alloc([128, 8, D], BF16)
            WOST = [A.alloc([128, D], F32) for _ in range(2)]
            G1 = A.alloc([128, D], F32)
            B1 = A.alloc([128, D], F32)
            WRf = A.alloc([128, 8, NE], F32)
            XB = [A.alloc([128, D], F32) for _ in range(8)]
            XBb = [A.alloc([128, D], BF16) for _ in range(8)]
            H1T = [A.alloc([128, 8, 128], F32) for _ in range(4)]
            MASKS = A.alloc([128, 16, NE], BF16)
            ST5 = [A.alloc([128, 12], F32) for _ in range(8)]
            MV5 = [A.alloc([128, 2], F32) for _ in range(8)]
            RS5 = [A.alloc([128, 4], F32) for _ in range(8)]
            LGg = [A.alloc([128, 4, NE], F32) for _ in range(2)]
            POSg = [A.alloc([128, 4, NE], F32) for _ in range(2)]
            MX8 = [A.alloc([128, 8], F32) for _ in range(8)]
            IX8 = [A.alloc([128, 8], U32) for _ in range(8)]
            IXF = [A.alloc([128, 4], F32) for _ in range(8)]
            NMX = [A.alloc([128, 2], F32) for _ in range(8)]
            EXP4 = [A.alloc([128, 4], F32) for _ in range(8)]
            SUM4 = [A.alloc([128, 2], F32) for _ in range(8)]
            OH = [[A.alloc([128, NE], F32) for _ in range(4)] for _ in range(8)]
            PK = [A.alloc([128, 4], F32) for _ in range(8)]
            PK2 = [A.alloc([128, 4], F32) for _ in range(8)]
            DF = [A.alloc([128, 4], F32) for _ in range(8)]
            OV = [A.alloc([128, 4], F32) for _ in range(8)]

            for c in range(8):
                DMA(WOST[c % 2], wo[c * 128:(c + 1) * 128, :], [], [("WOST", c % 2)])
                cast(WO[:, c, :], WOST[c % 2], [("WOST", c % 2)], [("WO", c)])
            DMA(G1, ln1g.partition_broadcast(128), [], ["G1"])
            DMA(B1, ln1b.partition_broadcast(128), [], ["B1"])
            DMA(WRf, wr.rearrange("(c p) e -> p c e", p=128), [], ["WRf"])

            def layer_norm(zt, zkey, out, okey, st, mv, rs, Gb, Bb, p, eps_ap, gb_eng):
                for hf in range(2):
                    S.op("dve", lambda e, hf=hf: e.bn_stats(st[:, hf * 6:(hf + 1) * 6], zt[:, hf * 512:(hf + 1) * 512]),
                         [zkey], [("ST", p)])
                    yield
                S.op("dve", lambda e: e.bn_aggr(mv, st), [("ST", p)], [("MV", p)])
                yield
                ACTV(rs[:, 0:1], mv[:, 1:2], AF.Sqrt, [("MV", p), "CF"], [("RS", p)], bias=eps_ap, scale=1.0)
                yield
                S.op("dve", lambda e: e.reciprocal(rs[:, 1:2], rs[:, 0:1]), [("RS", p)], [("RS", p)])
                yield
                TS("dve", rs[:, 2:3], mv[:, 0:1], rs[:, 1:2], ALU.mult, [("MV", p), ("RS", p)], [("RS", p)], s2=-1.0, op1=ALU.mult)
                yield
                ACTV(out, zt, AF.Identity, [zkey, ("RS", p)], [okey], bias=rs[:, 2:3], scale=rs[:, 1:2])
                yield
                TT(gb_eng, out, out, Gb, ALU.mult, [okey, "G1", "G2"], [okey])
                yield
                TT(gb_eng, out, out, Bb, ALU.add, [okey, "B1", "B2"], [okey])
                yield

            atkeys = [("AT", c, p, g) for c in range(8) for p in range(2) for g in range(4)]
            def pairbank(T_):
                return 2 * (T_ % 3)

            def stageA(g):
                b = g % 2
                tiles = [(t, g * 4 + t, b * 4 + t) for t in range(4)]
                for t, T_, sl in tiles:
                    DMA(XB[sl], xtok[T_ * 128:(T_ + 1) * 128, :], [], [("XB", sl)])
                for sub in (tiles[0:3], tiles[3:4]):
                    for t, T_, sl in sub:
                        pb = pairbank(T_)
                        ts_ = slice(T_ * 128, (T_ + 1) * 128)
                        for hf in range(2):
                            for c in range(8):
                                MM(PS(pb + hf), AT[:, c, ts_], WO[:, c, hf * 512:(hf + 1) * 512], c == 0, c == 7,
                                   [("WO", c)], [("ps", pb + hf)])
                    for t, T_, sl in sub:
                        pb = pairbank(T_)
                        for hf in range(2):
                            hs = slice(hf * 512, (hf + 1) * 512)
                            STT(XB[sl][:, hs], XB[sl][:, hs], DN_ALPHA, PS(pb + hf), ALU.mult, ALU.add,
                                [("XB", sl), ("ps", pb + hf)], [("XB", sl)])
                for t, T_, sl in tiles:
                    for hf in range(2):
                        S.op("dve", lambda e, st=ST5[sl], z=XB[sl], hf=hf: e.bn_stats(st[:, hf * 6:(hf + 1) * 6], z[:, hf * 512:(hf + 1) * 512]),
                             [("XB", sl)], [("ST", sl)])
                for t, T_, sl in tiles:
                    S.op("dve", lambda e, mv=MV5[sl], st=ST5[sl]: e.bn_aggr(mv, st), [("ST", sl)], [("MV", sl)])
                for t, T_, sl in tiles:
                    ACTV(RS5[sl][:, 0:1], MV5[sl][:, 1:2], AF.Sqrt, [("MV", sl), "CF"], [("RS", sl)], bias=EPS5, scale=1.0)
                for t, T_, sl in tiles:
                    S.op("dve", lambda e, rs=RS5[sl]: e.reciprocal(rs[:, 1:2], rs[:, 0:1]), [("RS", sl)], [("RS", sl)])
                for t, T_, sl in tiles:
                    TS("dve", RS5[sl][:, 2:3], MV5[sl][:, 0:1], RS5[sl][:, 1:2], ALU.mult, [("MV", sl), ("RS", sl)], [("RS", sl)],
                       s2=-1.0, op1=ALU.mult)
                for t, T_, sl in tiles:
                    ACTV(XB[sl], XB[sl], AF.Identity, [("XB", sl), ("RS", sl)], [("XB", sl)], bias=RS5[sl][:, 2:3], scale=RS5[sl][:, 1:2])
                for t, T_, sl in tiles:
                    TT("dve", XB[sl], XB[sl], G1, ALU.mult, [("XB", sl), "G1"], [("XB", sl)])
                for t, T_, sl in tiles:
                    TT("dve", XB[sl], XB[sl], B1, ALU.add, [("XB", sl), "B1"], [("XB", sl)])
                for t, T_, sl in tiles:
                    DMA(H1[T_ * 128:(T_ + 1) * 128, :], XB[sl], [("XB", sl)], [("H1", T_)])
                for t, T_, sl in tiles:
                    CP("act", XBb[sl], XB[sl], [("XB", sl)], [("XBb", sl)])
                for t, T_, sl in tiles:
                    pb = pairbank(T_)
                    for hf in range(2):
                        for cc in range(4):
                            c = hf * 4 + cc
                            TR(PS(pb + hf, 128, cc * 128, (cc + 1) * 128), XB[sl][:, c * 128:(c + 1) * 128], IDENTf,
                               [("XB", sl), "CF"], [("ps", pb + hf)])
                        CP("act", H1T[t][:, hf * 4:(hf + 1) * 4, :], PS(pb + hf).rearrange("p (a b) -> p a b", a=4),
                           [("ps", pb + hf)], [("H1T", t, hf)])
                for t, T_, sl in tiles:
                    for c in range(8):
                        MM(PS(6, 128, t * NE, (t + 1) * NE), H1T[t][:, c, :], WRf[:, c, :], c == 0, False,
                           [("H1T", t, 0), ("H1T", t, 1), "WRf"], [("ps", 6)])
                    MM(PS(6, 128, t * NE, (t + 1) * NE), ONESF[0:1, :], BR[0:1, :], False, True, ["ONESF", "BR"], [("ps", 6)])
                CP("dve", LGg[b].rearrange("p a e -> p (a e)"), PS(6, 128, 0, 4 * NE), [("ps", 6)], [("LGg", b)])

            def stageB(g):
                b = g % 2
                tiles = [(t, g * 4 + t, b * 4 + t) for t in range(4)]
                for t, T_, sl in tiles:
                    S.op("dve", lambda e, o=MX8[sl], i=LGg[b][:, t, :]: e.max(o, i), [("LGg", b)], [("MX8", sl)])
                for t, T_, sl in tiles:
                    S.op("dve", lambda e, o=IX8[sl], m=MX8[sl], i=LGg[b][:, t, :]: e.max_index(o, m, i),
                         [("LGg", b), ("MX8", sl)], [("IX8", sl)])
                for t, T_, sl in tiles:
                    TS("dve", NMX[sl][:, 0:1], MX8[sl][:, 0:1], -1.0, ALU.mult, [("MX8", sl)], [("NMX", sl)])
                for t, T_, sl in tiles:
                    ACTV(EXP4[sl], MX8[sl][:, 0:4], AF.Exp, [("MX8", sl), ("NMX", sl)], [("EXP4", sl)], bias=NMX[sl][:, 0:1], scale=1.0)
                for t, T_, sl in tiles:
                    S.op("dve", lambda e, o=SUM4[sl], i=EXP4[sl]: e.tensor_reduce(o[:, 0:1], i, AX.X, ALU.add), [("EXP4", sl)], [("SUM4", sl)])
                for t, T_, sl in tiles:
                    S.op("dve", lambda e, o=SUM4[sl]: e.reciprocal(o[:, 1:2], o[:, 0:1]), [("SUM4", sl)], [("SUM4", sl)])
                for t, T_, sl in tiles:
                    TS("dve", GATE[:, T_ * 4:(T_ + 1) * 4], EXP4[sl], SUM4[sl][:, 1:2], ALU.mult, [("EXP4", sl), ("SUM4", sl)], [("GATE", T_)])
                for t, T_, sl in tiles:
                    TS("dve", MASKS[:, T_, :], LGg[b][:, t, :], MX8[sl][:, 3:4], ALU.is_ge, [("LGg", b), ("MX8", sl)], [("MASKS", T_)])
                for t, T_, sl in tiles:
                    MM(PS(7, 128, t * NE, (t + 1) * NE), TRISb, MASKS[:, T_, :], True, T_ == 0, ["CB", ("MASKS", T_)], [("ps", 7)])
                    for tj in range(T_):
                        MM(PS(7, 128, t * NE, (t + 1) * NE), ONESb, MASKS[:, tj, :], False, tj == T_ - 1, ["CBo", ("MASKS", tj)], [("ps", 7)])
                CP("dve", POSg[b].rearrange("p a e -> p (a e)"), PS(7, 128, 0, 4 * NE), [("ps", 7)], [("POSg", b)])
                for t, T_, sl in tiles:
                    CP("dve", IXF[sl], IX8[sl][:, 0:4], [("IX8", sl)], [("IXF", sl)])
                for k in range(4):
                    for t, T_, sl in tiles:
                        TS("dve", OH[sl][k], IOTA32, IXF[sl][:, k:k + 1], ALU.is_equal, ["CF", ("IXF", sl)], [("OH", sl, k)])
                for k in range(4):
                    for t, T_, sl in tiles:
                        TT("dve", OH[sl][k], OH[sl][k], POSg[b][:, t, :], ALU.mult, [("OH", sl, k), ("POSg", b)], [("OH", sl, k)])
                for k in range(4):
                    for t, T_, sl in tiles:
                        S.op("dve", lambda e, o=PK[sl], i=OH[sl][k], k=k: e.tensor_reduce(o[:, k:k + 1], i, AX.X, ALU.add),
                             [("OH", sl, k)], [("PK", sl, k)])
                for t, T_, sl in tiles:
                    STT(DF[sl], IXF[sl], float(CAP), PK[sl], ALU.mult, ALU.add, [("IXF", sl)] + [("PK", sl, k) for k in range(4)], [("DF", sl)])
                for t, T_, sl in tiles:
                    TS("dve", OV[sl], PK[sl], float(CAP), ALU.is_ge, [("PK", sl, k) for k in range(4)], [("OV", sl)])
                for t, T_, sl in tiles:
                    TS("dve", PK2[sl], DF[sl], -1.0, ALU.mult, [("DF", sl)], [("PK2", sl)], s2=PCOL, op1=ALU.add)
                for t, T_, sl in tiles:
                    TT("dve", PK2[sl], PK2[sl], OV[sl], ALU.mult, [("PK2", sl), ("OV", sl)], [("PK2", sl)])
                for t, T_, sl in tiles:
                    TT("dve", DF[sl], DF[sl], PK2[sl], ALU.add, [("DF", sl), ("PK2", sl)], [("DF", sl)])
                for t, T_, sl in tiles:
                    CP("dve", DEST[:, T_ * 4:(T_ + 1) * 4], DF[sl], [("DF", sl)], [("DEST", T_)])
                for t, T_, sl in tiles:
                    for k in range(4):
                        S.op("pool", lambda e, src=XBb[sl], T_=T_, k=k: e.indirect_dma_start(
                            out=XG, out_offset=bass.IndirectOffsetOnAxis(DEST[:, T_ * 4 + k:T_ * 4 + k + 1], 0),
                            in_=src, in_offset=None),
                            [("XBb", sl), ("DEST", T_)], [("XGs", T_, k)], dma=True)

            stageA(0)
            stageA(1)
            stageB(0)
            stageA(2)
            stageB(1)
            stageA(3)
            stageB(2)
            stageB(3)
            if debug:
                DMA(dbg["dest"], DEST, [("DEST", ti) for ti in range(16)], [])
                DMA(dbg["gate"], GATE, [("GATE", ti) for ti in range(16)], [])
            ckpt(5)

            barrier()
            A.reset(0)
            WGb = [A.alloc([128, 8, D], BF16) for _ in range(2)]
            WUb = [A.alloc([128, 8, D], BF16) for _ in range(2)]
            WDb = A.alloc([128, 8, D], BF16)
            NST = 8
            WST6 = [A.alloc([128, D], F32) for _ in range(NST)]
            XS = [A.alloc([128, D], BF16) for _ in range(NBLK)]
            XTE = [A.alloc([128, 8, CAP], BF16) for _ in range(2)]
            HTE = [A.alloc([128, 8, CAP], BF16) for _ in range(2)]
            SG = [A.alloc([128, CAP], F32) for _ in range(2)]
            GG = [A.alloc([128, CAP], F32) for _ in range(2)]
            UU = [A.alloc([128, CAP], F32) for _ in range(2)]
            TTm = [A.alloc([128, CAP], F32) for _ in range(2)]
            YS = [A.alloc([128, D], F32) for _ in range(NBLK)]
            BDB = [A.alloc([128, D], F32) for _ in range(2)]

            wcnt = [0]
            import os as _os
            W_CAST = ["act", "dve", "act"]
            if _os.environ.get("KDBG_WCAST"):
                W_CAST = _os.environ["KDBG_WCAST"].split(",")

            def _load(src_ap, dst_ap, key):
                i = wcnt[0] % NST
                eng = W_CAST[wcnt[0] % len(W_CAST)]
                wcnt[0] += 1
                DMA(WST6[i], src_ap, [], [("WST6", i)])
                CP(eng, dst_ap, WST6[i], [("WST6", i)], [key])

            def load_gu(e, j):
                c = j // 2
                if e >= NE - NPRE:
                    idx = (e - (NE - NPRE)) * 24 + j
                    dst, key = (WGb[e % 2][:, c, :], ("WG", e % 2, c)) if j % 2 == 0 else (WUb[e % 2][:, c, :], ("WU", e % 2, c))
                    DMA(dst, WB[idx * 128:(idx + 1) * 128, :], [], [key])
                    return
                if j % 2 == 0:
                    _load(wg[e, c * 128:(c + 1) * 128, :], WGb[e % 2][:, c, :], ("WG", e % 2, c))
                else:
                    _load(wu[e, c * 128:(c + 1) * 128, :], WUb[e % 2][:, c, :], ("WU", e % 2, c))

            def load_d(e, f):
                if e >= NE - NPRE:
                    idx = (e - (NE - NPRE)) * 24 + 16 + f
                    DMA(WDb[:, f, :], WB[idx * 128:(idx + 1) * 128, :], [], [("WD", f)])
                    return
                _load(wd[e, f * 128:(f + 1) * 128, :], WDb[:, f, :], ("WD", f))

            def load_xs(e):
                rd = [("XGs", ti, k) for ti in range(16) for k in range(4)] if e == 0 else []
                for blk in range(NBLK):
                    r0 = e * CAP + blk * 128
                    import os as _os
                    if _os.environ.get("KDBG_XSZERO"):
                        S.op("pool", lambda e, ap=XS[blk]: e.memset(ap, 0.5), [], [("XS", blk)])
                    else:
                        DMA(XS[blk], XG[r0:r0 + 128, :], rd, [("XS", blk)])
                DMA(BDB[e % 2], bd[e, :].partition_broadcast(128), [], [("BDB", e % 2)])

            def expert(e):
                eb = e % 2
                xte_keys = [("XTE", eb, c) for c in range(8)]
                for c in range(8):
                    pb = 6 + (c % 2)
                    pst = PSUM[:, pb * 512:pb * 512 + CAP // 2].bitcast(BF16)
                    for blk in range(NBLK):
                        TR(pst[:, blk * 128:(blk + 1) * 128], XS[blk][:, c * 128:(c + 1) * 128], IDENTb,
                           [("XS", blk), "CB"], [("ps", pb)])
                    CP("act", XTE[eb][:, c, :], pst, [("ps", pb)], [("XTE", eb, c)])
                import os as _os
                _sub = int(_os.environ.get("KDBG_SUB", 9))
                if _sub < 1:
                    return
                if e + 1 < NE:
                    load_xs(e + 1)
                if _sub < 2:
                    return
                for f in range(8):
                    pg = (f % 2) * 2
                    pu = pg + 1
                    fs = slice(f * 128, (f + 1) * 128)
                    for c in range(8):
                        MM(PS(pg, 128, 0, CAP), WGb[eb][:, c, fs], XTE[eb][:, c, :], c == 0, c == 7,
                           [("WG", eb, c)] + xte_keys, [("ps", pg)])
                    for c in range(8):
                        MM(PS(pu, 128, 0, CAP), WUb[eb][:, c, fs], XTE[eb][:, c, :], c == 0, c == 7,
                           [("WU", eb, c)] + xte_keys, [("ps", pu)])
                    q = f % 2
                    bcol = slice(e * 8 + f, e * 8 + f + 1)
                    if _os.environ.get("KDBG_NOSW"):
                        continue
                    TS("dve", GG[q], PS(pg, 128, 0, CAP), BG[:, bcol], ALU.add, [("ps", pg), "BG"], [("GG", q)], s2=7.0, op1=ALU.min)
                    ACTV(SG[q], GG[q], AF.Sigmoid, [("GG", q)], [("SG", q)], scale=1.702)
                    TS("dve", UU[q], PS(pu, 128, 0, CAP), BU1[:, bcol], ALU.add, [("ps", pu), "BU1"], [("UU", q)], s2=8.0, op1=ALU.min)
                    TT("dve", TTm[q], SG[q], GG[q], ALU.mult, [("SG", q), ("GG", q)], [("TTm", q)])
                    STT(HTE[eb][:, f, :], UU[q], -6.0, TTm[q], ALU.max, ALU.mult, [("UU", q), ("TTm", q)], [("HTE", eb, f)])
                    load_d(e, f)
                    if e + 1 < NE:
                        load_gu(e + 1, f)
                if _sub < 3:
                    return
                for blk in range(NBLK):
                    for hf in range(2):
                        pb = 4 + hf
                        for f in range(8):
                            MM(PS(pb), HTE[eb][:, f, blk * 128:(blk + 1) * 128], WDb[:, f, hf * 512:(hf + 1) * 512],
                               f == 0, f == 7, [("HTE", eb, f), ("WD", f)], [("ps", pb)])
                        TT("dve", YS[blk][:, hf * 512:(hf + 1) * 512], PS(pb), BDB[eb][:, hf * 512:(hf + 1) * 512], ALU.add,
                           [("ps", pb), ("BDB", eb)], [("YS", blk)])
                        if e + 1 < NE:
                            gi = blk * 2 + hf
                            for j in DOWN_SPREAD[gi]:
                                load_gu(e + 1, j)
                for blk in range(NBLK):
                    r0 = e * CAP + blk * 128
                    DMA(YG[r0:r0 + 128, :], YS[blk], [("YS", blk)], [("YG", e, blk)])

            S.op("pool", lambda e: e.memset(YS[0], 0.0), [], [("YS", 0)])
            DMA(YG[NSLOT:NSLOT + 128, :], YS[0], [("YS", 0)], [("YGtrash",)])
            DOWN_SPREAD = [[8, 9], [10], [11], [12, 13], [14], [15]]
            load_xs(0)
            for j in range(16):
                load_gu(0, j)
            import os as _os
            for e in range(int(_os.environ.get("KDBG_NE", NE))):
                expert(e)
            ckpt(6)

            barrier()
            A.reset(0)
            G2 = A.alloc([128, D], F32)
            B2 = A.alloc([128, D], F32)
            NB7 = 3
            YK = [[A.alloc([128, D], F32) for _ in range(4)] for _ in range(NB7)]
            HH = [A.alloc([128, D], F32) for _ in range(NB7)]
            ACC = [A.alloc([128, D], F32) for _ in range(NB7)]
            OUT = [A.alloc([128, D], F32) for _ in range(NB7)]
            ST7 = [A.alloc([128, 12], F32) for _ in range(NB7)]
            MV7 = [A.alloc([128, 2], F32) for _ in range(NB7)]
            RS7 = [A.alloc([128, 4], F32) for _ in range(NB7)]
            DMA(G2, ln2g.partition_broadcast(128), [], ["G2"])
            DMA(B2, ln2b.partition_broadcast(128), [], ["B2"])
            ygk = [("YG", e, blk) for e in range(NE) for blk in range(NBLK)]

            def fetch7(ti):
                p = ti % NB7
                ts_ = slice(ti * 128, (ti + 1) * 128)
                DMA(HH[p], H1[ts_, :], [("H1", ti)], [("HH", p)])
                for k in range(4):
                    S.op("pool", lambda e, p=p, ti=ti, k=k: e.indirect_dma_start(
                        out=YK[p][k], out_offset=None, in_=YG,
                        in_offset=bass.IndirectOffsetOnAxis(DEST[:, ti * 4 + k:ti * 4 + k + 1], 0)),
                        [("DEST", ti)] + (ygk if (ti == 0 and k == 0) else []), [("YK", p, k)], dma=True)

            def tile7(ti):
                p = ti % NB7
                ts_ = slice(ti * 128, (ti + 1) * 128)
                if ti + 2 < 16:
                    fetch7(ti + 2)
                yield
                for k in (0, 2):
                    S.op("act", lambda e, ap=YK[p][k], g=GATE[:, ti * 4 + k:ti * 4 + k + 1]: e.mul(ap, ap, g),
                         [("YK", p, k), ("GATE", ti)], [("YK", p, k)])
                    yield
                for k in (1, 3):
                    STT(YK[p][k], YK[p][k], GATE[:, ti * 4 + k:ti * 4 + k + 1], YK[p][k - 1], ALU.mult, ALU.add,
                        [("YK", p, k), ("YK", p, k - 1), ("GATE", ti)], [("YK", p, k)])
                    yield
                TT("pool", ACC[p], YK[p][1], YK[p][3], ALU.add, [("YK", p, 1), ("YK", p, 3)], [("ACC", p)])
                yield
                STT(ACC[p], HH[p], DN_ALPHA, ACC[p], ALU.mult, ALU.add, [("HH", p), ("ACC", p)], [("ACC", p)])
                yield
                yield from layer_norm(ACC[p], ("ACC", p), OUT[p], ("OUT", p), ST7[p], MV7[p], RS7[p], G2, B2, p, EPS5, "dve")
                DMA(y[ts_, :], OUT[p], [("OUT", p)], [("y", ti)])
                yield

            fetch7(0)
            fetch7(1)
            gens7 = []
            nxt7 = [0]
            st7 = {}
            LAG7 = 8
            while nxt7[0] < 16 or gens7:
                if nxt7[0] < 16 and len(gens7) < 2 and (not gens7 or st7[gens7[0][0]] >= LAG7):
                    gens7.append((nxt7[0], tile7(nxt7[0])))
                    st7[nxt7[0]] = 0
                    nxt7[0] += 1
                for item in list(gens7):
                    tid, gen = item
                    try:
                        next(gen)
                        st7[tid] += 1
                    except StopIteration:
                        gens7.remove(item)

        except _Stop:
            pass
        run_block(nc, S, sems)
    return nc


def _consts():
    bf = ml_dtypes.bfloat16
    ident = np.eye(128, dtype=np.float32)
    k = np.arange(128)[:, None]
    q = np.arange(128)[None, :]
    tri = np.where(k <= q, 0.0, NEG).astype(np.float32)
    tris = (k < q).astype(np.float32)
    cbf = np.concatenate([ident, tri, tris], axis=1).astype(bf)
    cf = np.zeros((128, 176), np.float32)
    cf[:, 0:128] = ident
    cf[:, 128:160] = np.arange(32, dtype=np.float32)[None, :]
    cf[:, 160:168] = (np.arange(128)[:, None] // 16 == np.arange(8)[None, :]).astype(np.float32)
    inv_a = np.power(np.float32(500000.0), -np.arange(0, 32, 2, dtype=np.float32) / np.float32(32)).astype(np.float32)
    inv_b = np.power(np.float32(500000.0), -np.arange(0, 16, 2, dtype=np.float32) / np.float32(16)).astype(np.float32)
    fa = np.zeros((128, 2), np.float32)
    for base in (0, 64):
        fa[base:base + 16, 0] = inv_a
        fa[base + 16:base + 32, 0] = inv_a
        fa[base:base + 16, 1] = -1.0
        fa[base + 16:base + 32, 1] = 1.0
    fb = np.zeros((128, 2), np.float32)
    for h in range(8):
        fb[16 * h:16 * h + 8, 0] = inv_b
        fb[16 * h + 8:16 * h + 16, 0] = inv_b
        fb[16 * h:16 * h + 8, 1] = -1.0
        fb[16 * h + 8:16 * h + 16, 1] = 1.0
    cf[:, 168:170] = fa
    cf[:, 170:172] = fb
    cf[:, 172] = np.float32(math.pi / 2)
    cf[:, 173] = 1e-6
    cf[:, 174] = 1e-5
    cf[:, 175] = NSLOT + np.arange(128, dtype=np.float32)
    pm = np.zeros((8, 8, 8), np.float32)
    own = np.zeros((8, 8, 8), np.float32)
    for cur in range(8):
        pm[cur, :, cur:] = -1e30
        own[cur, :, cur] = 1.0
    cpm = np.concatenate([pm.reshape(1, 512), own.reshape(1, 512)], axis=1)
    cpm = np.ascontiguousarray(np.broadcast_to(cpm, (128, 1024))).astype(np.float32)
    ind = (np.arange(T)[None, :] // 256 == np.arange(8)[:, None]).astype(np.float32).astype(bf)
    return cbf, cf, cpm, ind


def _prep_shared(inp):
    f = lambda a: np.ascontiguousarray(np.asarray(a, dtype=np.float32))
    w_in = f(inp["w_in"])[0]
    kr = w_in[:, 384:416]
    kr_sw = np.concatenate([kr[:, 16:32], kr[:, 0:16]], axis=1)
    w1a = np.concatenate([w_in[:, 0:384], kr, kr_sw], axis=1)

    def moba_cols(wm):
        wh = wm.reshape(D, 8, 64)
        c0 = wh[:, :, 0:16].reshape(D, 128)
        c0s = np.concatenate([wh[:, :, 8:16], wh[:, :, 0:8]], axis=2).reshape(D, 128)
        rest = [wh[:, :, 16 + 16 * j:32 + 16 * j].reshape(D, 128) for j in range(3)]
        return [c0, c0s] + rest

    w1b = np.concatenate(moba_cols(w_in[:, 416:928]) + moba_cols(w_in[:, 928:1440]) + [w_in[:, 1440:1952]], axis=1)
    wq = f(inp["w_q_b"])[0]
    wqh = wq.reshape(256, 8, 96)
    wqs = np.concatenate([wqh[:, :, 0:64], wqh[:, :, 80:96], wqh[:, :, 64:80]], axis=2).reshape(256, 768)
    wkv = f(inp["w_kv_b"])[0].reshape(128, 8, 128)
    wk = wkv[:, :, 0:64].reshape(128, 512)
    wv = wkv[:, :, 64:128].reshape(128, 512)
    cbf, cf, cpm, ind = _consts()
    sh = {
        "w1a": w1a, "w1b": w1b, "wq": wq, "wqs": wqs,
        "qg": f(inp["q_a_norm"])[0].reshape(2, 128).T, "wk": wk, "wv": wv,
        "kvg": f(inp["kv_a_norm"])[0].reshape(128, 1),
        "wo": f(inp["w_o"])[0], "ln1g": f(inp["ln1_g"])[0], "ln1b": f(inp["ln1_b"])[0],
        "wr": f(inp["w_router"])[0], "br": f(inp["b_router"])[0].reshape(1, NE),
        "wg": f(inp["w_gate"])[0], "wu": f(inp["w_up"])[0], "wd": f(inp["w_down"])[0],
        "bg": f(inp["b_gate"])[0].reshape(NE, 8, 128).transpose(2, 0, 1).reshape(128, NE * 8),
        "bu": f(inp["b_up"])[0].reshape(NE, 8, 128).transpose(2, 0, 1).reshape(128, NE * 8),
        "bd": f(inp["b_down"])[0], "ln2g": f(inp["ln2_g"])[0], "ln2b": f(inp["ln2_b"])[0],
        "cbf": cbf, "cf": cf, "cpm": cpm, "cind": ind,
    }
    return {k: np.ascontiguousarray(v) for k, v in sh.items()}


def make_in_maps(inp, n_cores=8):
    sh = _prep_shared(inp)
    x = np.asarray(inp["x"], dtype=np.float32)
    posn = np.asarray(inp["positions"]).astype(np.int32)
    maps = []
    for c in range(n_cores):
        m = dict(sh)
        m["xT"] = np.ascontiguousarray(x[c].T)
        m["xtok"] = np.ascontiguousarray(x[c])
        m["pos"] = np.ascontiguousarray(posn[c])
        maps.append(m)
    return maps


def kernel(**inputs):
    nc = build_program(debug=False)
    maps = make_in_maps(inputs, 8)
    res = run_bass_kernel_spmd(nc, maps, core_ids=list(range(8)))
    out = np.stack([np.asarray(r["y"], dtype=np.float32) for r in res.results], axis=0)
    return out.reshape(8, T, D)
```

```python
import math
from contextlib import ExitStack

import numpy as np
import ml_dtypes

import concourse.bass as bass
import concourse.mybir as mybir
from concourse.bass_utils import run_bass_kernel_spmd

F32 = mybir.dt.float32
BF16 = mybir.dt.bfloat16
I32 = mybir.dt.int32
U32 = mybir.dt.uint32
ALU = mybir.AluOpType
AF = mybir.ActivationFunctionType
AX = mybir.AxisListType

T = 2048
D = 1024
NE = 32
CAP = 384
NSLOT = NE * CAP
NBLK = CAP // 128
NPRE = 7
DN_ALPHA = 2.0 ** 0.25
NEG = -30000.0
SIGC = float(1.0 / (1.0 + math.exp(-1.702 * 7.0)))

COMPUTE = ("pe", "act", "dve", "pool")
SAME_ENGINE_SYNC = {"act": True, "dve": True, "pool": True, "pe": False}
DMA_POOL = {"sync": 24, "act": 8, "pool": 16}


class Op:
    __slots__ = ("eng", "fn", "dma", "deps", "idx", "has_dep", "tok", "pre")

    def __init__(self, eng, fn, dma, idx):
        self.eng = eng
        self.fn = fn
        self.dma = dma
        self.deps = set()
        self.idx = idx
        self.has_dep = False
        self.tok = None
        self.pre = None


class Sched:
    def __init__(self):
        self.ops = []
        self.last_writer = {}
        self.readers = {}

    def op(self, eng, fn, reads=(), writes=(), dma=False):
        o = Op(eng, fn, dma, len(self.ops))
        self.ops.append(o)
        reads = list(reads) + ["PHASE"]
        writes = list(writes) + [r for r in reads if isinstance(r, tuple) and r[0] == "ps" and r not in writes]
        for r in reads:
            w = self.last_writer.get(r)
            if w is not None:
                o.deps.add(w)
        for wkey in writes:
            w = self.last_writer.get(wkey)
            if w is not None:
                o.deps.add(w)
            rd = self.readers.get(wkey)
            if rd:
                for x in rd["c"].values():
                    o.deps.add(x)
                for x in rd["d"]:
                    o.deps.add(x)
            self.last_writer[wkey] = o
            self.readers[wkey] = {"c": {}, "d": []}
        for r in reads:
            rd = self.readers.setdefault(r, {"c": {}, "d": []})
            if dma:
                rd["d"].append(o)
            else:
                rd["c"][eng] = o
        o.deps.discard(o)
        return o

    def dma(self, eng, out, in_, reads=(), writes=(), **kw):
        return self.op(eng, lambda e: e.dma_start(out=out, in_=in_, **kw), reads, writes, dma=True)

    def emit(self, sems):
        ops = self.ops
        for o in ops:
            keep = set()
            for d in o.deps:
                if (not d.dma) and (not o.dma) and d.eng == o.eng and not SAME_ENGINE_SYNC[o.eng]:
                    continue
                keep.add(d)
            o.deps = keep
            for d in keep:
                d.has_dep = True
        cnt = {e: 0 for e in COMPUTE}
        dcnt = {e: 0 for e in DMA_POOL}
        for o in ops:
            if o.dma:
                i = dcnt[o.eng]
                dcnt[o.eng] += 1
                n = DMA_POOL[o.eng]
                sem = sems["d_%s_%d" % (o.eng, i % n)]
                o.tok = (sem, 16 * (i // n + 1))
                o.pre = (sem, 16 * (i // n)) if i >= n else None
            elif o.has_dep:
                cnt[o.eng] += 1
                o.tok = (sems["c_" + o.eng], cnt[o.eng])
        self.final_dma = []
        for e in DMA_POOL:
            n = DMA_POOL[e]
            tot = dcnt[e]
            for j in range(min(n, tot)):
                k = (tot - 1 - j) // n + 1
                self.final_dma.append((sems["d_%s_%d" % (e, j)], 16 * k))
        self.streams = {e: [] for e in ("pe", "act", "dve", "pool", "sync")}
        for o in ops:
            self.streams[o.eng].append(o)

    def run_stream(self, name, eng, extra_final=()):
        waited = {}

        def wait(sem, val):
            key = id(sem)
            if waited.get(key, 0) < val:
                eng.wait_ge(sem, val)
                waited[key] = val

        for o in self.streams[name]:
            for d in sorted(o.deps, key=lambda x: x.idx):
                wait(*d.tok)
            if o.pre is not None:
                wait(*o.pre)
            ins = o.fn(eng)
            if o.tok is not None:
                ins.then_inc(o.tok[0], 16 if o.dma else 1)
        for (sem, val) in extra_final:
            wait(sem, val)


def make_sems(nc, stack):
    sems = {}
    for e in COMPUTE:
        sems["c_" + e] = stack.enter_context(nc.semaphore("c_" + e))
    for e, n in DMA_POOL.items():
        for i in range(n):
            sems["d_%s_%d" % (e, i)] = stack.enter_context(nc.semaphore("d_%s_%d" % (e, i)))
    return sems


def run_block(nc, sched, sems):
    sched.emit(sems)
    finals = sched.final_dma
    with nc.Block() as block:
        @block.sync
        def _(eng):
            sched.run_stream("sync", eng, extra_final=finals)

        @block.scalar
        def _(eng):
            sched.run_stream("act", eng)

        @block.vector
        def _(eng):
            sched.run_stream("dve", eng)

        @block.gpsimd
        def _(eng):
            sched.run_stream("pool", eng)

        @block.tensor
        def _(eng):
            sched.run_stream("pe", eng)


DT_SIZE = {F32: 4, BF16: 2, I32: 4, U32: 4}


class Arena:
    def __init__(self, ar, size_f32):
        self.ar = ar
        self.size = size_f32
        self.off = 0
        self.peak = 0

    def reset(self, off=0):
        self.off = off

    def alloc(self, shape, dtype):
        n = 1
        for s in shape[1:]:
            n *= s
        n32 = (n * DT_SIZE[dtype] + 3) // 4
        n32 = (n32 + 1) // 2 * 2
        assert self.off + n32 <= min(self.size, getattr(self, "limit", self.size)), ("arena overflow", self.off, n32, self.size)
        v = self.ar[:, self.off:self.off + n32]
        self.off += n32
        self.peak = max(self.peak, self.off)
        if dtype != F32:
            v = v.bitcast(dtype)
        v = v[:, 0:n]
        if len(shape) == 3:
            v = v.rearrange("p (a b) -> p a b", a=shape[1])
        elif len(shape) == 4:
            v = v.rearrange("p (a b c) -> p a b c", a=shape[1], b=shape[2])
        return v[0:shape[0]]


class _Stop(Exception):
    pass


def build_program(debug=False, stop=99):
    nc = bass.Bass("TRN2", target_bir_lowering=False)
    S = Sched()

    def dram_in(name, shape, dt):
        return nc.dram_tensor(name, list(shape), dt, kind="ExternalInput").ap()

    xT = dram_in("xT", [D, T], F32)
    xtok = dram_in("xtok", [T, D], F32)
    pos = dram_in("pos", [T], I32)
    w1a = dram_in("w1a", [D, 448], F32)
    w1b = dram_in("w1b", [D, 1792], F32)
    wq = dram_in("wq", [256, 768], F32)
    wqs = dram_in("wqs", [256, 768], F32)
    qg = dram_in("qg", [128, 2], F32)
    wk = dram_in("wk", [128, 512], F32)
    wv = dram_in("wv", [128, 512], F32)
    kvg = dram_in("kvg", [128, 1], F32)
    wo = dram_in("wo", [D, D], F32)
    ln1g = dram_in("ln1g", [D], F32)
    ln1b = dram_in("ln1b", [D], F32)
    wr = dram_in("wr", [D, NE], F32)
    br = dram_in("br", [1, NE], F32)
    wg = dram_in("wg", [NE, D, D], F32)
    wu = dram_in("wu", [NE, D, D], F32)
    wd = dram_in("wd", [NE, D, D], F32)
    bg = dram_in("bg", [128, NE * 8], F32)
    bu = dram_in("bu", [128, NE * 8], F32)
    bd = dram_in("bd", [NE, D], F32)
    ln2g = dram_in("ln2g", [D], F32)
    ln2b = dram_in("ln2b", [D], F32)
    cbf = dram_in("cbf", [128, 384], BF16)
    cf = dram_in("cf", [128, 176], F32)
    cpm = dram_in("cpm", [128, 1024], F32)
    cind = dram_in("cind", [8, T], BF16)
    y = nc.dram_tensor("y", [T, D], F32, kind="ExternalOutput").ap()
    XG = nc.dram_tensor("XG", [NSLOT + 128, D], BF16, kind="Internal").ap()
    YG = nc.dram_tensor("YG", [NSLOT + 128, D], F32, kind="Internal").ap()
    WB = nc.dram_tensor("WB", [NPRE * 24 * 128, D], BF16, kind="Internal").ap()
    H1 = nc.dram_tensor("H1", [T, D], F32, kind="ExternalOutput" if debug else "Internal").ap()
    dbg = {}
    if debug:
        dbg["AT"] = nc.dram_tensor("dAT", [128, 8 * T], BF16, kind="ExternalOutput").ap()
        dbg["dest"] = nc.dram_tensor("ddest", [128, 64], I32, kind="ExternalOutput").ap()
        dbg["gate"] = nc.dram_tensor("dgate", [128, 64], F32, kind="ExternalOutput").ap()

    with ExitStack() as st:
        sems = make_sems(nc, st)
        ARN = 47616
        AR = st.enter_context(nc.sbuf_tensor("AR", [128, ARN], F32))
        CB = st.enter_context(nc.sbuf_tensor("CB", [128, 512], BF16))
        CF = st.enter_context(nc.sbuf_tensor("CF", [128, 176], F32))
        SM = st.enter_context(nc.sbuf_tensor("SM", [128, 2048], F32))
        PSUM = st.enter_context(nc.psum_tensor("PSUM", [128, 4096], F32))
        A = Arena(AR, ARN)

        def PS(b, rows=128, c0=0, c1=512):
            return PSUM[0:rows, b * 512 + c0:b * 512 + c1]

        IDENTb = CB[:, 0:128]
        TRIb = CB[:, 128:256]
        TRISb = CB[:, 256:384]
        ONESb = CB[:, 384:512]
        IDENTf = CF[:, 0:128]
        IOTA32 = CF[:, 128:160]
        HEADM = CF[:, 160:168]
        FA = CF[:, 168:170]
        FB = CF[:, 170:172]
        HALFPI = CF[:, 172:173]
        EPS6 = CF[:, 173:174]
        EPS5 = CF[:, 174:175]
        PCOL = CF[:, 175:176]
        smo = [0]

        def sm_alloc(n, dtype=F32):
            v = SM[:, smo[0]:smo[0] + n]
            smo[0] += n
            assert smo[0] <= 2048
            return v.bitcast(dtype) if dtype != F32 else v

        DEST = sm_alloc(64, I32)
        GATE = sm_alloc(64)
        ONESF = sm_alloc(128)
        BGS = sm_alloc(256)
        BG = sm_alloc(256)
        BU1 = sm_alloc(256)
        QG = sm_alloc(2)
        KVG = sm_alloc(2)
        BR = sm_alloc(32)

        def MM(out, lhsT, rhs, start, stop, R, W):
            S.op("pe", lambda e: e.matmul(out, lhsT, rhs, start=start, stop=stop), R, W)

        def TR(out, in_, ident, R, W):
            S.op("pe", lambda e: e.transpose(out, in_, ident), R, W)

        def ACTV(out, in_, func, R, W, **kw):
            S.op("act", lambda e: e.activation(out, in_, func, **kw), R, W)

        def CP(eng, out, in_, R, W):
            if eng == "act":
                S.op("act", lambda e: e.copy(out, in_), R, W)
            else:
                S.op(eng, lambda e: e.tensor_copy(out, in_), R, W)

        def TT(eng, out, a, b, op, R, W):
            S.op(eng, lambda e: e.tensor_tensor(out, a, b, op), R, W)

        def TS(eng, out, a, s1, op0, R, W, s2=None, op1=None):
            if op1 is None:
                S.op(eng, lambda e: e.tensor_scalar(out, a, s1, None, op0), R, W)
            else:
                S.op(eng, lambda e: e.tensor_scalar(out, a, s1, s2, op0, op1), R, W)

        def STT(out, in0, scalar, in1, op0, op1, R, W):
            S.op("dve", lambda e: e.scalar_tensor_tensor(out, in0, scalar, in1, op0, op1), R, W)

        def DMA(out, in_, R, W, eng="sync"):
            S.dma(eng, out, in_, R, W)

        def barrier():
            S.op("pool", lambda e: e.memset(SM[0:1, 2040:2042], 0.0), reads=[], writes=["PHASE"])

        def dump(name, ap2d, shape, dt, keys):
            if not debug:
                return
            t = nc.dram_tensor("dd_" + name, list(shape), dt, kind="ExternalOutput").ap()
            DMA(t, ap2d, keys, [])

        def ckpt(k):
            if k >= stop:
                raise _Stop()

        try:
            DMA(CB[:, 0:384], cbf, [], ["CB"])
            DMA(CF[:], cf, [], ["CF"])
            S.op("pool", lambda e: e.memset(ONESb, 1.0), [], ["CBo"])
            S.op("pool", lambda e: e.memset(ONESF, 1.0), [], ["ONESF"])
            DMA(BG, bg, [], ["BG"])
            DMA(BU1, bu, [], ["BU1"])
            DMA(QG, qg, [], ["QG"])
            DMA(KVG[:, 0:1], kvg, [], ["KVG"])
            DMA(BR[0:1, :], br, [], ["BR"])
            ZT = sm_alloc(512)
            S.op("pool", lambda e: e.memset(ZT, 0.0), [], ["ZT"])
            XGv = XG
            ZTb = ZT.bitcast(BF16)
            NZ = (NSLOT + 128) // 128
            zc = [0]

            def zfill(n):
                for _ in range(n):
                    if zc[0] >= NZ:
                        return
                    i = zc[0]
                    zc[0] += 1
                    DMA(XGv[i * 128:(i + 1) * 128, :], ZTb, ["ZT"], [("XGz", i)])

            TS("pool", BGS, BG, 1.702, ALU.mult, ["BG"], ["BGS"])
            TS("pool", BU1, BU1, 1.0, ALU.add, ["BU1"], ["BU1"])

            def build_tables(fcol, out_c, out_s, kc, ks, tmp):
                posi, ang, kk, rr = tmp
                DMA(posi, pos.partition_broadcast(128), [], ["t_posi"])
                CP("dve", ang, posi, ["t_posi"], ["t_ang"])
                TS("dve", ang, ang, fcol[:, 0:1], ALU.mult, ["t_ang", "CF"], ["t_ang"])
                TS("dve", kk, ang, 1.0 / (2.0 * math.pi), ALU.mult, ["t_ang"], ["t_k"], s2=12582912.0, op1=ALU.add)
                TS("dve", kk, kk, -12582912.0, ALU.add, ["t_k"], ["t_k"])
                C1 = 6.28125
                C2 = float(np.float32(2.0 * math.pi - 6.28125))
                STT(rr, kk, -C1, ang, ALU.mult, ALU.add, ["t_k", "t_ang"], ["t_r"])
                STT(rr, kk, -C2, rr, ALU.mult, ALU.add, ["t_k", "t_r"], ["t_r"])
                TS("dve", rr, rr, -3.1415925, ALU.max, ["t_r"], ["t_r"], s2=3.1415925, op1=ALU.min)
                ACTV(out_s, rr, AF.Sin, ["t_r", "CF"], [ks], scale=fcol[:, 1:2])
                STT(kk, rr, -1.0, rr, ALU.mult, ALU.max, ["t_r"], ["t_k"])
                ACTV(out_c, kk, AF.Sin, ["t_k", "CF"], [kc], scale=-1.0, bias=HALFPI)

            AT = A.alloc([128, 8, T], BF16)
            base_after_AT = A.off
            BGTOP = ARN - 4 * 512
            BGB = [AR[:, BGTOP + i * 512:BGTOP + (i + 1) * 512].bitcast(BF16) for i in range(4)]
            A.limit = BGTOP
            bg_seq = []

            def _bg_in(idx, src_ap):
                i = idx % 4
                return lambda: S.dma("pool", BGB[i], src_ap, [], [("BGB", i)])

            def _bg_out(idx):
                i = idx % 4
                return lambda: S.dma("pool", WB[idx * 128:(idx + 1) * 128, :], BGB[i], [("BGB", i)], [("WB", idx)])

            _ins, _outs = [], []
            for ei in range(NPRE):
                ee = NE - NPRE + ei
                for j in range(24):
                    idx = ei * 24 + j
                    if j < 16:
                        src = (wg if j % 2 == 0 else wu)[ee, (j // 2) * 128:(j // 2 + 1) * 128, :]
                    else:
                        src = wd[ee, (j - 16) * 128:(j - 15) * 128, :]
                    _ins.append(_bg_in(idx, src))
                    _outs.append(_bg_out(idx))
            nchunk = len(_ins)
            for k in range(nchunk + 2):
                if k < nchunk:
                    bg_seq.append(_ins[k])
                if k >= 2:
                    bg_seq.append(_outs[k - 2])
            bgc = [0]

            def bg(n):
                for _ in range(n):
                    if bgc[0] >= len(bg_seq):
                        return
                    bg_seq[bgc[0]]()
                    bgc[0] += 1


            TAc = A.alloc([128, T], F32)
            TAs = A.alloc([128, T], F32)
            off0 = A.off
            tmp_tab = [A.alloc([128, T], I32), A.alloc([128, T], F32), A.alloc([128, T], F32), A.alloc([128, T], F32)]
            build_tables(FA, TAc, TAs, "TAc", "TAs", tmp_tab)
            ckpt(0)
            barrier()
            A.reset(off0)
            QN = A.alloc([128, 2, T], BF16)
            KVN = A.alloc([128, T], BF16)
            KR = A.alloc([32, T], BF16)
            VALL = A.alloc([128, 16, 512], BF16)
            QAb = [A.alloc([128, T], BF16) for _ in range(2)]
            KAb = [A.alloc([128, T], BF16) for _ in range(2)]
            VAb = [A.alloc([128, 16, 128], BF16) for _ in range(2)]
            PT = [A.alloc([128, 512], BF16) for _ in range(4)]
            WQ = A.alloc([128, 2, 768], BF16)
            WQS = A.alloc([128, 2, 768], BF16)
            WK = A.alloc([128, 512], BF16)
            WV = A.alloc([128, 512], BF16)
            RC = [A.alloc([128, 512], F32) for _ in range(2)]
            T1 = A.alloc([128, 512], F32)
            T2 = A.alloc([128, 512], F32)
            ph12_common_end = A.off
            W1A = A.alloc([128, 8, 448], BF16)
            XG16 = [A.alloc([128, 8, 512], BF16) for _ in range(2)]
            XST = [A.alloc([128, 512], F32) for _ in range(4)]
            WST = [A.alloc([128, 768], F32) for _ in range(2)]
            SQ = [A.alloc([128, 512], BF16) for _ in range(2)]
            RQ = A.alloc([128, 512], F32)

            cast_rr = [0]
            CAST_ENG = ["act", "dve", "pool"]

            def cast(out, in_, R, W, eng=None):
                if eng is None:
                    eng = CAST_ENG[cast_rr[0] % 3]
                    cast_rr[0] += 1
                CP(eng, out, in_, R, W)

            for c in range(2):
                DMA(WST[0][:, 0:768], wq[c * 128:(c + 1) * 128, :], [], ["WST0"])
                TS("dve", WQ[:, c, :], WST[0][:, 0:768], QG[:, c:c + 1], ALU.mult, ["WST0", "QG"], ["WQ"])
                DMA(WST[1][:, 0:768], wqs[c * 128:(c + 1) * 128, :], [], ["WST1"])
                TS("pool", WQS[:, c, :], WST[1][:, 0:768], QG[:, c:c + 1], ALU.mult, ["WST1", "QG"], ["WQS"])
            DMA(WST[0][:, 0:512], wk, [], ["WST0"])
            TS("dve", WK, WST[0][:, 0:512], KVG[:, 0:1], ALU.mult, ["WST0", "KVG"], ["WK"])
            DMA(WST[1][:, 0:512], wv, [], ["WST1"])
            TS("pool", WV, WST[1][:, 0:512], KVG[:, 0:1], ALU.mult, ["WST1", "KVG"], ["WV"])
            for c in range(8):
                DMA(WST[c % 2][:, 0:448], w1a[c * 128:(c + 1) * 128, :], [], ["WST%d" % (c % 2)])
                cast(W1A[:, c, :], WST[c % 2][:, 0:448], ["WST%d" % (c % 2)], [("W1A", c)])

            psrr = [0]

            def nbank():
                b = psrr[0] % 8
                psrr[0] += 1
                return b

            xcnt = [0]

            def load_xgroup(g):
                xb = XG16[g % 2]
                for c in range(8):
                    i = xcnt[0] % 4
                    xcnt[0] += 1
                    DMA(XST[i], xT[c * 128:(c + 1) * 128, g * 512:(g + 1) * 512], [], [("XST", i)])
                    cast(xb[:, c, :], XST[i], [("XST", i)], [("XG16", g % 2, c)])
                    zfill(2)
                return xb

            def proj_chunk(xb, g, W, wkeys, c0, c1, bank):
                M = c1 - c0
                for c in range(8):
                    MM(PS(bank, M), W[:, c, c0:c1], xb[:, c, :], c == 0, c == 7,
                       [wkeys(c), ("XG16", g % 2, c)], [("ps", bank)])

            for g in range(4):
                gs = slice(g * 512, (g + 1) * 512)
                xb = load_xgroup(g)
                wkey = lambda c: ("W1A", c)
                for j in range(2):
                    b = nbank()
                    proj_chunk(xb, g, W1A, wkey, j * 128, (j + 1) * 128, b)
                    CP("act", QN[:, j, gs], PS(b), [("ps", b)], [("QN", j, g)])
                    ACTV(SQ[j], PS(b), AF.Square, [("ps", b)], [("SQ", j)])
                b = nbank()
                MM(PS(b), ONESb, SQ[0], True, False, ["CBo", ("SQ", 0)], [("ps", b)])
                MM(PS(b), ONESb, SQ[1], False, True, ["CBo", ("SQ", 1)], [("ps", b)])
                ACTV(RQ, PS(b), AF.Sqrt, [("ps", b), "CF"], ["RQ"], scale=1.0 / 256.0, bias=EPS6)
                S.op("dve", lambda e: e.reciprocal(RQ, RQ), ["RQ"], ["RQ"])
                for j in range(2):
                    TT("dve", QN[:, j, gs], QN[:, j, gs], RQ, ALU.mult, [("QN", j, g), "RQ"], [("QN", j, g)])
                b = nbank()
                proj_chunk(xb, g, W1A, wkey, 256, 384, b)
                CP("act", KVN[:, gs], PS(b), [("ps", b)], [("KVN", g)])
                ACTV(SQ[0], PS(b), AF.Square, [("ps", b)], [("SQ", 0)])
                b = nbank()
                MM(PS(b), ONESb, SQ[0], True, True, ["CBo", ("SQ", 0)], [("ps", b)])
                ACTV(RQ, PS(b), AF.Sqrt, [("ps", b), "CF"], ["RQ"], scale=1.0 / 128.0, bias=EPS6)
                S.op("dve", lambda e: e.reciprocal(RQ, RQ), ["RQ"], ["RQ"])
                TT("dve", KVN[:, gs], KVN[:, gs], RQ, ALU.mult, [("KVN", g), "RQ"], [("KVN", g)])
                ba = nbank()
                proj_chunk(xb, g, W1A, wkey, 384, 416, ba)
                bb = nbank()
                proj_chunk(xb, g, W1A, wkey, 416, 448, bb)
                TT("dve", T1[0:32, :], PS(ba, 32), TAc[0:32, gs], ALU.mult, [("ps", ba), "TAc"], ["T1"])
                TT("dve", T2[0:32, :], PS(bb, 32), TAs[0:32, gs], ALU.mult, [("ps", bb), "TAs"], ["T2"])
                TT("dve", KR[0:32, gs], T1[0:32, :], T2[0:32, :], ALU.add, ["T1", "T2"], [("KR", g)])
                for tt in range(4):
                    ti = g * 4 + tt
                    b = nbank()
                    MM(PS(b), KVN[:, ti * 128:(ti + 1) * 128], WV, True, True, [("KVN", g), "WV"], [("ps", b)])
                    CP("act", VALL[:, ti, :], PS(b), [("ps", b)], [("VALL", ti)])
            if stop == 1:
                dump("QN", QN.rearrange("p a t -> p (a t)"), [128, 2 * T], BF16, [("QN", j, g) for j in range(2) for g in range(4)])
                dump("KVN", KVN, [128, T], BF16, [("KVN", g) for g in range(4)])
                dump("KR", KR, [32, T], BF16, [("KR", g) for g in range(4)])
                dump("TAc", TAc, [128, T], F32, ["TAc"])
                dump("TAs", TAs, [128, T], F32, ["TAs"])
                dump("VALL", VALL.rearrange("p a t -> p (a t)"), [128, 16 * 512], BF16, [("VALL", ti) for ti in range(16)])
            ckpt(1)

            def attention(b, Kq, scale, parity, chunk, after_g, sbanks=(0, 1, 2)):
                Kq = 128
                NSB = len(sbanks)
                QA, KA, VA = QAb[b], KAb[b], VAb[b]
                steps = [(g, kt) for g in range(4) for kt in range(4 * g + 4)]
                n = len(steps)
                qa_keys = [("QA", b, "lo"), ("QA", b, "hi")]
                ka_keys = [("KA", b, "lo"), ("KA", b, "hi")]

                def qk(i):
                    g, kt = steps[i]
                    j = kt - 4 * g
                    c0 = 128 * max(0, j)
                    sb = sbanks[i % NSB]
                    MM(PS(sb, 128, c0, 512), KA[0:Kq, kt * 128:(kt + 1) * 128], QA[0:Kq, g * 512 + c0:(g + 1) * 512],
                       True, j < 0, qa_keys + ka_keys, [("ps", sb)])
                    if j >= 0:
                        MM(PS(sb, 128, c0, c0 + 128), IDENTb, TRIb, False, True, ["CB"], [("ps", sb)])

                def ex(i):
                    g, kt = steps[i]
                    c0 = 128 * max(0, kt - 4 * g)
                    sb = sbanks[i % NSB]
                    ACTV(PT[i % 4][:, c0:512], PS(sb, 128, c0, 512), AF.Exp, [("ps", sb)], [("PT", i % 4)], scale=scale)

                def pv(i):
                    g, kt = steps[i]
                    c0 = 128 * max(0, kt - 4 * g)
                    ob = 3 + (g % 2)
                    MM(PS(ob, 128, c0, 512), VA[:, kt, :], PT[i % 4][:, c0:512], kt == 0, kt == 4 * g + 3,
                       [("VA", b), ("PT", i % 4)], [("ps", ob)])

                def norm(g):
                    ob = 3 + (g % 2)
                    gs = slice(g * 512, (g + 1) * 512)
                    rc = RC[g % 2]
                    if parity == 0:
                        S.op("dve", lambda e: e.reciprocal(rc[0:64, :], PS(ob)[64:128, :]), [("ps", ob)], [("RC", g % 2)])
                        TT("dve", AT[0:64, chunk, gs], PS(ob)[0:64, :], rc[0:64, :], ALU.mult,
                           [("ps", ob), ("RC", g % 2)], [("AT", chunk, parity, g)])
                    else:
                        S.op("dve", lambda e: e.reciprocal(rc[64:128, :], PS(ob)[0:64, :]), [("ps", ob)], [("RC", g % 2)])
                        TT("dve", AT[64:128, chunk, gs], PS(ob)[64:128, :], rc[64:128, :], ALU.mult,
                           [("ps", ob), ("RC", g % 2)], [("AT", chunk, parity, g)])

                for i0 in range(NSB - 1):
                    qk(i0)
                for i in range(n):
                    ex(i)
                    pv(i)
                    if i + NSB - 1 < n:
                        qk(i + NSB - 1)
                    g, kt = steps[i]
                    if kt == 4 * g + 3:
                        norm(g)
                        after_g(g)
                        zfill(4)
                        bg(6)

            def mla_prep(h, g):
                b = h % 2
                gs = slice(g * 512, (g + 1) * 512)
                hs = slice(h * 96, (h + 1) * 96)
                MM(PS(5, 96), WQ[:, 0, hs], QN[:, 0, gs], True, False, ["WQ", ("QN", 0, g)], [("ps", 5)])
                MM(PS(5, 96), WQ[:, 1, hs], QN[:, 1, gs], False, True, ["WQ", ("QN", 1, g)], [("ps", 5)])
                MM(PS(6, 96), WQS[:, 0, hs], QN[:, 0, gs], True, False, ["WQS", ("QN", 0, g)], [("ps", 6)])
                MM(PS(6, 96), WQS[:, 1, hs], QN[:, 1, gs], False, True, ["WQS", ("QN", 1, g)], [("ps", 6)])
                MM(PS(7, 64), WK[:, h * 64:(h + 1) * 64], KVN[:, gs], True, True, ["WK", ("KVN", g)], [("ps", 7)])
                CP("act", QAb[b][0:64, gs], PS(5, 64), [("ps", 5)], [("QA", b, "lo")])
                TT("dve", T1[64:96, :], PS(5)[64:96, :], TAc[64:96, gs], ALU.mult, [("ps", 5), "TAc"], ["T1"])
                TT("dve", T2[64:96, :], PS(6)[64:96, :], TAs[64:96, gs], ALU.mult, [("ps", 6), "TAs"], ["T2"])
                TT("dve", QAb[b][64:96, gs], T1[64:96, :], T2[64:96, :], ALU.add, ["T1", "T2"], [("QA", b, "hi")])
                CP("act", KAb[b][0:64, gs], PS(7, 64), [("ps", 7)], [("KA", b, "lo")])
                if g == 0:
                    vsl = slice(0, 64) if b == 0 else slice(64, 128)
                    CP("pool", VAb[b][:, :, vsl], VALL[:, :, h * 64:(h + 1) * 64],
                       [("VALL", ti) for ti in range(16)], [("VA", b)])

            for b in range(2):
                osl = slice(64, 128) if b == 0 else slice(0, 64)
                S.op("pool", lambda e, ap=VAb[b][:, :, osl]: e.memset(ap, 1.0), [], [("VA", b)])
                S.op("pool", lambda e, ap=QAb[b][96:128, :]: e.memset(ap, 0.0), [], [("QA", b, "hi")])
                S.op("pool", lambda e, ap=KAb[b][96:128, :]: e.memset(ap, 0.0), [], [("KA", b, "hi")])
                DMA(KAb[b][64:96, :], KR[0:32, :], [("KR", g) for g in range(4)], [("KA", b, "hi")])
            for g in range(4):
                mla_prep(0, g)
            sc_mla = 1.0 / math.sqrt(96.0)
            for h in range(8):
                def after(g, h=h):
                    if h + 1 < 8:
                        mla_prep(h + 1, g)
                attention(h % 2, 96, sc_mla, h % 2, h // 2, after)
            ckpt(2)

            barrier()
            A.reset(base_after_AT)
            TBc = A.alloc([128, T], F32)
            TBs = A.alloc([128, T], F32)
            off1 = A.off
            tmp_tab = [A.alloc([128, T], I32), A.alloc([128, T], F32), A.alloc([128, T], F32), A.alloc([128, T], F32)]
            build_tables(FB, TBc, TBs, "TBc", "TBs", tmp_tab)
            barrier()
            A.reset(off1)
            QM = A.alloc([128, 4, T], BF16)
            KM = A.alloc([128, 4, T], BF16)
            VALLm = A.alloc([128, 16, 512], BF16)
            T1 = A.alloc([128, 512], F32)
            T2 = A.alloc([128, 512], F32)
            MASKT = A.alloc([64, T], BF16)
            KMF = A.alloc([128, 32], F32)
            KMB = A.alloc([128, 32], BF16)
            KMBLK = A.alloc([128, 4, 64], BF16)
            GM = [A.alloc([128, 64], F32) for _ in range(2)]
            MX = [A.alloc([128, 64], F32) for _ in range(2)]
            THR = [A.alloc([128, 8], F32) for _ in range(2)]
            SEL = [A.alloc([128, 64], F32) for _ in range(2)]
            MB = [A.alloc([128, 64], BF16) for _ in range(2)]
            CPM = A.alloc([128, 1024], F32)
            off2 = A.off
            W1B = A.alloc([128, 8, 1792], BF16)
            XG16 = [A.alloc([128, 8, 512], BF16) for _ in range(2)]
            XST = [A.alloc([128, 512], F32) for _ in range(4)]
            WSTb = [A.alloc([128, 1792], F32) for _ in range(2)]

            DMA(CPM, cpm, [], ["CPM"])
            for c in range(8):
                DMA(WSTb[c % 2], w1b[c * 128:(c + 1) * 128, :], [], ["WSTb%d" % (c % 2)])
                cast(W1B[:, c, :], WSTb[c % 2], ["WSTb%d" % (c % 2)], [("W1B", c)])
            for g in range(4):
                gs = slice(g * 512, (g + 1) * 512)
                xb = load_xgroup(g)
                wkey = lambda c: ("W1B", c)
                for (dst, nm, off) in ((QM, "QM", 0), (KM, "KM", 640)):
                    ba = nbank()
                    proj_chunk(xb, g, W1B, wkey, off, off + 128, ba)
                    bb = nbank()
                    proj_chunk(xb, g, W1B, wkey, off + 128, off + 256, bb)
                    TT("dve", T1, PS(ba), TBc[:, gs], ALU.mult, [("ps", ba), "TBc"], ["T1"])
                    TT("dve", T2, PS(bb), TBs[:, gs], ALU.mult, [("ps", bb), "TBs"], ["T2"])
                    TT("pool", dst[:, 0, gs], T1, T2, ALU.add, ["T1", "T2"], [(nm, g)])
                    for j in range(1, 4):
                        b = nbank()
                        proj_chunk(xb, g, W1B, wkey, off + 128 + 128 * j, off + 256 + 128 * j, b)
                        CP("act", dst[:, j, gs], PS(b), [("ps", b)], [(nm, g)])
                for tt in range(4):
                    ti = g * 4 + tt
                    b = nbank()
                    for c in range(8):
                        MM(PS(b), xb[:, c, tt * 128:(tt + 1) * 128], W1B[:, c, 1280:1792], c == 0, c == 7,
                           [("W1B", c), ("XG16", g % 2, c)], [("ps", b)])
                    CP("act", VALLm[:, ti, :], PS(b), [("ps", b)], [("VALLm", ti)])

            kmk = [("KM", g) for g in range(4)]
            S.op("dve", lambda e: e.tensor_reduce(KMF.rearrange("p (a b) -> p a b", a=4),
                                                  KM.rearrange("p a (n k) -> p a n k", n=8), AX.X, ALU.add),
                 kmk, ["KMF"])
            TS("dve", KMB, KMF, 1.0 / 256.0, ALU.mult, ["KMF"], ["KMB"])
            KMBv = KMB.rearrange("p (a n) -> p a n", a=4)
            for hh in range(8):
                TS("dve", KMBLK[:, :, hh * 8:(hh + 1) * 8], KMBv, HEADM[:, hh:hh + 1], ALU.mult, ["KMB", "CF"], ["KMBLK"])
            ckpt(3)

            barrier()
            A.reset(off2)
            QAb = [A.alloc([128, T], BF16) for _ in range(2)]
            KAb = [A.alloc([128, T], BF16) for _ in range(2)]
            VAb = [A.alloc([128, 16, 128], BF16) for _ in range(2)]
            PT = [A.alloc([128, 512], BF16) for _ in range(4)]
            RC = [A.alloc([128, 512], F32) for _ in range(2)]
            GMt = [A.alloc([128, 64], F32) for _ in range(16)]
            MXt = [A.alloc([128, 64], F32) for _ in range(16)]
            THRt = [A.alloc([128, 8], F32) for _ in range(16)]
            SELt = [A.alloc([128, 64], F32) for _ in range(16)]
            MBt = [A.alloc([128, 64], BF16) for _ in range(16)]

            gb = [nbank(), nbank()]
            for ti in range(16):
                bk = gb[ti // 8]
                c0 = (ti % 8) * 64
                for j in range(4):
                    MM(PS(bk, 128, c0, c0 + 64), QM[:, j, ti * 128:(ti + 1) * 128], KMBLK[:, j, :], j == 0, j == 3,
                       [("QM", ti // 4), "KMBLK"], [("ps", bk)])
            for ti in range(16):
                cur = ti // 2
                bk = gb[ti // 8]
                c0 = (ti % 8) * 64
                TT("dve", GMt[ti], PS(bk, 128, c0, c0 + 64), CPM[:, cur * 64:(cur + 1) * 64], ALU.add, [("ps", bk), "CPM"], [("GM", ti)])
            for ti in range(16):
                for hh in range(8):
                    S.op("dve", lambda e, o=MXt[ti][:, hh * 8:(hh + 1) * 8], i=GMt[ti][:, hh * 8:(hh + 1) * 8]: e.max(o, i),
                         [("GM", ti)], [("MX", ti, hh)])
            for ti in range(16):
                MXv = MXt[ti].rearrange("p (h n) -> p h n", h=8)
                TS("dve", THRt[ti], MXv[:, :, 2], -1e29, ALU.max, [("MX", ti, hh) for hh in range(8)], [("THR", ti)])
            for ti in range(16):
                for hh in range(8):
                    TS("dve", SELt[ti][:, hh * 8:(hh + 1) * 8], GMt[ti][:, hh * 8:(hh + 1) * 8], THRt[ti][:, hh:hh + 1], ALU.is_ge,
                       [("GM", ti), ("THR", ti)], [("SEL", ti, hh)])
            for ti in range(16):
                cur = ti // 2
                TT("dve", SELt[ti], SELt[ti], CPM[:, 512 + cur * 64:512 + (cur + 1) * 64], ALU.add,
                   [("SEL", ti, hh) for hh in range(8)] + ["CPM"], [("SEL", ti)])
            for ti in range(16):
                TS("dve", MBt[ti], SELt[ti], -1.0, ALU.add, [("SEL", ti)], [("MB", ti)], s2=-NEG, op1=ALU.mult)
            tb = [nbank(), nbank()]
            for ti in range(16):
                bk = tb[ti // 8]
                c0 = (ti % 8) * 64
                pst = PSUM[0:64, bk * 512 + c0:bk * 512 + c0 + 64].bitcast(BF16)
                TR(pst, MBt[ti], IDENTb, [("MB", ti), "CB"], [("ps", bk)])
            for hb in range(2):
                bk = tb[hb]
                pall = PSUM[0:64, bk * 512:(bk + 1) * 512].bitcast(BF16)
                CP("dve", MASKT[:, hb * 1024:(hb + 1) * 1024], pall, [("ps", bk)], [("MASKT", hb * 8 + q) for q in range(8)])

            def moba_prep(h):
                b = h % 2
                for j in range(4):
                    DMA(QAb[b][16 * j:16 * j + 16, :], QM[16 * h:16 * h + 16, j, :], [("QM", g) for g in range(4)], [("QA", b, "lo")])
                    DMA(KAb[b][16 * j:16 * j + 16, :], KM[16 * h:16 * h + 16, j, :], kmk, [("KA", b, "lo")])
                DMA(QAb[b][64:72, :], MASKT[8 * h:8 * h + 8, :], [("MASKT", ti) for ti in range(16)], [("QA", b, "hi")])
                vsl = slice(0, 64) if b == 0 else slice(64, 128)
                CP("pool", VAb[b][:, :, vsl], VALLm[:, :, h * 64:(h + 1) * 64], [("VALLm", ti) for ti in range(16)], [("VA", b)])

            for b in range(2):
                osl = slice(64, 128) if b == 0 else slice(0, 64)
                S.op("pool", lambda e, ap=VAb[b][:, :, osl]: e.memset(ap, 1.0), [], [("VA", b)])
                S.op("pool", lambda e, ap=QAb[b][64:128, :]: e.memset(ap, 0.0), [], [("QA", b, "hi")])
                S.op("pool", lambda e, ap=KAb[b][64:128, :]: e.memset(ap, 0.0), [], [("KA", b, "hi")])
                DMA(KAb[b][64:72, :], cind, [], [("KA", b, "hi")])
            moba_prep(0)
            for h in range(8):
                def after(g, h=h):
                    if g == 0 and h + 1 < 8:
                        moba_prep(h + 1)
                attention(h % 2, 72, 0.125, h % 2, 4 + h // 2, after, sbanks=(0, 1, 2, 5, 6, 7))
            zfill(1000)
            bg(100000)
            ckpt(4)

            if debug:
                DMA(dbg["AT"], AT.rearrange("p a t -> p (a t)"),
                    [("AT", c, p, g) for c in range(8) for p in range(2) for g in range(4)], [])
            barrier()
            A.limit = ARN
            A.reset(base_after_AT)
            WO = A.alloc([128, 8, D], BF16)
            WOST = [A.alloc([128, D], F32) for _ in range(2)]
            G1 = A.alloc([128, D], F32)
            B1 = A.alloc([128, D], F32)
            WRf = A.alloc([128, 8, NE], F32)
            XB = [A.alloc([128, D], F32) for _ in range(8)]
            XBb = [A.alloc([128, D], BF16) for _ in range(8)]
            H1T = [A.alloc([128, 8, 128], F32) for _ in range(4)]
            MASKS = A.alloc([128, 16, NE], BF16)
            ST5 = [A.alloc([128, 12], F32) for _ in range(8)]
            MV5 = [A.alloc([128, 2], F32) for _ in range(8)]
            RS5 = [A.alloc([128, 4], F32) for _ in range(8)]
            LGg = [A.alloc([128, 4, NE], F32) for _ in range(2)]
            POSg = [A.alloc([128, 4, NE], F32) for _ in range(2)]
            MX8 = [A.alloc([128, 8], F32) for _ in range(8)]
            IX8 = [A.alloc([128, 8], U32) for _ in range(8)]
            IXF = [A.alloc([128, 4], F32) for _ in range(8)]
            NMX = [A.alloc([128, 2], F32) for _ in range(8)]
            EXP4 = [A.alloc([128, 4], F32) for _ in range(8)]
            SUM4 = [A.alloc([128, 2], F32) for _ in range(8)]
            OH = [[A.alloc([128, NE], F32) for _ in range(4)] for _ in range(8)]
            PK = [A.alloc([128, 4], F32) for _ in range(8)]
            PK2 = [A.alloc([128, 4], F32) for _ in range(8)]
            DF = [A.alloc([128, 4], F32) for _ in range(8)]
            OV = [A.alloc([128, 4], F32) for _ in range(8)]

            for c in range(8):
                DMA(WOST[c % 2], wo[c * 128:(c + 1) * 128, :], [], [("WOST", c % 2)])
                cast(WO[:, c, :], WOST[c % 2], [("WOST", c % 2)], [("WO", c)])
            DMA(G1, ln1g.partition_broadcast(128), [], ["G1"])
            DMA(B1, ln1b.partition_broadcast(128), [], ["B1"])
            DMA(WRf, wr.rearrange("(c p) e -> p c e", p=128), [], ["WRf"])

            def layer_norm(zt, zkey, out, okey, st, mv, rs, Gb, Bb, p, eps_ap, gb_eng):
                for hf in range(2):
                    S.op("dve", lambda e, hf=hf: e.bn_stats(st[:, hf * 6:(hf + 1) * 6], zt[:, hf * 512:(hf + 1) * 512]),
                         [zkey], [("ST", p)])
                    yield
                S.op("dve", lambda e: e.bn_aggr(mv, st), [("ST", p)], [("MV", p)])
                yield
                ACTV(rs[:, 0:1], mv[:, 1:2], AF.Sqrt, [("MV", p), "CF"], [("RS", p)], bias=eps_ap, scale=1.0)
                yield
                S.op("dve", lambda e: e.reciprocal(rs[:, 1:2], rs[:, 0:1]), [("RS", p)], [("RS", p)])
                yield
                TS("dve", rs[:, 2:3], mv[:, 0:1], rs[:, 1:2], ALU.mult, [("MV", p), ("RS", p)], [("RS", p)], s2=-1.0, op1=ALU.mult)
                yield
                ACTV(out, zt, AF.Identity, [zkey, ("RS", p)], [okey], bias=rs[:, 2:3], scale=rs[:, 1:2])
                yield
                TT(gb_eng, out, out, Gb, ALU.mult, [okey, "G1", "G2"], [okey])
                yield
                TT(gb_eng, out, out, Bb, ALU.add, [okey, "B1", "B2"], [okey])
                yield

            atkeys = [("AT", c, p, g) for c in range(8) for p in range(2) for g in range(4)]
            def pairbank(T_):
                return 2 * (T_ % 3)

            def stageA(g):
                b = g % 2
                tiles = [(t, g * 4 + t, b * 4 + t) for t in range(4)]
                for t, T_, sl in tiles:
                    DMA(XB[sl], xtok[T_ * 128:(T_ + 1) * 128, :], [], [("XB", sl)])
                for sub in (tiles[0:3], tiles[3:4]):
                    for t, T_, sl in sub:
                        pb = pairbank(T_)
                        ts_ = slice(T_ * 128, (T_ + 1) * 128)
                        for hf in range(2):
                            for c in range(8):
                                MM(PS(pb + hf), AT[:, c, ts_], WO[:, c, hf * 512:(hf + 1) * 512], c == 0, c == 7,
                                   [("WO", c)], [("ps", pb + hf)])
                    for t, T_, sl in sub:
                        pb = pairbank(T_)
                        for hf in range(2):
                            hs = slice(hf * 512, (hf + 1) * 512)
                            STT(XB[sl][:, hs], XB[sl][:, hs], DN_ALPHA, PS(pb + hf), ALU.mult, ALU.add,
                                [("XB", sl), ("ps", pb + hf)], [("XB", sl)])
                for t, T_, sl in tiles:
                    for hf in range(2):
                        S.op("dve", lambda e, st=ST5[sl], z=XB[sl], hf=hf: e.bn_stats(st[:, hf * 6:(hf + 1) * 6], z[:, hf * 512:(hf + 1) * 512]),
                             [("XB", sl)], [("ST", sl)])
                for t, T_, sl in tiles:
                    S.op("dve", lambda e, mv=MV5[sl], st=ST5[sl]: e.bn_aggr(mv, st), [("ST", sl)], [("MV", sl)])
                for t, T_, sl in tiles:
                    ACTV(RS5[sl][:, 0:1], MV5[sl][:, 1:2], AF.Sqrt, [("MV", sl), "CF"], [("RS", sl)], bias=EPS5, scale=1.0)
                for t, T_, sl in tiles:
                    S.op("dve", lambda e, rs=RS5[sl]: e.reciprocal(rs[:, 1:2], rs[:, 0:1]), [("RS", sl)], [("RS", sl)])
                for t, T_, sl in tiles:
                    TS("dve", RS5[sl][:, 2:3], MV5[sl][:, 0:1], RS5[sl][:, 1:2], ALU.mult, [("MV", sl), ("RS", sl)], [("RS", sl)],
                       s2=-1.0, op1=ALU.mult)

            def stageA1b(g):
                b = g % 2
                tiles = [(t, g * 4 + t, b * 4 + t) for t in range(4)]
                for t, T_, sl in tiles:
                    ACTV(XB[sl], XB[sl], AF.Identity, [("XB", sl), ("RS", sl)], [("XB", sl)], bias=RS5[sl][:, 2:3], scale=RS5[sl][:, 1:2])
                for t, T_, sl in tiles:
                    TT("dve", XB[sl], XB[sl], G1, ALU.mult, [("XB", sl), "G1"], [("XB", sl)])
                for t, T_, sl in tiles:
                    TT("dve", XB[sl], XB[sl], B1, ALU.add, [("XB", sl), "B1"], [("XB", sl)])

            def stageA2(g):
                b = g % 2
                tiles = [(t, g * 4 + t, b * 4 + t) for t in range(4)]
                for t, T_, sl in tiles:
                    DMA(H1[T_ * 128:(T_ + 1) * 128, :], XB[sl], [("XB", sl)], [("H1", T_)])
                for t, T_, sl in tiles:
                    CP("act", XBb[sl], XB[sl], [("XB", sl)], [("XBb", sl)])
                for t, T_, sl in tiles:
                    pb = pairbank(T_)
                    for hf in range(2):
                        for cc in range(4):
                            c = hf * 4 + cc
                            TR(PS(pb + hf, 128, cc * 128, (cc + 1) * 128), XB[sl][:, c * 128:(c + 1) * 128], IDENTf,
                               [("XB", sl), "CF"], [("ps", pb + hf)])
                        CP("act", H1T[t][:, hf * 4:(hf + 1) * 4, :], PS(pb + hf).rearrange("p (a b) -> p a b", a=4),
                           [("ps", pb + hf)], [("H1T", t, hf)])
                for t, T_, sl in tiles:
                    for c in range(8):
                        MM(PS(6, 128, t * NE, (t + 1) * NE), H1T[t][:, c, :], WRf[:, c, :], c == 0, False,
                           [("H1T", t, 0), ("H1T", t, 1), "WRf"], [("ps", 6)])
                    MM(PS(6, 128, t * NE, (t + 1) * NE), ONESF[0:1, :], BR[0:1, :], False, True, ["ONESF", "BR"], [("ps", 6)])
                CP("dve", LGg[b].rearrange("p a e -> p (a e)"), PS(6, 128, 0, 4 * NE), [("ps", 6)], [("LGg", b)])

            def stageB(g):
                b = g % 2
                tiles = [(t, g * 4 + t, b * 4 + t) for t in range(4)]
                for t, T_, sl in tiles:
                    S.op("dve", lambda e, o=MX8[sl], i=LGg[b][:, t, :]: e.max(o, i), [("LGg", b)], [("MX8", sl)])
                for t, T_, sl in tiles:
                    S.op("dve", lambda e, o=IX8[sl], m=MX8[sl], i=LGg[b][:, t, :]: e.max_index(o, m, i),
                         [("LGg", b), ("MX8", sl)], [("IX8", sl)])
                for t, T_, sl in tiles:
                    TS("dve", NMX[sl][:, 0:1], MX8[sl][:, 0:1], -1.0, ALU.mult, [("MX8", sl)], [("NMX", sl)])
                for t, T_, sl in tiles:
                    ACTV(EXP4[sl], MX8[sl][:, 0:4], AF.Exp, [("MX8", sl), ("NMX", sl)], [("EXP4", sl)], bias=NMX[sl][:, 0:1], scale=1.0)
                for t, T_, sl in tiles:
                    S.op("dve", lambda e, o=SUM4[sl], i=EXP4[sl]: e.tensor_reduce(o[:, 0:1], i, AX.X, ALU.add), [("EXP4", sl)], [("SUM4", sl)])
                for t, T_, sl in tiles:
                    S.op("dve", lambda e, o=SUM4[sl]: e.reciprocal(o[:, 1:2], o[:, 0:1]), [("SUM4", sl)], [("SUM4", sl)])
                for t, T_, sl in tiles:
                    TS("dve", GATE[:, T_ * 4:(T_ + 1) * 4], EXP4[sl], SUM4[sl][:, 1:2], ALU.mult, [("EXP4", sl), ("SUM4", sl)], [("GATE", T_)])
                for t, T_, sl in tiles:
                    TS("dve", MASKS[:, T_, :], LGg[b][:, t, :], MX8[sl][:, 3:4], ALU.is_ge, [("LGg", b), ("MX8", sl)], [("MASKS", T_)])
                for t, T_, sl in tiles:
                    MM(PS(7, 128, t * NE, (t + 1) * NE), TRISb, MASKS[:, T_, :], True, T_ == 0, ["CB", ("MASKS", T_)], [("ps", 7)])
                    for tj in range(T_):
                        MM(PS(7, 128, t * NE, (t + 1) * NE), ONESb, MASKS[:, tj, :], False, tj == T_ - 1, ["CBo", ("MASKS", tj)], [("ps", 7)])
                CP("dve", POSg[b].rearrange("p a e -> p (a e)"), PS(7, 128, 0, 4 * NE), [("ps", 7)], [("POSg", b)])
                for t, T_, sl in tiles:
                    CP("dve", IXF[sl], IX8[sl][:, 0:4], [("IX8", sl)], [("IXF", sl)])
                for k in range(4):
                    for t, T_, sl in tiles:
                        TS("dve", OH[sl][k], IOTA32, IXF[sl][:, k:k + 1], ALU.is_equal, ["CF", ("IXF", sl)], [("OH", sl, k)])
                for k in range(4):
                    for t, T_, sl in tiles:
                        TT("dve", OH[sl][k], OH[sl][k], POSg[b][:, t, :], ALU.mult, [("OH", sl, k), ("POSg", b)], [("OH", sl, k)])
                for k in range(4):
                    for t, T_, sl in tiles:
                        S.op("dve", lambda e, o=PK[sl], i=OH[sl][k], k=k: e.tensor_reduce(o[:, k:k + 1], i, AX.X, ALU.add),
                             [("OH", sl, k)], [("PK", sl, k)])
                for t, T_, sl in tiles:
                    STT(DF[sl], IXF[sl], float(CAP), PK[sl], ALU.mult, ALU.add, [("IXF", sl)] + [("PK", sl, k) for k in range(4)], [("DF", sl)])
                for t, T_, sl in tiles:
                    TS("dve", OV[sl], PK[sl], float(CAP), ALU.is_ge, [("PK", sl, k) for k in range(4)], [("OV", sl)])
                for t, T_, sl in tiles:
                    TS("dve", PK2[sl], DF[sl], -1.0, ALU.mult, [("DF", sl)], [("PK2", sl)], s2=PCOL, op1=ALU.add)
                for t, T_, sl in tiles:
                    TT("dve", PK2[sl], PK2[sl], OV[sl], ALU.mult, [("PK2", sl), ("OV", sl)], [("PK2", sl)])
                for t, T_, sl in tiles:
                    TT("dve", DF[sl], DF[sl], PK2[sl], ALU.add, [("DF", sl), ("PK2", sl)], [("DF", sl)])
                for t, T_, sl in tiles:
                    CP("dve", DEST[:, T_ * 4:(T_ + 1) * 4], DF[sl], [("DF", sl)], [("DEST", T_)])
                for t, T_, sl in tiles:
                    for k in range(4):
                        S.op("pool", lambda e, src=XBb[sl], T_=T_, k=k: e.indirect_dma_start(
                            out=XG, out_offset=bass.IndirectOffsetOnAxis(DEST[:, T_ * 4 + k:T_ * 4 + k + 1], 0),
                            in_=src, in_offset=None),
                            [("XBb", sl), ("DEST", T_)], [("XGs", T_, k)], dma=True)

            stageA(0)
            stageA1b(0)
            stageA(1)
            stageA2(0)
            stageA1b(1)
            stageB(0)
            stageA(2)
            stageA2(1)
            stageA1b(2)
            stageB(1)
            stageA(3)
            stageA2(2)
            stageA1b(3)
            stageB(2)
            stageA2(3)
            stageB(3)
            if debug:
                DMA(dbg["dest"], DEST, [("DEST", ti) for ti in range(16)], [])
                DMA(dbg["gate"], GATE, [("GATE", ti) for ti in range(16)], [])
            ckpt(5)

            barrier()
            A.reset(0)
            WGb = [A.alloc([128, 8, D], BF16) for _ in range(2)]
            WUb = [A.alloc([128, 8, D], BF16) for _ in range(2)]
            WDb = A.alloc([128, 8, D], BF16)
            NST = 8
            WST6 = [A.alloc([128, D], F32) for _ in range(NST)]
            XS = [A.alloc([128, D], BF16) for _ in range(NBLK)]
            XTE = [A.alloc([128, 8, CAP], BF16) for _ in range(2)]
            HTE = [A.alloc([128, 8, CAP], BF16) for _ in range(2)]
            SG = [A.alloc([128, CAP], F32) for _ in range(2)]
            GG = [A.alloc([128, CAP], F32) for _ in range(2)]
            UU = [A.alloc([128, CAP], F32) for _ in range(2)]
            TTm = [A.alloc([128, CAP], F32) for _ in range(2)]
            YS = [A.alloc([128, D], F32) for _ in range(NBLK)]
            BDB = [A.alloc([128, D], F32) for _ in range(2)]

            wcnt = [0]
            import os as _os
            W_CAST = ["act", "dve", "act"]
            if _os.environ.get("KDBG_WCAST"):
                W_CAST = _os.environ["KDBG_WCAST"].split(",")

            def _load(src_ap, dst_ap, key):
                i = wcnt[0] % NST
                eng = W_CAST[wcnt[0] % len(W_CAST)]
                wcnt[0] += 1
                DMA(WST6[i], src_ap, [], [("WST6", i)])
                CP(eng, dst_ap, WST6[i], [("WST6", i)], [key])

            def load_gu(e, j):
                c = j // 2
                if e >= NE - NPRE:
                    idx = (e - (NE - NPRE)) * 24 + j
                    dst, key = (WGb[e % 2][:, c, :], ("WG", e % 2, c)) if j % 2 == 0 else (WUb[e % 2][:, c, :], ("WU", e % 2, c))
                    DMA(dst, WB[idx * 128:(idx + 1) * 128, :], [], [key])
                    return
                if j % 2 == 0:
                    _load(wg[e, c * 128:(c + 1) * 128, :], WGb[e % 2][:, c, :], ("WG", e % 2, c))
                else:
                    _load(wu[e, c * 128:(c + 1) * 128, :], WUb[e % 2][:, c, :], ("WU", e % 2, c))

            def load_d(e, f):
                if e >= NE - NPRE:
                    idx = (e - (NE - NPRE)) * 24 + 16 + f
                    DMA(WDb[:, f, :], WB[idx * 128:(idx + 1) * 128, :], [], [("WD", f)])
                    return
                _load(wd[e, f * 128:(f + 1) * 128, :], WDb[:, f, :], ("WD", f))

            def load_xs(e):
                rd = [("XGs", ti, k) for ti in range(16) for k in range(4)] if e == 0 else []
                for blk in range(NBLK):
                    r0 = e * CAP + blk * 128
                    import os as _os
                    if _os.environ.get("KDBG_XSZERO"):
                        S.op("pool", lambda e, ap=XS[blk]: e.memset(ap, 0.5), [], [("XS", blk)])
                    else:
                        DMA(XS[blk], XG[r0:r0 + 128, :], rd, [("XS", blk)])
                DMA(BDB[e % 2], bd[e, :].partition_broadcast(128), [], [("BDB", e % 2)])

            def expert(e):
                eb = e % 2
                xte_keys = [("XTE", eb, c) for c in range(8)]
                for c in range(8):
                    pb = 6 + (c % 2)
                    pst = PSUM[:, pb * 512:pb * 512 + CAP // 2].bitcast(BF16)
                    for blk in range(NBLK):
                        TR(pst[:, blk * 128:(blk + 1) * 128], XS[blk][:, c * 128:(c + 1) * 128], IDENTb,
                           [("XS", blk), "CB"], [("ps", pb)])
                    CP("act", XTE[eb][:, c, :], pst, [("ps", pb)], [("XTE", eb, c)])
                import os as _os
                _sub = int(_os.environ.get("KDBG_SUB", 9))
                if _sub < 1:
                    return
                if e + 1 < NE:
                    load_xs(e + 1)
                if _sub < 2:
                    return
                for f in range(8):
                    pg = (f % 2) * 2
                    pu = pg + 1
                    fs = slice(f * 128, (f + 1) * 128)
                    for c in range(8):
                        MM(PS(pg, 128, 0, CAP), WGb[eb][:, c, fs], XTE[eb][:, c, :], c == 0, c == 7,
                           [("WG", eb, c)] + xte_keys, [("ps", pg)])
                    for c in range(8):
                        MM(PS(pu, 128, 0, CAP), WUb[eb][:, c, fs], XTE[eb][:, c, :], c == 0, c == 7,
                           [("WU", eb, c)] + xte_keys, [("ps", pu)])
                    q = f % 2
                    bcol = slice(e * 8 + f, e * 8 + f + 1)
                    if _os.environ.get("KDBG_NOSW"):
                        continue
                    TS("dve", GG[q], PS(pg, 128, 0, CAP), BG[:, bcol], ALU.add, [("ps", pg), "BG"], [("GG", q)], s2=7.0, op1=ALU.min)
                    ACTV(SG[q], GG[q], AF.Sigmoid, [("GG", q)], [("SG", q)], scale=1.702)
                    TS("dve", UU[q], PS(pu, 128, 0, CAP), BU1[:, bcol], ALU.add, [("ps", pu), "BU1"], [("UU", q)], s2=8.0, op1=ALU.min)
                    TT("dve", TTm[q], SG[q], GG[q], ALU.mult, [("SG", q), ("GG", q)], [("TTm", q)])
                    STT(HTE[eb][:, f, :], UU[q], -6.0, TTm[q], ALU.max, ALU.mult, [("UU", q), ("TTm", q)], [("HTE", eb, f)])
                    load_d(e, f)
                    if e + 1 < NE:
                        load_gu(e + 1, f)
                if _sub < 3:
                    return
                for blk in range(NBLK):
                    for hf in range(2):
                        pb = 4 + hf
                        for f in range(8):
                            MM(PS(pb), HTE[eb][:, f, blk * 128:(blk + 1) * 128], WDb[:, f, hf * 512:(hf + 1) * 512],
                               f == 0, f == 7, [("HTE", eb, f), ("WD", f)], [("ps", pb)])
                        TT("dve", YS[blk][:, hf * 512:(hf + 1) * 512], PS(pb), BDB[eb][:, hf * 512:(hf + 1) * 512], ALU.add,
                           [("ps", pb), ("BDB", eb)], [("YS", blk)])
                        if e + 1 < NE:
                            gi = blk * 2 + hf
                            for j in DOWN_SPREAD[gi]:
                                load_gu(e + 1, j)
                for blk in range(NBLK):
                    r0 = e * CAP + blk * 128
                    DMA(YG[r0:r0 + 128, :], YS[blk], [("YS", blk)], [("YG", e, blk)])

            S.op("pool", lambda e: e.memset(YS[0], 0.0), [], [("YS", 0)])
            DMA(YG[NSLOT:NSLOT + 128, :], YS[0], [("YS", 0)], [("YGtrash",)])
            DOWN_SPREAD = [[8, 9], [10], [11], [12, 13], [14], [15]]
            load_xs(0)
            for j in range(16):
                load_gu(0, j)
            import os as _os
            for e in range(int(_os.environ.get("KDBG_NE", NE))):
                expert(e)
            ckpt(6)

            barrier()
            A.reset(0)
            G2 = A.alloc([128, D], F32)
            B2 = A.alloc([128, D], F32)
            NB7 = 3
            YK = [[A.alloc([128, D], F32) for _ in range(4)] for _ in range(NB7)]
            HH = [A.alloc([128, D], F32) for _ in range(NB7)]
            ACC = [A.alloc([128, D], F32) for _ in range(NB7)]
            OUT = [A.alloc([128, D], F32) for _ in range(NB7)]
            ST7 = [A.alloc([128, 12], F32) for _ in range(NB7)]
            MV7 = [A.alloc([128, 2], F32) for _ in range(NB7)]
            RS7 = [A.alloc([128, 4], F32) for _ in range(NB7)]
            DMA(G2, ln2g.partition_broadcast(128), [], ["G2"])
            DMA(B2, ln2b.partition_broadcast(128), [], ["B2"])
            ygk = [("YG", e, blk) for e in range(NE) for blk in range(NBLK)]

            def fetch7(ti):
                p = ti % NB7
                ts_ = slice(ti * 128, (ti + 1) * 128)
                DMA(HH[p], H1[ts_, :], [("H1", ti)], [("HH", p)])
                for k in range(4):
                    S.op("pool", lambda e, p=p, ti=ti, k=k: e.indirect_dma_start(
                        out=YK[p][k], out_offset=None, in_=YG,
                        in_offset=bass.IndirectOffsetOnAxis(DEST[:, ti * 4 + k:ti * 4 + k + 1], 0)),
                        [("DEST", ti)] + (ygk if (ti == 0 and k == 0) else []), [("YK", p, k)], dma=True)

            def tile7(ti):
                p = ti % NB7
                ts_ = slice(ti * 128, (ti + 1) * 128)
                if ti + 2 < 16:
                    fetch7(ti + 2)
                yield
                for k in (0, 2):
                    S.op("act", lambda e, ap=YK[p][k], g=GATE[:, ti * 4 + k:ti * 4 + k + 1]: e.mul(ap, ap, g),
                         [("YK", p, k), ("GATE", ti)], [("YK", p, k)])
                    yield
                for k in (1, 3):
                    STT(YK[p][k], YK[p][k], GATE[:, ti * 4 + k:ti * 4 + k + 1], YK[p][k - 1], ALU.mult, ALU.add,
                        [("YK", p, k), ("YK", p, k - 1), ("GATE", ti)], [("YK", p, k)])
                    yield
                TT("pool", ACC[p], YK[p][1], YK[p][3], ALU.add, [("YK", p, 1), ("YK", p, 3)], [("ACC", p)])
                yield
                STT(ACC[p], HH[p], DN_ALPHA, ACC[p], ALU.mult, ALU.add, [("HH", p), ("ACC", p)], [("ACC", p)])
                yield
                yield from layer_norm(ACC[p], ("ACC", p), OUT[p], ("OUT", p), ST7[p], MV7[p], RS7[p], G2, B2, p, EPS5, "dve")
                DMA(y[ts_, :], OUT[p], [("OUT", p)], [("y", ti)])
                yield

            fetch7(0)
            fetch7(1)
            gens7 = []
            nxt7 = [0]
            st7 = {}
            LAG7 = 8
            while nxt7[0] < 16 or gens7:
                if nxt7[0] < 16 and len(gens7) < 2 and (not gens7 or st7[gens7[0][0]] >= LAG7):
                    gens7.append((nxt7[0], tile7(nxt7[0])))
                    st7[nxt7[0]] = 0
                    nxt7[0] += 1
                for item in list(gens7):
                    tid, gen = item
                    try:
                        next(gen)
                        st7[tid] += 1
                    except StopIteration:
                        gens7.remove(item)

        except _Stop:
            pass
        run_block(nc, S, sems)
    return nc


def _consts():
    bf = ml_dtypes.bfloat16
    ident = np.eye(128, dtype=np.float32)
    k = np.arange(128)[:, None]
    q = np.arange(128)[None, :]
    tri = np.where(k <= q, 0.0, NEG).astype(np.float32)
    tris = (k < q).astype(np.float32)
    cbf = np.concatenate([ident, tri, tris], axis=1).astype(bf)
    cf = np.zeros((128, 176), np.float32)
    cf[:, 0:128] = ident
    cf[:, 128:160] = np.arange(32, dtype=np.float32)[None, :]
    cf[:, 160:168] = (np.arange(128)[:, None] // 16 == np.arange(8)[None, :]).astype(np.float32)
    inv_a = np.power(np.float32(500000.0), -np.arange(0, 32, 2, dtype=np.float32) / np.float32(32)).astype(np.float32)
    inv_b = np.power(np.float32(500000.0), -np.arange(0, 16, 2, dtype=np.float32) / np.float32(16)).astype(np.float32)
    fa = np.zeros((128, 2), np.float32)
    for base in (0, 64):
        fa[base:base + 16, 0] = inv_a
        fa[base + 16:base + 32, 0] = inv_a
        fa[base:base + 16, 1] = -1.0
        fa[base + 16:base + 32, 1] = 1.0
    fb = np.zeros((128, 2), np.float32)
    for h in range(8):
        fb[16 * h:16 * h + 8, 0] = inv_b
        fb[16 * h + 8:16 * h + 16, 0] = inv_b
        fb[16 * h:16 * h + 8, 1] = -1.0
        fb[16 * h + 8:16 * h + 16, 1] = 1.0
    cf[:, 168:170] = fa
    cf[:, 170:172] = fb
    cf[:, 172] = np.float32(math.pi / 2)
    cf[:, 173] = 1e-6
    cf[:, 174] = 1e-5
    cf[:, 175] = NSLOT + np.arange(128, dtype=np.float32)
    pm = np.zeros((8, 8, 8), np.float32)
    own = np.zeros((8, 8, 8), np.float32)
    for cur in range(8):
        pm[cur, :, cur:] = -1e30
        own[cur, :, cur] = 1.0
    cpm = np.concatenate([pm.reshape(1, 512), own.reshape(1, 512)], axis=1)
    cpm = np.ascontiguousarray(np.broadcast_to(cpm, (128, 1024))).astype(np.float32)
    ind = (np.arange(T)[None, :] // 256 == np.arange(8)[:, None]).astype(np.float32).astype(bf)
    return cbf, cf, cpm, ind


def _prep_shared(inp):
    f = lambda a: np.ascontiguousarray(np.asarray(a, dtype=np.float32))
    w_in = f(inp["w_in"])[0]
    kr = w_in[:, 384:416]
    kr_sw = np.concatenate([kr[:, 16:32], kr[:, 0:16]], axis=1)
    w1a = np.concatenate([w_in[:, 0:384], kr, kr_sw], axis=1)

    def moba_cols(wm):
        wh = wm.reshape(D, 8, 64)
        c0 = wh[:, :, 0:16].reshape(D, 128)
        c0s = np.concatenate([wh[:, :, 8:16], wh[:, :, 0:8]], axis=2).reshape(D, 128)
        rest = [wh[:, :, 16 + 16 * j:32 + 16 * j].reshape(D, 128) for j in range(3)]
        return [c0, c0s] + rest

    w1b = np.concatenate(moba_cols(w_in[:, 416:928]) + moba_cols(w_in[:, 928:1440]) + [w_in[:, 1440:1952]], axis=1)
    wq = f(inp["w_q_b"])[0]
    wqh = wq.reshape(256, 8, 96)
    wqs = np.concatenate([wqh[:, :, 0:64], wqh[:, :, 80:96], wqh[:, :, 64:80]], axis=2).reshape(256, 768)
    wkv = f(inp["w_kv_b"])[0].reshape(128, 8, 128)
    wk = wkv[:, :, 0:64].reshape(128, 512)
    wv = wkv[:, :, 64:128].reshape(128, 512)
    cbf, cf, cpm, ind = _consts()
    sh = {
        "w1a": w1a, "w1b": w1b, "wq": wq, "wqs": wqs,
        "qg": f(inp["q_a_norm"])[0].reshape(2, 128).T, "wk": wk, "wv": wv,
        "kvg": f(inp["kv_a_norm"])[0].reshape(128, 1),
        "wo": f(inp["w_o"])[0], "ln1g": f(inp["ln1_g"])[0], "ln1b": f(inp["ln1_b"])[0],
        "wr": f(inp["w_router"])[0], "br": f(inp["b_router"])[0].reshape(1, NE),
        "wg": f(inp["w_gate"])[0], "wu": f(inp["w_up"])[0], "wd": f(inp["w_down"])[0],
        "bg": f(inp["b_gate"])[0].reshape(NE, 8, 128).transpose(2, 0, 1).reshape(128, NE * 8),
        "bu": f(inp["b_up"])[0].reshape(NE, 8, 128).transpose(2, 0, 1).reshape(128, NE * 8),
        "bd": f(inp["b_down"])[0], "ln2g": f(inp["ln2_g"])[0], "ln2b": f(inp["ln2_b"])[0],
        "cbf": cbf, "cf": cf, "cpm": cpm, "cind": ind,
    }
    return {k: np.ascontiguousarray(v) for k, v in sh.items()}


def make_in_maps(inp, n_cores=8):
    sh = _prep_shared(inp)
    x = np.asarray(inp["x"], dtype=np.float32)
    posn = np.asarray(inp["positions"]).astype(np.int32)
    maps = []
    for c in range(n_cores):
        m = dict(sh)
        m["xT"] = np.ascontiguousarray(x[c].T)
        m["xtok"] = np.ascontiguousarray(x[c])
        m["pos"] = np.ascontiguousarray(posn[c])
        maps.append(m)
    return maps


def kernel(**inputs):
    nc = build_program(debug=False)
    maps = make_in_maps(inputs, 8)
    res = run_bass_kernel_spmd(nc, maps, core_ids=list(range(8)))
    out = np.stack([np.asarray(r["y"], dtype=np.float32) for r in res.results], axis=0)
    return out.reshape(8, T, D)
```

```python
import math
from contextlib import ExitStack

import numpy as np
import ml_dtypes

import concourse.bass as bass
import concourse.mybir as mybir
from concourse.bass_utils import run_bass_kernel_spmd

F32 = mybir.dt.float32
BF16 = mybir.dt.bfloat16
I32 = mybir.dt.int32
U32 = mybir.dt.uint32
ALU = mybir.AluOpType
AF = mybir.ActivationFunctionType
AX = mybir.AxisListType

T = 2048
D = 1024
NE = 32
CAP = 384
NSLOT = NE * CAP
NBLK = CAP // 128
NPRE = 7
DN_ALPHA = 2.0 ** 0.25
NEG = -30000.0
SIGC = float(1.0 / (1.0 + math.exp(-1.702 * 7.0)))

COMPUTE = ("pe", "act", "dve", "pool")
SAME_ENGINE_SYNC = {"act": True, "dve": True, "pool": True, "pe": False}
DMA_POOL = {"sync": 24, "act": 8, "pool": 16}


class Op:
    __slots__ = ("eng", "fn", "dma", "deps", "idx", "has_dep", "tok", "pre")

    def __init__(self, eng, fn, dma, idx):
        self.eng = eng
        self.fn = fn
        self.dma = dma
        self.deps = set()
        self.idx = idx
        self.has_dep = False
        self.tok = None
        self.pre = None


class Sched:
    def __init__(self):
        self.ops = []
        self.last_writer = {}
        self.readers = {}

    def op(self, eng, fn, reads=(), writes=(), dma=False):
        o = Op(eng, fn, dma, len(self.ops))
        self.ops.append(o)
        reads = list(reads) + ["PHASE"]
        writes = list(writes) + [r for r in reads if isinstance(r, tuple) and r[0] == "ps" and r not in writes]
        for r in reads:
            w = self.last_writer.get(r)
            if w is not None:
                o.deps.add(w)
        for wkey in writes:
            w = self.last_writer.get(wkey)
            if w is not None:
                o.deps.add(w)
            rd = self.readers.get(wkey)
            if rd:
                for x in rd["c"].values():
                    o.deps.add(x)
                for x in rd["d"]:
                    o.deps.add(x)
            self.last_writer[wkey] = o
            self.readers[wkey] = {"c": {}, "d": []}
        for r in reads:
            rd = self.readers.setdefault(r, {"c": {}, "d": []})
            if dma:
                rd["d"].append(o)
            else:
                rd["c"][eng] = o
        o.deps.discard(o)
        return o

    def dma(self, eng, out, in_, reads=(), writes=(), **kw):
        return self.op(eng, lambda e: e.dma_start(out=out, in_=in_, **kw), reads, writes, dma=True)

    def emit(self, sems):
        ops = self.ops
        for o in ops:
            keep = set()
            for d in o.deps:
                if (not d.dma) and (not o.dma) and d.eng == o.eng and not SAME_ENGINE_SYNC[o.eng]:
                    continue
                keep.add(d)
            o.deps = keep
            for d in keep:
                d.has_dep = True
        cnt = {e: 0 for e in COMPUTE}
        dcnt = {e: 0 for e in DMA_POOL}
        for o in ops:
            if o.dma:
                i = dcnt[o.eng]
                dcnt[o.eng] += 1
                n = DMA_POOL[o.eng]
                sem = sems["d_%s_%d" % (o.eng, i % n)]
                o.tok = (sem, 16 * (i // n + 1))
                o.pre = (sem, 16 * (i // n)) if i >= n else None
            elif o.has_dep:
                cnt[o.eng] += 1
                o.tok = (sems["c_" + o.eng], cnt[o.eng])
        self.final_dma = []
        for e in DMA_POOL:
            n = DMA_POOL[e]
            tot = dcnt[e]
            for j in range(min(n, tot)):
                k = (tot - 1 - j) // n + 1
                self.final_dma.append((sems["d_%s_%d" % (e, j)], 16 * k))
        self.streams = {e: [] for e in ("pe", "act", "dve", "pool", "sync")}
        for o in ops:
            self.streams[o.eng].append(o)

    def run_stream(self, name, eng, extra_final=()):
        waited = {}

        def wait(sem, val):
            key = id(sem)
            if waited.get(key, 0) < val:
                eng.wait_ge(sem, val)
                waited[key] = val

        for o in self.streams[name]:
            for d in sorted(o.deps, key=lambda x: x.idx):
                wait(*d.tok)
            if o.pre is not None:
                wait(*o.pre)
            ins = o.fn(eng)
            if o.tok is not None:
                ins.then_inc(o.tok[0], 16 if o.dma else 1)
        for (sem, val) in extra_final:
            wait(sem, val)


def make_sems(nc, stack):
    sems = {}
    for e in COMPUTE:
        sems["c_" + e] = stack.enter_context(nc.semaphore("c_" + e))
    for e, n in DMA_POOL.items():
        for i in range(n):
            sems["d_%s_%d" % (e, i)] = stack.enter_context(nc.semaphore("d_%s_%d" % (e, i)))
    return sems


def run_block(nc, sched, sems):
    sched.emit(sems)
    finals = sched.final_dma
    with nc.Block() as block:
        @block.sync
        def _(eng):
            sched.run_stream("sync", eng, extra_final=finals)

        @block.scalar
        def _(eng):
            sched.run_stream("act", eng)

        @block.vector
        def _(eng):
            sched.run_stream("dve", eng)

        @block.gpsimd
        def _(eng):
            sched.run_stream("pool", eng)

        @block.tensor
        def _(eng):
            sched.run_stream("pe", eng)


DT_SIZE = {F32: 4, BF16: 2, I32: 4, U32: 4}


class Arena:
    def __init__(self, ar, size_f32):
        self.ar = ar
        self.size = size_f32
        self.off = 0
        self.peak = 0

    def reset(self, off=0):
        self.off = off

    def alloc(self, shape, dtype):
        n = 1
        for s in shape[1:]:
            n *= s
        n32 = (n * DT_SIZE[dtype] + 3) // 4
        n32 = (n32 + 1) // 2 * 2
        assert self.off + n32 <= min(self.size, getattr(self, "limit", self.size)), ("arena overflow", self.off, n32, self.size)
        v = self.ar[:, self.off:self.off + n32]
        self.off += n32
        self.peak = max(self.peak, self.off)
        if dtype != F32:
            v = v.bitcast(dtype)
        v = v[:, 0:n]
        if len(shape) == 3:
            v = v.rearrange("p (a b) -> p a b", a=shape[1])
        elif len(shape) == 4:
            v = v.rearrange("p (a b c) -> p a b c", a=shape[1], b=shape[2])
        return v[0:shape[0]]


class _Stop(Exception):
    pass


def build_program(debug=False, stop=99):
    nc = bass.Bass("TRN2", target_bir_lowering=False)
    S = Sched()

    def dram_in(name, shape, dt):
        return nc.dram_tensor(name, list(shape), dt, kind="ExternalInput").ap()

    xT = dram_in("xT", [D, T], F32)
    xtok = dram_in("xtok", [T, D], F32)
    pos = dram_in("pos", [T], I32)
    w1a = dram_in("w1a", [D, 448], F32)
    w1b = dram_in("w1b", [D, 1792], F32)
    wq = dram_in("wq", [256, 768], F32)
    wqs = dram_in("wqs", [256, 768], F32)
    qg = dram_in("qg", [128, 2], F32)
    wk = dram_in("wk", [128, 512], F32)
    wv = dram_in("wv", [128, 512], F32)
    kvg = dram_in("kvg", [128, 1], F32)
    wo = dram_in("wo", [D, D], F32)
    ln1g = dram_in("ln1g", [D], F32)
    ln1b = dram_in("ln1b", [D], F32)
    wr = dram_in("wr", [D, NE], F32)
    br = dram_in("br", [1, NE], F32)
    wg = dram_in("wg", [NE, D, D], F32)
    wu = dram_in("wu", [NE, D, D], F32)
    wd = dram_in("wd", [NE, D, D], F32)
    bg = dram_in("bg", [128, NE * 8], F32)
    bu = dram_in("bu", [128, NE * 8], F32)
    bd = dram_in("bd", [NE, D], F32)
    ln2g = dram_in("ln2g", [D], F32)
    ln2b = dram_in("ln2b", [D], F32)
    cbf = dram_in("cbf", [128, 384], BF16)
    cf = dram_in("cf", [128, 176], F32)
    cpm = dram_in("cpm", [128, 1024], F32)
    cind = dram_in("cind", [8, T], BF16)
    y = nc.dram_tensor("y", [T, D], F32, kind="ExternalOutput").ap()
    XG = nc.dram_tensor("XG", [NSLOT + 128, D], BF16, kind="Internal").ap()
    YG = nc.dram_tensor("YG", [NSLOT + 128, D], F32, kind="Internal").ap()
    WB = nc.dram_tensor("WB", [NPRE * 24 * 128, D], BF16, kind="Internal").ap()
    H1 = nc.dram_tensor("H1", [T, D], F32, kind="ExternalOutput" if debug else "Internal").ap()
    dbg = {}
    if debug:
        dbg["AT"] = nc.dram_tensor("dAT", [128, 8 * T], BF16, kind="ExternalOutput").ap()
        dbg["dest"] = nc.dram_tensor("ddest", [128, 64], I32, kind="ExternalOutput").ap()
        dbg["gate"] = nc.dram_tensor("dgate", [128, 64], F32, kind="ExternalOutput").ap()

    with ExitStack() as st:
        sems = make_sems(nc, st)
        ARN = 47616
        AR = st.enter_context(nc.sbuf_tensor("AR", [128, ARN], F32))
        CB = st.enter_context(nc.sbuf_tensor("CB", [128, 512], BF16))
        CF = st.enter_context(nc.sbuf_tensor("CF", [128, 176], F32))
        SM = st.enter_context(nc.sbuf_tensor("SM", [128, 2048], F32))
        PSUM = st.enter_context(nc.psum_tensor("PSUM", [128, 4096], F32))
        A = Arena(AR, ARN)

        def PS(b, rows=128, c0=0, c1=512):
            return PSUM[0:rows, b * 512 + c0:b * 512 + c1]

        IDENTb = CB[:, 0:128]
        TRIb = CB[:, 128:256]
        TRISb = CB[:, 256:384]
        ONESb = CB[:, 384:512]
        IDENTf = CF[:, 0:128]
        IOTA32 = CF[:, 128:160]
        HEADM = CF[:, 160:168]
        FA = CF[:, 168:170]
        FB = CF[:, 170:172]
        HALFPI = CF[:, 172:173]
        EPS6 = CF[:, 173:174]
        EPS5 = CF[:, 174:175]
        PCOL = CF[:, 175:176]
        smo = [0]

        def sm_alloc(n, dtype=F32):
            v = SM[:, smo[0]:smo[0] + n]
            smo[0] += n
            assert smo[0] <= 2048
            return v.bitcast(dtype) if dtype != F32 else v

        DEST = sm_alloc(64, I32)
        GATE = sm_alloc(64)
        ONESF = sm_alloc(128)
        BGS = sm_alloc(256)
        BG = sm_alloc(256)
        BU1 = sm_alloc(256)
        QG = sm_alloc(2)
        KVG = sm_alloc(2)
        BR = sm_alloc(32)

        def MM(out, lhsT, rhs, start, stop, R, W):
            S.op("pe", lambda e: e.matmul(out, lhsT, rhs, start=start, stop=stop), R, W)

        def TR(out, in_, ident, R, W):
            S.op("pe", lambda e: e.transpose(out, in_, ident), R, W)

        def ACTV(out, in_, func, R, W, **kw):
            S.op("act", lambda e: e.activation(out, in_, func, **kw), R, W)

        def CP(eng, out, in_, R, W):
            if eng == "act":
                S.op("act", lambda e: e.copy(out, in_), R, W)
            else:
                S.op(eng, lambda e: e.tensor_copy(out, in_), R, W)

        def TT(eng, out, a, b, op, R, W):
            S.op(eng, lambda e: e.tensor_tensor(out, a, b, op), R, W)

        def TS(eng, out, a, s1, op0, R, W, s2=None, op1=None):
            if op1 is None:
                S.op(eng, lambda e: e.tensor_scalar(out, a, s1, None, op0), R, W)
            else:
                S.op(eng, lambda e: e.tensor_scalar(out, a, s1, s2, op0, op1), R, W)

        def STT(out, in0, scalar, in1, op0, op1, R, W):
            S.op("dve", lambda e: e.scalar_tensor_tensor(out, in0, scalar, in1, op0, op1), R, W)

        def DMA(out, in_, R, W, eng="sync"):
            S.dma(eng, out, in_, R, W)

        def barrier():
            S.op("pool", lambda e: e.memset(SM[0:1, 2040:2042], 0.0), reads=[], writes=["PHASE"])

        def dump(name, ap2d, shape, dt, keys):
            if not debug:
                return
            t = nc.dram_tensor("dd_" + name, list(shape), dt, kind="ExternalOutput").ap()
            DMA(t, ap2d, keys, [])

        def ckpt(k):
            if k >= stop:
                raise _Stop()

        try:
            DMA(CB[:, 0:384], cbf, [], ["CB"])
            DMA(CF[:], cf, [], ["CF"])
            S.op("pool", lambda e: e.memset(ONESb, 1.0), [], ["CBo"])
            S.op("pool", lambda e: e.memset(ONESF, 1.0), [], ["ONESF"])
            DMA(BG, bg, [], ["BG"])
            DMA(BU1, bu, [], ["BU1"])
            DMA(QG, qg, [], ["QG"])
            DMA(KVG[:, 0:1], kvg, [], ["KVG"])
            DMA(BR[0:1, :], br, [], ["BR"])
            ZT = sm_alloc(512)
            S.op("pool", lambda e: e.memset(ZT, 0.0), [], ["ZT"])
            XGv = XG
            ZTb = ZT.bitcast(BF16)
            NZ = (NSLOT + 128) // 128
            zc = [0]

            def zfill(n):
                for _ in range(n):
                    if zc[0] >= NZ:
                        return
                    i = zc[0]
                    zc[0] += 1
                    DMA(XGv[i * 128:(i + 1) * 128, :], ZTb, ["ZT"], [("XGz", i)])

            TS("pool", BGS, BG, 1.702, ALU.mult, ["BG"], ["BGS"])
            TS("pool", BU1, BU1, 1.0, ALU.add, ["BU1"], ["BU1"])

            def build_tables(fcol, out_c, out_s, kc, ks, tmp):
                posi, ang, kk, rr = tmp
                DMA(posi, pos.partition_broadcast(128), [], ["t_posi"])
                CP("dve", ang, posi, ["t_posi"], ["t_ang"])
                TS("dve", ang, ang, fcol[:, 0:1], ALU.mult, ["t_ang", "CF"], ["t_ang"])
                TS("dve", kk, ang, 1.0 / (2.0 * math.pi), ALU.mult, ["t_ang"], ["t_k"], s2=12582912.0, op1=ALU.add)
                TS("dve", kk, kk, -12582912.0, ALU.add, ["t_k"], ["t_k"])
                C1 = 6.28125
                C2 = float(np.float32(2.0 * math.pi - 6.28125))
                STT(rr, kk, -C1, ang, ALU.mult, ALU.add, ["t_k", "t_ang"], ["t_r"])
                STT(rr, kk, -C2, rr, ALU.mult, ALU.add, ["t_k", "t_r"], ["t_r"])
                TS("dve", rr, rr, -3.1415925, ALU.max, ["t_r"], ["t_r"], s2=3.1415925, op1=ALU.min)
                ACTV(out_s, rr, AF.Sin, ["t_r", "CF"], [ks], scale=fcol[:, 1:2])
                STT(kk, rr, -1.0, rr, ALU.mult, ALU.max, ["t_r"], ["t_k"])
                ACTV(out_c, kk, AF.Sin, ["t_k", "CF"], [kc], scale=-1.0, bias=HALFPI)

            AT = A.alloc([128, 8, T], BF16)
            base_after_AT = A.off
            BGTOP = ARN - 4 * 512
            BGB = [AR[:, BGTOP + i * 512:BGTOP + (i + 1) * 512].bitcast(BF16) for i in range(4)]
            A.limit = BGTOP
            bg_seq = []

            def _bg_in(idx, src_ap):
                i = idx % 4
                return lambda: S.dma("pool", BGB[i], src_ap, [], [("BGB", i)])

            def _bg_out(idx):
                i = idx % 4
                return lambda: S.dma("pool", WB[idx * 128:(idx + 1) * 128, :], BGB[i], [("BGB", i)], [("WB", idx)])

            _ins, _outs = [], []
            for ei in range(NPRE):
                ee = ei
                for j in range(24):
                    idx = ei * 24 + j
                    if j < 16:
                        src = (wg if j % 2 == 0 else wu)[ee, (j // 2) * 128:(j // 2 + 1) * 128, :]
                    else:
                        src = wd[ee, (j - 16) * 128:(j - 15) * 128, :]
                    _ins.append(_bg_in(idx, src))
                    _outs.append(_bg_out(idx))
            nchunk = len(_ins)
            for k in range(nchunk + 2):
                if k < nchunk:
                    bg_seq.append(_ins[k])
                if k >= 2:
                    bg_seq.append(_outs[k - 2])
            bgc = [0]

            def bg(n):
                for _ in range(n):
                    if bgc[0] >= len(bg_seq):
                        return
                    bg_seq[bgc[0]]()
                    bgc[0] += 1


            TAc = A.alloc([128, T], F32)
            TAs = A.alloc([128, T], F32)
            off0 = A.off
            tmp_tab = [A.alloc([128, T], I32), A.alloc([128, T], F32), A.alloc([128, T], F32), A.alloc([128, T], F32)]
            build_tables(FA, TAc, TAs, "TAc", "TAs", tmp_tab)
            ckpt(0)
            barrier()
            A.reset(off0)
            QN = A.alloc([128, 2, T], BF16)
            KVN = A.alloc([128, T], BF16)
            KR = A.alloc([32, T], BF16)
            VALL = A.alloc([128, 16, 512], BF16)
            QAb = [A.alloc([128, T], BF16) for _ in range(2)]
            KAb = [A.alloc([128, T], BF16) for _ in range(2)]
            VAb = [A.alloc([128, 16, 128], BF16) for _ in range(2)]
            PT = [A.alloc([128, 512], BF16) for _ in range(4)]
            WQ = A.alloc([128, 2, 768], BF16)
            WQS = A.alloc([128, 2, 768], BF16)
            WK = A.alloc([128, 512], BF16)
            WV = A.alloc([128, 512], BF16)
            RC = [A.alloc([128, 512], F32) for _ in range(2)]
            T1 = A.alloc([128, 512], F32)
            T2 = A.alloc([128, 512], F32)
            ph12_common_end = A.off
            W1A = A.alloc([128, 8, 448], BF16)
            XG16 = [A.alloc([128, 8, 512], BF16) for _ in range(2)]
            XST = [A.alloc([128, 512], F32) for _ in range(4)]
            WST = [A.alloc([128, 768], F32) for _ in range(2)]
            SQ = [A.alloc([128, 512], BF16) for _ in range(2)]
            RQ = A.alloc([128, 512], F32)

            cast_rr = [0]
            CAST_ENG = ["act", "dve", "pool"]

            def cast(out, in_, R, W, eng=None):
                if eng is None:
                    eng = CAST_ENG[cast_rr[0] % 3]
                    cast_rr[0] += 1
                CP(eng, out, in_, R, W)

            for c in range(2):
                DMA(WST[0][:, 0:768], wq[c * 128:(c + 1) * 128, :], [], ["WST0"])
                TS("dve", WQ[:, c, :], WST[0][:, 0:768], QG[:, c:c + 1], ALU.mult, ["WST0", "QG"], ["WQ"])
                DMA(WST[1][:, 0:768], wqs[c * 128:(c + 1) * 128, :], [], ["WST1"])
                TS("pool", WQS[:, c, :], WST[1][:, 0:768], QG[:, c:c + 1], ALU.mult, ["WST1", "QG"], ["WQS"])
            DMA(WST[0][:, 0:512], wk, [], ["WST0"])
            TS("dve", WK, WST[0][:, 0:512], KVG[:, 0:1], ALU.mult, ["WST0", "KVG"], ["WK"])
            DMA(WST[1][:, 0:512], wv, [], ["WST1"])
            TS("pool", WV, WST[1][:, 0:512], KVG[:, 0:1], ALU.mult, ["WST1", "KVG"], ["WV"])
            for c in range(8):
                DMA(WST[c % 2][:, 0:448], w1a[c * 128:(c + 1) * 128, :], [], ["WST%d" % (c % 2)])
                cast(W1A[:, c, :], WST[c % 2][:, 0:448], ["WST%d" % (c % 2)], [("W1A", c)])

            psrr = [0]

            def nbank():
                b = psrr[0] % 8
                psrr[0] += 1
                return b

            xcnt = [0]

            def load_xgroup(g):
                xb = XG16[g % 2]
                for c in range(8):
                    i = xcnt[0] % 4
                    xcnt[0] += 1
                    DMA(XST[i], xT[c * 128:(c + 1) * 128, g * 512:(g + 1) * 512], [], [("XST", i)])
                    cast(xb[:, c, :], XST[i], [("XST", i)], [("XG16", g % 2, c)])
                    zfill(2)
                return xb

            def proj_chunk(xb, g, W, wkeys, c0, c1, bank):
                M = c1 - c0
                for c in range(8):
                    MM(PS(bank, M), W[:, c, c0:c1], xb[:, c, :], c == 0, c == 7,
                       [wkeys(c), ("XG16", g % 2, c)], [("ps", bank)])

            for g in range(4):
                gs = slice(g * 512, (g + 1) * 512)
                xb = load_xgroup(g)
                wkey = lambda c: ("W1A", c)
                for j in range(2):
                    b = nbank()
                    proj_chunk(xb, g, W1A, wkey, j * 128, (j + 1) * 128, b)
                    CP("act", QN[:, j, gs], PS(b), [("ps", b)], [("QN", j, g)])
                    ACTV(SQ[j], PS(b), AF.Square, [("ps", b)], [("SQ", j)])
                b = nbank()
                MM(PS(b), ONESb, SQ[0], True, False, ["CBo", ("SQ", 0)], [("ps", b)])
                MM(PS(b), ONESb, SQ[1], False, True, ["CBo", ("SQ", 1)], [("ps", b)])
                ACTV(RQ, PS(b), AF.Sqrt, [("ps", b), "CF"], ["RQ"], scale=1.0 / 256.0, bias=EPS6)
                S.op("dve", lambda e: e.reciprocal(RQ, RQ), ["RQ"], ["RQ"])
                for j in range(2):
                    TT("dve", QN[:, j, gs], QN[:, j, gs], RQ, ALU.mult, [("QN", j, g), "RQ"], [("QN", j, g)])
                b = nbank()
                proj_chunk(xb, g, W1A, wkey, 256, 384, b)
                CP("act", KVN[:, gs], PS(b), [("ps", b)], [("KVN", g)])
                ACTV(SQ[0], PS(b), AF.Square, [("ps", b)], [("SQ", 0)])
                b = nbank()
                MM(PS(b), ONESb, SQ[0], True, True, ["CBo", ("SQ", 0)], [("ps", b)])
                ACTV(RQ, PS(b), AF.Sqrt, [("ps", b), "CF"], ["RQ"], scale=1.0 / 128.0, bias=EPS6)
                S.op("dve", lambda e: e.reciprocal(RQ, RQ), ["RQ"], ["RQ"])
                TT("dve", KVN[:, gs], KVN[:, gs], RQ, ALU.mult, [("KVN", g), "RQ"], [("KVN", g)])
                ba = nbank()
                proj_chunk(xb, g, W1A, wkey, 384, 416, ba)
                bb = nbank()
                proj_chunk(xb, g, W1A, wkey, 416, 448, bb)
                TT("dve", T1[0:32, :], PS(ba, 32), TAc[0:32, gs], ALU.mult, [("ps", ba), "TAc"], ["T1"])
                TT("dve", T2[0:32, :], PS(bb, 32), TAs[0:32, gs], ALU.mult, [("ps", bb), "TAs"], ["T2"])
                TT("dve", KR[0:32, gs], T1[0:32, :], T2[0:32, :], ALU.add, ["T1", "T2"], [("KR", g)])
                for tt in range(4):
                    ti = g * 4 + tt
                    b = nbank()
                    MM(PS(b), KVN[:, ti * 128:(ti + 1) * 128], WV, True, True, [("KVN", g), "WV"], [("ps", b)])
                    CP("act", VALL[:, ti, :], PS(b), [("ps", b)], [("VALL", ti)])
            if stop == 1:
                dump("QN", QN.rearrange("p a t -> p (a t)"), [128, 2 * T], BF16, [("QN", j, g) for j in range(2) for g in range(4)])
                dump("KVN", KVN, [128, T], BF16, [("KVN", g) for g in range(4)])
                dump("KR", KR, [32, T], BF16, [("KR", g) for g in range(4)])
                dump("TAc", TAc, [128, T], F32, ["TAc"])
                dump("TAs", TAs, [128, T], F32, ["TAs"])
                dump("VALL", VALL.rearrange("p a t -> p (a t)"), [128, 16 * 512], BF16, [("VALL", ti) for ti in range(16)])
            ckpt(1)

            def attention(b, Kq, scale, parity, chunk, after_g, sbanks=(0, 1, 2)):
                Kq = 128
                NSB = len(sbanks)
                QA, KA, VA = QAb[b], KAb[b], VAb[b]
                steps = [(g, kt) for g in range(4) for kt in range(4 * g + 4)]
                n = len(steps)
                qa_keys = [("QA", b, "lo"), ("QA", b, "hi")]
                ka_keys = [("KA", b, "lo"), ("KA", b, "hi")]

                def qk(i):
                    g, kt = steps[i]
                    j = kt - 4 * g
                    c0 = 128 * max(0, j)
                    sb = sbanks[i % NSB]
                    MM(PS(sb, 128, c0, 512), KA[0:Kq, kt * 128:(kt + 1) * 128], QA[0:Kq, g * 512 + c0:(g + 1) * 512],
                       True, j < 0, qa_keys + ka_keys, [("ps", sb)])
                    if j >= 0:
                        MM(PS(sb, 128, c0, c0 + 128), IDENTb, TRIb, False, True, ["CB"], [("ps", sb)])

                def ex(i):
                    g, kt = steps[i]
                    c0 = 128 * max(0, kt - 4 * g)
                    sb = sbanks[i % NSB]
                    ACTV(PT[i % 4][:, c0:512], PS(sb, 128, c0, 512), AF.Exp, [("ps", sb)], [("PT", i % 4)], scale=scale)

                def pv(i):
                    g, kt = steps[i]
                    c0 = 128 * max(0, kt - 4 * g)
                    ob = 3 + (g % 2)
                    MM(PS(ob, 128, c0, 512), VA[:, kt, :], PT[i % 4][:, c0:512], kt == 0, kt == 4 * g + 3,
                       [("VA", b), ("PT", i % 4)], [("ps", ob)])

                def norm(g):
                    ob = 3 + (g % 2)
                    gs = slice(g * 512, (g + 1) * 512)
                    rc = RC[g % 2]
                    if parity == 0:
                        S.op("dve", lambda e: e.reciprocal(rc[0:64, :], PS(ob)[64:128, :]), [("ps", ob)], [("RC", g % 2)])
                        TT("dve", AT[0:64, chunk, gs], PS(ob)[0:64, :], rc[0:64, :], ALU.mult,
                           [("ps", ob), ("RC", g % 2)], [("AT", chunk, parity, g)])
                    else:
                        S.op("dve", lambda e: e.reciprocal(rc[64:128, :], PS(ob)[0:64, :]), [("ps", ob)], [("RC", g % 2)])
                        TT("dve", AT[64:128, chunk, gs], PS(ob)[64:128, :], rc[64:128, :], ALU.mult,
                           [("ps", ob), ("RC", g % 2)], [("AT", chunk, parity, g)])

                for i0 in range(NSB - 1):
                    qk(i0)
                for i in range(n):
                    ex(i)
                    pv(i)
                    if i + NSB - 1 < n:
                        qk(i + NSB - 1)
                    g, kt = steps[i]
                    if kt == 4 * g + 3:
                        norm(g)
                        after_g(g)
                        zfill(4)
                        bg(6)

            def mla_prep(h, g):
                b = h % 2
                gs = slice(g * 512, (g + 1) * 512)
                hs = slice(h * 96, (h + 1) * 96)
                MM(PS(5, 96), WQ[:, 0, hs], QN[:, 0, gs], True, False, ["WQ", ("QN", 0, g)], [("ps", 5)])
                MM(PS(5, 96), WQ[:, 1, hs], QN[:, 1, gs], False, True, ["WQ", ("QN", 1, g)], [("ps", 5)])
                MM(PS(6, 96), WQS[:, 0, hs], QN[:, 0, gs], True, False, ["WQS", ("QN", 0, g)], [("ps", 6)])
                MM(PS(6, 96), WQS[:, 1, hs], QN[:, 1, gs], False, True, ["WQS", ("QN", 1, g)], [("ps", 6)])
                MM(PS(7, 64), WK[:, h * 64:(h + 1) * 64], KVN[:, gs], True, True, ["WK", ("KVN", g)], [("ps", 7)])
                CP("act", QAb[b][0:64, gs], PS(5, 64), [("ps", 5)], [("QA", b, "lo")])
                TT("dve", T1[64:96, :], PS(5)[64:96, :], TAc[64:96, gs], ALU.mult, [("ps", 5), "TAc"], ["T1"])
                TT("dve", T2[64:96, :], PS(6)[64:96, :], TAs[64:96, gs], ALU.mult, [("ps", 6), "TAs"], ["T2"])
                TT("dve", QAb[b][64:96, gs], T1[64:96, :], T2[64:96, :], ALU.add, ["T1", "T2"], [("QA", b, "hi")])
                CP("act", KAb[b][0:64, gs], PS(7, 64), [("ps", 7)], [("KA", b, "lo")])
                if g == 0:
                    vsl = slice(0, 64) if b == 0 else slice(64, 128)
                    CP("pool", VAb[b][:, :, vsl], VALL[:, :, h * 64:(h + 1) * 64],
                       [("VALL", ti) for ti in range(16)], [("VA", b)])

            for b in range(2):
                osl = slice(64, 128) if b == 0 else slice(0, 64)
                S.op("pool", lambda e, ap=VAb[b][:, :, osl]: e.memset(ap, 1.0), [], [("VA", b)])
                S.op("pool", lambda e, ap=QAb[b][96:128, :]: e.memset(ap, 0.0), [], [("QA", b, "hi")])
                S.op("pool", lambda e, ap=KAb[b][96:128, :]: e.memset(ap, 0.0), [], [("KA", b, "hi")])
                DMA(KAb[b][64:96, :], KR[0:32, :], [("KR", g) for g in range(4)], [("KA", b, "hi")])
            for g in range(4):
                mla_prep(0, g)
            sc_mla = 1.0 / math.sqrt(96.0)
            for h in range(8):
                def after(g, h=h):
                    if h + 1 < 8:
                        mla_prep(h + 1, g)
                attention(h % 2, 96, sc_mla, h % 2, h // 2, after)
            ckpt(2)

            barrier()
            A.reset(base_after_AT)
            TBc = A.alloc([128, T], F32)
            TBs = A.alloc([128, T], F32)
            off1 = A.off
            tmp_tab = [A.alloc([128, T], I32), A.alloc([128, T], F32), A.alloc([128, T], F32), A.alloc([128, T], F32)]
            build_tables(FB, TBc, TBs, "TBc", "TBs", tmp_tab)
            barrier()
            A.reset(off1)
            QM = A.alloc([128, 4, T], BF16)
            KM = A.alloc([128, 4, T], BF16)
            VALLm = A.alloc([128, 16, 512], BF16)
            T1 = A.alloc([128, 512], F32)
            T2 = A.alloc([128, 512], F32)
            MASKT = A.alloc([64, T], BF16)
            KMF = A.alloc([128, 32], F32)
            KMB = A.alloc([128, 32], BF16)
            KMBLK = A.alloc([128, 4, 64], BF16)
            GM = [A.alloc([128, 64], F32) for _ in range(2)]
            MX = [A.alloc([128, 64], F32) for _ in range(2)]
            THR = [A.alloc([128, 8], F32) for _ in range(2)]
            SEL = [A.alloc([128, 64], F32) for _ in range(2)]
            MB = [A.alloc([128, 64], BF16) for _ in range(2)]
            CPM = A.alloc([128, 1024], F32)
            off2 = A.off
            W1B = A.alloc([128, 8, 1792], BF16)
            XG16 = [A.alloc([128, 8, 512], BF16) for _ in range(2)]
            XST = [A.alloc([128, 512], F32) for _ in range(4)]
            WSTb = [A.alloc([128, 1792], F32) for _ in range(2)]

            DMA(CPM, cpm, [], ["CPM"])
            for c in range(8):
                DMA(WSTb[c % 2], w1b[c * 128:(c + 1) * 128, :], [], ["WSTb%d" % (c % 2)])
                cast(W1B[:, c, :], WSTb[c % 2], ["WSTb%d" % (c % 2)], [("W1B", c)])
            for g in range(4):
                gs = slice(g * 512, (g + 1) * 512)
                xb = load_xgroup(g)
                wkey = lambda c: ("W1B", c)
                for (dst, nm, off) in ((QM, "QM", 0), (KM, "KM", 640)):
                    ba = nbank()
                    proj_chunk(xb, g, W1B, wkey, off, off + 128, ba)
                    bb = nbank()
                    proj_chunk(xb, g, W1B, wkey, off + 128, off + 256, bb)
                    TT("dve", T1, PS(ba), TBc[:, gs], ALU.mult, [("ps", ba), "TBc"], ["T1"])
                    TT("dve", T2, PS(bb), TBs[:, gs], ALU.mult, [("ps", bb), "TBs"], ["T2"])
                    TT("pool", dst[:, 0, gs], T1, T2, ALU.add, ["T1", "T2"], [(nm, g)])
                    for j in range(1, 4):
                        b = nbank()
                        proj_chunk(xb, g, W1B, wkey, off + 128 + 128 * j, off + 256 + 128 * j, b)
                        CP("act", dst[:, j, gs], PS(b), [("ps", b)], [(nm, g)])
                for tt in range(4):
                    ti = g * 4 + tt
                    b = nbank()
                    for c in range(8):
                        MM(PS(b), xb[:, c, tt * 128:(tt + 1) * 128], W1B[:, c, 1280:1792], c == 0, c == 7,
                           [("W1B", c), ("XG16", g % 2, c)], [("ps", b)])
                    CP("act", VALLm[:, ti, :], PS(b), [("ps", b)], [("VALLm", ti)])

            kmk = [("KM", g) for g in range(4)]
            S.op("dve", lambda e: e.tensor_reduce(KMF.rearrange("p (a b) -> p a b", a=4),
                                                  KM.rearrange("p a (n k) -> p a n k", n=8), AX.X, ALU.add),
                 kmk, ["KMF"])
            TS("dve", KMB, KMF, 1.0 / 256.0, ALU.mult, ["KMF"], ["KMB"])
            KMBv = KMB.rearrange("p (a n) -> p a n", a=4)
            for hh in range(8):
                TS("dve", KMBLK[:, :, hh * 8:(hh + 1) * 8], KMBv, HEADM[:, hh:hh + 1], ALU.mult, ["KMB", "CF"], ["KMBLK"])
            ckpt(3)

            barrier()
            A.reset(off2)
            QAb = [A.alloc([128, T], BF16) for _ in range(2)]
            KAb = [A.alloc([128, T], BF16) for _ in range(2)]
            VAb = [A.alloc([128, 16, 128], BF16) for _ in range(2)]
            PT = [A.alloc([128, 512], BF16) for _ in range(4)]
            RC = [A.alloc([128, 512], F32) for _ in range(2)]
            GMt = [A.alloc([128, 64], F32) for _ in range(16)]
            MXt = [A.alloc([128, 64], F32) for _ in range(16)]
            THRt = [A.alloc([128, 8], F32) for _ in range(16)]
            SELt = [A.alloc([128, 64], F32) for _ in range(16)]
            MBt = [A.alloc([128, 64], BF16) for _ in range(16)]

            gb = [nbank(), nbank()]
            for ti in range(16):
                bk = gb[ti // 8]
                c0 = (ti % 8) * 64
                for j in range(4):
                    MM(PS(bk, 128, c0, c0 + 64), QM[:, j, ti * 128:(ti + 1) * 128], KMBLK[:, j, :], j == 0, j == 3,
                       [("QM", ti // 4), "KMBLK"], [("ps", bk)])
            for ti in range(16):
                cur = ti // 2
                bk = gb[ti // 8]
                c0 = (ti % 8) * 64
                TT("dve", GMt[ti], PS(bk, 128, c0, c0 + 64), CPM[:, cur * 64:(cur + 1) * 64], ALU.add, [("ps", bk), "CPM"], [("GM", ti)])
            for ti in range(16):
                for hh in range(8):
                    S.op("dve", lambda e, o=MXt[ti][:, hh * 8:(hh + 1) * 8], i=GMt[ti][:, hh * 8:(hh + 1) * 8]: e.max(o, i),
                         [("GM", ti)], [("MX", ti, hh)])
            for ti in range(16):
                MXv = MXt[ti].rearrange("p (h n) -> p h n", h=8)
                TS("dve", THRt[ti], MXv[:, :, 2], -1e29, ALU.max, [("MX", ti, hh) for hh in range(8)], [("THR", ti)])
            for ti in range(16):
                for hh in range(8):
                    TS("dve", SELt[ti][:, hh * 8:(hh + 1) * 8], GMt[ti][:, hh * 8:(hh + 1) * 8], THRt[ti][:, hh:hh + 1], ALU.is_ge,
                       [("GM", ti), ("THR", ti)], [("SEL", ti, hh)])
            for ti in range(16):
                cur = ti // 2
                TT("dve", SELt[ti], SELt[ti], CPM[:, 512 + cur * 64:512 + (cur + 1) * 64], ALU.add,
                   [("SEL", ti, hh) for hh in range(8)] + ["CPM"], [("SEL", ti)])
            for ti in range(16):
                TS("dve", MBt[ti], SELt[ti], -1.0, ALU.add, [("SEL", ti)], [("MB", ti)], s2=-NEG, op1=ALU.mult)
            tb = [nbank(), nbank()]
            for ti in range(16):
                bk = tb[ti // 8]
                c0 = (ti % 8) * 64
                pst = PSUM[0:64, bk * 512 + c0:bk * 512 + c0 + 64].bitcast(BF16)
                TR(pst, MBt[ti], IDENTb, [("MB", ti), "CB"], [("ps", bk)])
            for hb in range(2):
                bk = tb[hb]
                pall = PSUM[0:64, bk * 512:(bk + 1) * 512].bitcast(BF16)
                CP("dve", MASKT[:, hb * 1024:(hb + 1) * 1024], pall, [("ps", bk)], [("MASKT", hb * 8 + q) for q in range(8)])

            def moba_prep(h):
                b = h % 2
                for j in range(4):
                    DMA(QAb[b][16 * j:16 * j + 16, :], QM[16 * h:16 * h + 16, j, :], [("QM", g) for g in range(4)], [("QA", b, "lo")])
                    DMA(KAb[b][16 * j:16 * j + 16, :], KM[16 * h:16 * h + 16, j, :], kmk, [("KA", b, "lo")])
                DMA(QAb[b][64:72, :], MASKT[8 * h:8 * h + 8, :], [("MASKT", ti) for ti in range(16)], [("QA", b, "hi")])
                vsl = slice(0, 64) if b == 0 else slice(64, 128)
                CP("pool", VAb[b][:, :, vsl], VALLm[:, :, h * 64:(h + 1) * 64], [("VALLm", ti) for ti in range(16)], [("VA", b)])

            for b in range(2):
                osl = slice(64, 128) if b == 0 else slice(0, 64)
                S.op("pool", lambda e, ap=VAb[b][:, :, osl]: e.memset(ap, 1.0), [], [("VA", b)])
                S.op("pool", lambda e, ap=QAb[b][64:128, :]: e.memset(ap, 0.0), [], [("QA", b, "hi")])
                S.op("pool", lambda e, ap=KAb[b][64:128, :]: e.memset(ap, 0.0), [], [("KA", b, "hi")])
                DMA(KAb[b][64:72, :], cind, [], [("KA", b, "hi")])
            moba_prep(0)
            for h in range(8):
                def after(g, h=h):
                    if g == 0 and h + 1 < 8:
                        moba_prep(h + 1)
                attention(h % 2, 72, 0.125, h % 2, 4 + h // 2, after, sbanks=(0, 1, 2, 5, 6, 7))
            zfill(1000)
            bg(100000)
            ckpt(4)

            if debug:
                DMA(dbg["AT"], AT.rearrange("p a t -> p (a t)"),
                    [("AT", c, p, g) for c in range(8) for p in range(2) for g in range(4)], [])
            barrier()
            A.limit = ARN
            A.reset(base_after_AT)
            WO = A.alloc([128, 8, D], BF16)
            WOST = [A.alloc([128, D], F32) for _ in range(2)]
            G1 = A.alloc([128, D], F32)
            B1 = A.alloc([128, D], F32)
            WRf = A.alloc([128, 8, NE], F32)
            XB = [A.alloc([128, D], F32) for _ in range(8)]
            XBb = [A.alloc([128, D], BF16) for _ in range(8)]
            H1T = [A.alloc([128, 8, 128], F32) for _ in range(4)]
            MASKS = A.alloc([128, 16, NE], BF16)
            ST5 = [A.alloc([128, 12], F32) for _ in range(8)]
            MV5 = [A.alloc([128, 2], F32) for _ in range(8)]
            RS5 = [A.alloc([128, 4], F32) for _ in range(8)]
            LGg = [A.alloc([128, 4, NE], F32) for _ in range(2)]
            POSg = [A.alloc([128, 4, NE], F32) for _ in range(2)]
            MX8 = [A.alloc([128, 8], F32) for _ in range(8)]
            IX8 = [A.alloc([128, 8], U32) for _ in range(8)]
            IXF = [A.alloc([128, 4], F32) for _ in range(8)]
            NMX = [A.alloc([128, 2], F32) for _ in range(8)]
            EXP4 = [A.alloc([128, 4], F32) for _ in range(8)]
            SUM4 = [A.alloc([128, 2], F32) for _ in range(8)]
            OH = [[A.alloc([128, NE], F32) for _ in range(4)] for _ in range(8)]
            PK = [A.alloc([128, 4], F32) for _ in range(8)]
            PK2 = [A.alloc([128, 4], F32) for _ in range(8)]
            DF = [A.alloc([128, 4], F32) for _ in range(8)]
            OV = [A.alloc([128, 4], F32) for _ in range(8)]

            for c in range(8):
                DMA(WOST[c % 2], wo[c * 128:(c + 1) * 128, :], [], [("WOST", c % 2)])
                cast(WO[:, c, :], WOST[c % 2], [("WOST", c % 2)], [("WO", c)])
            DMA(G1, ln1g.partition_broadcast(128), [], ["G1"])
            DMA(B1, ln1b.partition_broadcast(128), [], ["B1"])
            DMA(WRf, wr.rearrange("(c p) e -> p c e", p=128), [], ["WRf"])

            def layer_norm(zt, zkey, out, okey, st, mv, rs, Gb, Bb, p, eps_ap, gb_eng):
                for hf in range(2):
                    S.op("dve", lambda e, hf=hf: e.bn_stats(st[:, hf * 6:(hf + 1) * 6], zt[:, hf * 512:(hf + 1) * 512]),
                         [zkey], [("ST", p)])
                    yield
                S.op("dve", lambda e: e.bn_aggr(mv, st), [("ST", p)], [("MV", p)])
                yield
                ACTV(rs[:, 0:1], mv[:, 1:2], AF.Sqrt, [("MV", p), "CF"], [("RS", p)], bias=eps_ap, scale=1.0)
                yield
                S.op("dve", lambda e: e.reciprocal(rs[:, 1:2], rs[:, 0:1]), [("RS", p)], [("RS", p)])
                yield
                TS("dve", rs[:, 2:3], mv[:, 0:1], rs[:, 1:2], ALU.mult, [("MV", p), ("RS", p)], [("RS", p)], s2=-1.0, op1=ALU.mult)
                yield
                ACTV(out, zt, AF.Identity, [zkey, ("RS", p)], [okey], bias=rs[:, 2:3], scale=rs[:, 1:2])
                yield
                TT(gb_eng, out, out, Gb, ALU.mult, [okey, "G1", "G2"], [okey])
                yield
                TT(gb_eng, out, out, Bb, ALU.add, [okey, "B1", "B2"], [okey])
                yield

            atkeys = [("AT", c, p, g) for c in range(8) for p in range(2) for g in range(4)]
            def pairbank(T_):
                return 2 * (T_ % 3)

            def stageA(g):
                b = g % 2
                tiles = [(t, g * 4 + t, b * 4 + t) for t in range(4)]
                for t, T_, sl in tiles:
                    DMA(XB[sl], xtok[T_ * 128:(T_ + 1) * 128, :], [], [("XB", sl)])
                for sub in (tiles[0:3], tiles[3:4]):
                    for t, T_, sl in sub:
                        pb = pairbank(T_)
                        ts_ = slice(T_ * 128, (T_ + 1) * 128)
                        for hf in range(2):
                            for c in range(8):
                                MM(PS(pb + hf), AT[:, c, ts_], WO[:, c, hf * 512:(hf + 1) * 512], c == 0, c == 7,
                                   [("WO", c)], [("ps", pb + hf)])
                    for t, T_, sl in sub:
                        pb = pairbank(T_)
                        for hf in range(2):
                            hs = slice(hf * 512, (hf + 1) * 512)
                            STT(XB[sl][:, hs], XB[sl][:, hs], DN_ALPHA, PS(pb + hf), ALU.mult, ALU.add,
                                [("XB", sl), ("ps", pb + hf)], [("XB", sl)])
                for t, T_, sl in tiles:
                    for hf in range(2):
                        S.op("dve", lambda e, st=ST5[sl], z=XB[sl], hf=hf: e.bn_stats(st[:, hf * 6:(hf + 1) * 6], z[:, hf * 512:(hf + 1) * 512]),
                             [("XB", sl)], [("ST", sl)])
                for t, T_, sl in tiles:
                    S.op("dve", lambda e, mv=MV5[sl], st=ST5[sl]: e.bn_aggr(mv, st), [("ST", sl)], [("MV", sl)])
                for t, T_, sl in tiles:
                    ACTV(RS5[sl][:, 0:1], MV5[sl][:, 1:2], AF.Sqrt, [("MV", sl), "CF"], [("RS", sl)], bias=EPS5, scale=1.0)
                for t, T_, sl in tiles:
                    S.op("dve", lambda e, rs=RS5[sl]: e.reciprocal(rs[:, 1:2], rs[:, 0:1]), [("RS", sl)], [("RS", sl)])
                for t, T_, sl in tiles:
                    TS("dve", RS5[sl][:, 2:3], MV5[sl][:, 0:1], RS5[sl][:, 1:2], ALU.mult, [("MV", sl), ("RS", sl)], [("RS", sl)],
                       s2=-1.0, op1=ALU.mult)

            def stageA1b(g):
                b = g % 2
                tiles = [(t, g * 4 + t, b * 4 + t) for t in range(4)]
                for t, T_, sl in tiles:
                    ACTV(XB[sl], XB[sl], AF.Identity, [("XB", sl), ("RS", sl)], [("XB", sl)], bias=RS5[sl][:, 2:3], scale=RS5[sl][:, 1:2])
                for t, T_, sl in tiles:
                    TT("dve", XB[sl], XB[sl], G1, ALU.mult, [("XB", sl), "G1"], [("XB", sl)])
                for t, T_, sl in tiles:
                    TT("dve", XB[sl], XB[sl], B1, ALU.add, [("XB", sl), "B1"], [("XB", sl)])

            def stageA2(g):
                b = g % 2
                tiles = [(t, g * 4 + t, b * 4 + t) for t in range(4)]
                for t, T_, sl in tiles:
                    DMA(H1[T_ * 128:(T_ + 1) * 128, :], XB[sl], [("XB", sl)], [("H1", T_)])
                for t, T_, sl in tiles:
                    CP("act", XBb[sl], XB[sl], [("XB", sl)], [("XBb", sl)])
                for t, T_, sl in tiles:
                    pb = pairbank(T_)
                    for hf in range(2):
                        for cc in range(4):
                            c = hf * 4 + cc
                            TR(PS(pb + hf, 128, cc * 128, (cc + 1) * 128), XB[sl][:, c * 128:(c + 1) * 128], IDENTf,
                               [("XB", sl), "CF"], [("ps", pb + hf)])
                        CP("act", H1T[t][:, hf * 4:(hf + 1) * 4, :], PS(pb + hf).rearrange("p (a b) -> p a b", a=4),
                           [("ps", pb + hf)], [("H1T", t, hf)])
                for t, T_, sl in tiles:
                    for c in range(8):
                        MM(PS(6, 128, t * NE, (t + 1) * NE), H1T[t][:, c, :], WRf[:, c, :], c == 0, False,
                           [("H1T", t, 0), ("H1T", t, 1), "WRf"], [("ps", 6)])
                    MM(PS(6, 128, t * NE, (t + 1) * NE), ONESF[0:1, :], BR[0:1, :], False, True, ["ONESF", "BR"], [("ps", 6)])
                CP("dve", LGg[b].rearrange("p a e -> p (a e)"), PS(6, 128, 0, 4 * NE), [("ps", 6)], [("LGg", b)])

            def stageB(g):
                b = g % 2
                tiles = [(t, g * 4 + t, b * 4 + t) for t in range(4)]
                for t, T_, sl in tiles:
                    S.op("dve", lambda e, o=MX8[sl], i=LGg[b][:, t, :]: e.max(o, i), [("LGg", b)], [("MX8", sl)])
                for t, T_, sl in tiles:
                    S.op("dve", lambda e, o=IX8[sl], m=MX8[sl], i=LGg[b][:, t, :]: e.max_index(o, m, i),
                         [("LGg", b), ("MX8", sl)], [("IX8", sl)])
                for t, T_, sl in tiles:
                    TS("dve", NMX[sl][:, 0:1], MX8[sl][:, 0:1], -1.0, ALU.mult, [("MX8", sl)], [("NMX", sl)])
                for t, T_, sl in tiles:
                    ACTV(EXP4[sl], MX8[sl][:, 0:4], AF.Exp, [("MX8", sl), ("NMX", sl)], [("EXP4", sl)], bias=NMX[sl][:, 0:1], scale=1.0)
                for t, T_, sl in tiles:
                    S.op("dve", lambda e, o=SUM4[sl], i=EXP4[sl]: e.tensor_reduce(o[:, 0:1], i, AX.X, ALU.add), [("EXP4", sl)], [("SUM4", sl)])
                for t, T_, sl in tiles:
                    S.op("dve", lambda e, o=SUM4[sl]: e.reciprocal(o[:, 1:2], o[:, 0:1]), [("SUM4", sl)], [("SUM4", sl)])
                for t, T_, sl in tiles:
                    TS("dve", GATE[:, T_ * 4:(T_ + 1) * 4], EXP4[sl], SUM4[sl][:, 1:2], ALU.mult, [("EXP4", sl), ("SUM4", sl)], [("GATE", T_)])
                for t, T_, sl in tiles:
                    TS("dve", MASKS[:, T_, :], LGg[b][:, t, :], MX8[sl][:, 3:4], ALU.is_ge, [("LGg", b), ("MX8", sl)], [("MASKS", T_)])
                for t, T_, sl in tiles:
                    MM(PS(7, 128, t * NE, (t + 1) * NE), TRISb, MASKS[:, T_, :], True, T_ == 0, ["CB", ("MASKS", T_)], [("ps", 7)])
                    for tj in range(T_):
                        MM(PS(7, 128, t * NE, (t + 1) * NE), ONESb, MASKS[:, tj, :], False, tj == T_ - 1, ["CBo", ("MASKS", tj)], [("ps", 7)])
                CP("dve", POSg[b].rearrange("p a e -> p (a e)"), PS(7, 128, 0, 4 * NE), [("ps", 7)], [("POSg", b)])
                for t, T_, sl in tiles:
                    CP("dve", IXF[sl], IX8[sl][:, 0:4], [("IX8", sl)], [("IXF", sl)])
                for k in range(4):
                    for t, T_, sl in tiles:
                        TS("dve", OH[sl][k], IOTA32, IXF[sl][:, k:k + 1], ALU.is_equal, ["CF", ("IXF", sl)], [("OH", sl, k)])
                for k in range(4):
                    for t, T_, sl in tiles:
                        TT("dve", OH[sl][k], OH[sl][k], POSg[b][:, t, :], ALU.mult, [("OH", sl, k), ("POSg", b)], [("OH", sl, k)])
                for k in range(4):
                    for t, T_, sl in tiles:
                        S.op("dve", lambda e, o=PK[sl], i=OH[sl][k], k=k: e.tensor_reduce(o[:, k:k + 1], i, AX.X, ALU.add),
                             [("OH", sl, k)], [("PK", sl, k)])
                for t, T_, sl in tiles:
                    STT(DF[sl], IXF[sl], float(CAP), PK[sl], ALU.mult, ALU.add, [("IXF", sl)] + [("PK", sl, k) for k in range(4)], [("DF", sl)])
                for t, T_, sl in tiles:
                    TS("dve", OV[sl], PK[sl], float(CAP), ALU.is_ge, [("PK", sl, k) for k in range(4)], [("OV", sl)])
                for t, T_, sl in tiles:
                    TS("dve", PK2[sl], DF[sl], -1.0, ALU.mult, [("DF", sl)], [("PK2", sl)], s2=PCOL, op1=ALU.add)
                for t, T_, sl in tiles:
                    TT("dve", PK2[sl], PK2[sl], OV[sl], ALU.mult, [("PK2", sl), ("OV", sl)], [("PK2", sl)])
                for t, T_, sl in tiles:
                    TT("dve", DF[sl], DF[sl], PK2[sl], ALU.add, [("DF", sl), ("PK2", sl)], [("DF", sl)])
                for t, T_, sl in tiles:
                    CP("dve", DEST[:, T_ * 4:(T_ + 1) * 4], DF[sl], [("DF", sl)], [("DEST", T_)])
                for t, T_, sl in tiles:
                    for k in range(4):
                        S.op("pool", lambda e, src=XBb[sl], T_=T_, k=k: e.indirect_dma_start(
                            out=XG, out_offset=bass.IndirectOffsetOnAxis(DEST[:, T_ * 4 + k:T_ * 4 + k + 1], 0),
                            in_=src, in_offset=None),
                            [("XBb", sl), ("DEST", T_)], [("XGs", T_, k)], dma=True)

            stageA(0)
            stageA1b(0)
            stageA(1)
            stageA2(0)
            stageA1b(1)
            stageB(0)
            stageA(2)
            stageA2(1)
            stageA1b(2)
            stageB(1)
            stageA(3)
            stageA2(2)
            stageA1b(3)
            stageB(2)
            stageA2(3)
            stageB(3)
            if debug:
                DMA(dbg["dest"], DEST, [("DEST", ti) for ti in range(16)], [])
                DMA(dbg["gate"], GATE, [("GATE", ti) for ti in range(16)], [])
            ckpt(5)

            barrier()
            A.reset(0)
            WGb = [A.alloc([128, 8, D], BF16) for _ in range(2)]
            WUb = [A.alloc([128, 8, D], BF16) for _ in range(2)]
            WDb = A.alloc([128, 8, D], BF16)
            NST = 8
            WST6 = [A.alloc([128, D], F32) for _ in range(NST)]
            XS = [A.alloc([128, D], BF16) for _ in range(NBLK)]
            XTE = [A.alloc([128, 8, CAP], BF16) for _ in range(2)]
            HTE = [A.alloc([128, 8, CAP], BF16) for _ in range(2)]
            SG = [A.alloc([128, CAP], F32) for _ in range(2)]
            GG = [A.alloc([128, CAP], F32) for _ in range(2)]
            UU = [A.alloc([128, CAP], F32) for _ in range(2)]
            TTm = [A.alloc([128, CAP], F32) for _ in range(2)]
            YS = [A.alloc([128, D], F32) for _ in range(NBLK)]
            BDB = [A.alloc([128, D], F32) for _ in range(2)]

            wcnt = [0]
            import os as _os
            W_CAST = ["act", "dve", "act"]
            if _os.environ.get("KDBG_WCAST"):
                W_CAST = _os.environ["KDBG_WCAST"].split(",")

            def _load(src_ap, dst_ap, key):
                i = wcnt[0] % NST
                eng = W_CAST[wcnt[0] % len(W_CAST)]
                wcnt[0] += 1
                DMA(WST6[i], src_ap, [], [("WST6", i)])
                CP(eng, dst_ap, WST6[i], [("WST6", i)], [key])

            def load_gu(e, j):
                c = j // 2
                if e < NPRE:
                    idx = e * 24 + j
                    dst, key = (WGb[e % 2][:, c, :], ("WG", e % 2, c)) if j % 2 == 0 else (WUb[e % 2][:, c, :], ("WU", e % 2, c))
                    DMA(dst, WB[idx * 128:(idx + 1) * 128, :], [], [key])
                    return
                if j % 2 == 0:
                    _load(wg[e, c * 128:(c + 1) * 128, :], WGb[e % 2][:, c, :], ("WG", e % 2, c))
                else:
                    _load(wu[e, c * 128:(c + 1) * 128, :], WUb[e % 2][:, c, :], ("WU", e % 2, c))

            def load_d(e, f):
                if e < NPRE:
                    idx = e * 24 + 16 + f
                    DMA(WDb[:, f, :], WB[idx * 128:(idx + 1) * 128, :], [], [("WD", f)])
                    return
                _load(wd[e, f * 128:(f + 1) * 128, :], WDb[:, f, :], ("WD", f))

            def load_xs(e):
                rd = [("XGs", ti, k) for ti in range(16) for k in range(4)] if e == 0 else []
                for blk in range(NBLK):
                    r0 = e * CAP + blk * 128
                    import os as _os
                    if _os.environ.get("KDBG_XSZERO"):
                        S.op("pool", lambda e, ap=XS[blk]: e.memset(ap, 0.5), [], [("XS", blk)])
                    else:
                        DMA(XS[blk], XG[r0:r0 + 128, :], rd, [("XS", blk)])
                DMA(BDB[e % 2], bd[e, :].partition_broadcast(128), [], [("BDB", e % 2)])

            def expert(e):
                eb = e % 2
                xte_keys = [("XTE", eb, c) for c in range(8)]
                for c in range(8):
                    pb = 6 + (c % 2)
                    pst = PSUM[:, pb * 512:pb * 512 + CAP // 2].bitcast(BF16)
                    for blk in range(NBLK):
                        TR(pst[:, blk * 128:(blk + 1) * 128], XS[blk][:, c * 128:(c + 1) * 128], IDENTb,
                           [("XS", blk), "CB"], [("ps", pb)])
                    CP("act", XTE[eb][:, c, :], pst, [("ps", pb)], [("XTE", eb, c)])
                import os as _os
                _sub = int(_os.environ.get("KDBG_SUB", 9))
                if _sub < 1:
                    return
                if e + 1 < NE:
                    load_xs(e + 1)
                if _sub < 2:
                    return
                for f in range(8):
                    pg = (f % 2) * 2
                    pu = pg + 1
                    fs = slice(f * 128, (f + 1) * 128)
                    for c in range(8):
                        MM(PS(pg, 128, 0, CAP), WGb[eb][:, c, fs], XTE[eb][:, c, :], c == 0, c == 7,
                           [("WG", eb, c)] + xte_keys, [("ps", pg)])
                    for c in range(8):
                        MM(PS(pu, 128, 0, CAP), WUb[eb][:, c, fs], XTE[eb][:, c, :], c == 0, c == 7,
                           [("WU", eb, c)] + xte_keys, [("ps", pu)])
                    q = f % 2
                    bcol = slice(e * 8 + f, e * 8 + f + 1)
                    if _os.environ.get("KDBG_NOSW"):
                        continue
                    TS("dve", GG[q], PS(pg, 128, 0, CAP), BG[:, bcol], ALU.add, [("ps", pg), "BG"], [("GG", q)], s2=7.0, op1=ALU.min)
                    ACTV(SG[q], GG[q], AF.Sigmoid, [("GG", q)], [("SG", q)], scale=1.702)
                    TS("dve", UU[q], PS(pu, 128, 0, CAP), BU1[:, bcol], ALU.add, [("ps", pu), "BU1"], [("UU", q)], s2=8.0, op1=ALU.min)
                    TT("dve", TTm[q], SG[q], GG[q], ALU.mult, [("SG", q), ("GG", q)], [("TTm", q)])
                    STT(HTE[eb][:, f, :], UU[q], -6.0, TTm[q], ALU.max, ALU.mult, [("UU", q), ("TTm", q)], [("HTE", eb, f)])
                    load_d(e, f)
                    if e + 1 < NE:
                        load_gu(e + 1, f)
                if _sub < 3:
                    return
                for blk in range(NBLK):
                    for hf in range(2):
                        pb = 4 + hf
                        for f in range(8):
                            MM(PS(pb), HTE[eb][:, f, blk * 128:(blk + 1) * 128], WDb[:, f, hf * 512:(hf + 1) * 512],
                               f == 0, f == 7, [("HTE", eb, f), ("WD", f)], [("ps", pb)])
                        TT("dve", YS[blk][:, hf * 512:(hf + 1) * 512], PS(pb), BDB[eb][:, hf * 512:(hf + 1) * 512], ALU.add,
                           [("ps", pb), ("BDB", eb)], [("YS", blk)])
                        if e + 1 < NE:
                            gi = blk * 2 + hf
                            for j in DOWN_SPREAD[gi]:
                                load_gu(e + 1, j)
                for blk in range(NBLK):
                    r0 = e * CAP + blk * 128
                    DMA(YG[r0:r0 + 128, :], YS[blk], [("YS", blk)], [("YG", e, blk)])

            S.op("pool", lambda e: e.memset(YS[0], 0.0), [], [("YS", 0)])
            DMA(YG[NSLOT:NSLOT + 128, :], YS[0], [("YS", 0)], [("YGtrash",)])
            DOWN_SPREAD = [[8, 9], [10], [11], [12, 13], [14], [15]]
            load_xs(0)
            for j in range(16):
                load_gu(0, j)
            import os as _os
            for e in range(int(_os.environ.get("KDBG_NE", NE))):
                expert(e)
            ckpt(6)

            barrier()
            A.reset(0)
            G2 = A.alloc([128, D], F32)
            B2 = A.alloc([128, D], F32)
            NB7 = 3
            YK = [[A.alloc([128, D], F32) for _ in range(4)] for _ in range(NB7)]
            HH = [A.alloc([128, D], F32) for _ in range(NB7)]
            ACC = [A.alloc([128, D], F32) for _ in range(NB7)]
            OUT = [A.alloc([128, D], F32) for _ in range(NB7)]
            ST7 = [A.alloc([128, 12], F32) for _ in range(NB7)]
            MV7 = [A.alloc([128, 2], F32) for _ in range(NB7)]
            RS7 = [A.alloc([128, 4], F32) for _ in range(NB7)]
            DMA(G2, ln2g.partition_broadcast(128), [], ["G2"])
            DMA(B2, ln2b.partition_broadcast(128), [], ["B2"])
            ygk = [("YG", e, blk) for e in range(NE) for blk in range(NBLK)]

            def fetch7(ti):
                p = ti % NB7
                ts_ = slice(ti * 128, (ti + 1) * 128)
                DMA(HH[p], H1[ts_, :], [("H1", ti)], [("HH", p)])
                for k in range(4):
                    S.op("pool", lambda e, p=p, ti=ti, k=k: e.indirect_dma_start(
                        out=YK[p][k], out_offset=None, in_=YG,
                        in_offset=bass.IndirectOffsetOnAxis(DEST[:, ti * 4 + k:ti * 4 + k + 1], 0)),
                        [("DEST", ti)] + (ygk if (ti == 0 and k == 0) else []), [("YK", p, k)], dma=True)

            def tile7(ti):
                p = ti % NB7
                ts_ = slice(ti * 128, (ti + 1) * 128)
                if ti + 2 < 16:
                    fetch7(ti + 2)
                yield
                for k in (0, 2):
                    S.op("act", lambda e, ap=YK[p][k], g=GATE[:, ti * 4 + k:ti * 4 + k + 1]: e.mul(ap, ap, g),
                         [("YK", p, k), ("GATE", ti)], [("YK", p, k)])
                    yield
                for k in (1, 3):
                    STT(YK[p][k], YK[p][k], GATE[:, ti * 4 + k:ti * 4 + k + 1], YK[p][k - 1], ALU.mult, ALU.add,
                        [("YK", p, k), ("YK", p, k - 1), ("GATE", ti)], [("YK", p, k)])
                    yield
                TT("pool", ACC[p], YK[p][1], YK[p][3], ALU.add, [("YK", p, 1), ("YK", p, 3)], [("ACC", p)])
                yield
                STT(ACC[p], HH[p], DN_ALPHA, ACC[p], ALU.mult, ALU.add, [("HH", p), ("ACC", p)], [("ACC", p)])
                yield
                yield from layer_norm(ACC[p], ("ACC", p), OUT[p], ("OUT", p), ST7[p], MV7[p], RS7[p], G2, B2, p, EPS5, "dve")
                DMA(y[ts_, :], OUT[p], [("OUT", p)], [("y", ti)])
                yield

            fetch7(0)
            fetch7(1)
            gens7 = []
            nxt7 = [0]
            st7 = {}
            LAG7 = 8
            while nxt7[0] < 16 or gens7:
                if nxt7[0] < 16 and len(gens7) < 2 and (not gens7 or st7[gens7[0][0]] >= LAG7):
                    gens7.append((nxt7[0], tile7(nxt7[0])))
                    st7[nxt7[0]] = 0
                    nxt7[0] += 1
                for item in list(gens7):
                    tid, gen = item
                    try:
                        next(gen)
                        st7[tid] += 1
                    except StopIteration:
                        gens7.remove(item)

        except _Stop:
            pass
        run_block(nc, S, sems)
    return nc


def _consts():
    bf = ml_dtypes.bfloat16
    ident = np.eye(128, dtype=np.float32)
    k = np.arange(128)[:, None]
    q = np.arange(128)[None, :]
    tri = np.where(k <= q, 0.0, NEG).astype(np.float32)
    tris = (k < q).astype(np.float32)
    cbf = np.concatenate([ident, tri, tris], axis=1).astype(bf)
    cf = np.zeros((128, 176), np.float32)
    cf[:, 0:128] = ident
    cf[:, 128:160] = np.arange(32, dtype=np.float32)[None, :]
    cf[:, 160:168] = (np.arange(128)[:, None] // 16 == np.arange(8)[None, :]).astype(np.float32)
    inv_a = np.power(np.float32(500000.0), -np.arange(0, 32, 2, dtype=np.float32) / np.float32(32)).astype(np.float32)
    inv_b = np.power(np.float32(500000.0), -np.arange(0, 16, 2, dtype=np.float32) / np.float32(16)).astype(np.float32)
    fa = np.zeros((128, 2), np.float32)
    for base in (0, 64):
        fa[base:base + 16, 0] = inv_a
        fa[base + 16:base + 32, 0] = inv_a
        fa[base:base + 16, 1] = -1.0
        fa[base + 16:base + 32, 1] = 1.0
    fb = np.zeros((128, 2), np.float32)
    for h in range(8):
        fb[16 * h:16 * h + 8, 0] = inv_b
        fb[16 * h + 8:16 * h + 16, 0] = inv_b
        fb[16 * h:16 * h + 8, 1] = -1.0
        fb[16 * h + 8:16 * h + 16, 1] = 1.0
    cf[:, 168:170] = fa
    cf[:, 170:172] = fb
    cf[:, 172] = np.float32(math.pi / 2)
    cf[:, 173] = 1e-6
    cf[:, 174] = 1e-5
    cf[:, 175] = NSLOT + np.arange(128, dtype=np.float32)
    pm = np.zeros((8, 8, 8), np.float32)
    own = np.zeros((8, 8, 8), np.float32)
    for cur in range(8):
        pm[cur, :, cur:] = -1e30
        own[cur, :, cur] = 1.0
    cpm = np.concatenate([pm.reshape(1, 512), own.reshape(1, 512)], axis=1)
    cpm = np.ascontiguousarray(np.broadcast_to(cpm, (128, 1024))).astype(np.float32)
    ind = (np.arange(T)[None, :] // 256 == np.arange(8)[:, None]).astype(np.float32).astype(bf)
    return cbf, cf, cpm, ind


def _prep_shared(inp):
    f = lambda a: np.ascontiguousarray(np.asarray(a, dtype=np.float32))
    w_in = f(inp["w_in"])[0]
    kr = w_in[:, 384:416]
    kr_sw = np.concatenate([kr[:, 16:32], kr[:, 0:16]], axis=1)
    w1a = np.concatenate([w_in[:, 0:384], kr, kr_sw], axis=1)

    def moba_cols(wm):
        wh = wm.reshape(D, 8, 64)
        c0 = wh[:, :, 0:16].reshape(D, 128)
        c0s = np.concatenate([wh[:, :, 8:16], wh[:, :, 0:8]], axis=2).reshape(D, 128)
        rest = [wh[:, :, 16 + 16 * j:32 + 16 * j].reshape(D, 128) for j in range(3)]
        return [c0, c0s] + rest

    w1b = np.concatenate(moba_cols(w_in[:, 416:928]) + moba_cols(w_in[:, 928:1440]) + [w_in[:, 1440:1952]], axis=1)
    wq = f(inp["w_q_b"])[0]
    wqh = wq.reshape(256, 8, 96)
    wqs = np.concatenate([wqh[:, :, 0:64], wqh[:, :, 80:96], wqh[:, :, 64:80]], axis=2).reshape(256, 768)
    wkv = f(inp["w_kv_b"])[0].reshape(128, 8, 128)
    wk = wkv[:, :, 0:64].reshape(128, 512)
    wv = wkv[:, :, 64:128].reshape(128, 512)
    cbf, cf, cpm, ind = _consts()
    sh = {
        "w1a": w1a, "w1b": w1b, "wq": wq, "wqs": wqs,
        "qg": f(inp["q_a_norm"])[0].reshape(2, 128).T, "wk": wk, "wv": wv,
        "kvg": f(inp["kv_a_norm"])[0].reshape(128, 1),
        "wo": f(inp["w_o"])[0], "ln1g": f(inp["ln1_g"])[0], "ln1b": f(inp["ln1_b"])[0],
        "wr": f(inp["w_router"])[0], "br": f(inp["b_router"])[0].reshape(1, NE),
        "wg": f(inp["w_gate"])[0], "wu": f(inp["w_up"])[0], "wd": f(inp["w_down"])[0],
        "bg": f(inp["b_gate"])[0].reshape(NE, 8, 128).transpose(2, 0, 1).reshape(128, NE * 8),
        "bu": f(inp["b_up"])[0].reshape(NE, 8, 128).transpose(2, 0, 1).reshape(128, NE * 8),
        "bd": f(inp["b_down"])[0], "ln2g": f(inp["ln2_g"])[0], "ln2b": f(inp["ln2_b"])[0],
        "cbf": cbf, "cf": cf, "cpm": cpm, "cind": ind,
    }
    return {k: np.ascontiguousarray(v) for k, v in sh.items()}


def make_in_maps(inp, n_cores=8):
    sh = _prep_shared(inp)
    x = np.asarray(inp["x"], dtype=np.float32)
    posn = np.asarray(inp["positions"]).astype(np.int32)
    maps = []
    for c in range(n_cores):
        m = dict(sh)
        m["xT"] = np.ascontiguousarray(x[c].T)
        m["xtok"] = np.ascontiguousarray(x[c])
        m["pos"] = np.ascontiguousarray(posn[c])
        maps.append(m)
    return maps


def kernel(**inputs):
    nc = build_program(debug=False)
    maps = make_in_maps(inputs, 8)
    res = run_bass_kernel_spmd(nc, maps, core_ids=list(range(8)))
    out = np.stack([np.asarray(r["y"], dtype=np.float32) for r in res.results], axis=0)
    return out.reshape(8, T, D)
```
